# Optimizing a Trainium2 kernel written in Bass

```python
import math
import jax
import jax.numpy as jnp
from jax import lax
import numpy as np

D_MODEL = 1024
BATCH = 4
SEQ = 4096
DEPTH = 2

RMS_EPS = 1e-6
GN_EPS = 1e-5
ATTN_BLOCK = 128

RET_HEADS = 4
RET_DK = 64
RET_DV = 64
RET_CHUNK = 128
RET_THETA = 10000.0

MLA_HEADS = 4
MLA_Q_RANK = 256
MLA_KV_RANK = 128
MLA_NOPE = 64
MLA_ROPE = 32
MLA_V = 64

SSM_HEADS = 4
SSM_HEAD_DIM = 64
SSM_GROUPS = 2
SSM_STATE = 128
SSM_CONV = 5
SSM_CHUNK = 128
SSM_INNER = SSM_HEADS * SSM_HEAD_DIM
SSM_CONV_DIM = SSM_INNER + 2 * SSM_GROUPS * SSM_STATE

DIL_HEADS = 4
DIL_HEAD_DIM = 64
DIL_PATTERNS = ((128, 1), (512, 4), (2048, 16))

ROPE_THETA = 500000.0
ROPE_DIM = DIL_HEAD_DIM // 4

N_EXPERTS = 16
EXPERT_FF = 2048
EC_CAPACITY_FACTOR = 2

PROJ_SIZES = (
    RET_HEADS * RET_DK, RET_HEADS * RET_DK, RET_HEADS * RET_DV, RET_HEADS * RET_DV,
    MLA_Q_RANK, MLA_KV_RANK, MLA_ROPE,
    SSM_INNER, SSM_CONV_DIM, 2 * SSM_HEADS,
    DIL_HEADS * DIL_HEAD_DIM, DIL_HEADS * DIL_HEAD_DIM, DIL_HEADS * DIL_HEAD_DIM,
)
PROJ_WIDTH = sum(PROJ_SIZES)
MIX_WIDTH = RET_HEADS * RET_DV + MLA_HEADS * MLA_V + SSM_INNER + DIL_HEADS * DIL_HEAD_DIM

kernel_name = 'hybrid_parallel_mixer_ec_moe_encoder'


def _split_points():
    pts, acc = [], 0
    for size in PROJ_SIZES[:-1]:
        acc += size
        pts.append(acc)
    return pts


def rms_norm(x, w, eps=RMS_EPS):
    xf = x.astype(jnp.float32)
    y = xf * lax.rsqrt(jnp.mean(xf * xf, axis=-1, keepdims=True) + eps)
    return (y * w.astype(jnp.float32)).astype(x.dtype)


def rotary(x, rot_dim, theta):
    s = x.shape[-2]
    inv = 1.0 / (theta ** (jnp.arange(0, rot_dim, 2, dtype=jnp.float32) / rot_dim))
    ang = jnp.arange(s, dtype=jnp.float32)[:, None] * inv[None, :]
    cos, sin = jnp.cos(ang), jnp.sin(ang)
    xr = x[..., :rot_dim].astype(jnp.float32)
    x1, x2 = xr[..., :rot_dim // 2], xr[..., rot_dim // 2:]
    rot = jnp.concatenate([x1 * cos - x2 * sin, x2 * cos + x1 * sin], axis=-1).astype(x.dtype)
    return jnp.concatenate([rot, x[..., rot_dim:]], axis=-1)


def split_heads(t, n_heads):
    b, s, w = t.shape
    return t.reshape(b, s, n_heads, w // n_heads).transpose(0, 2, 1, 3)


def merge_heads(t):
    b, h, s, d = t.shape
    return t.transpose(0, 2, 1, 3).reshape(b, s, h * d)


def retention_log_decays(offset):
    exps = -5.0 - offset - jnp.arange(RET_HEADS, dtype=jnp.float32)
    return jnp.log1p(-jnp.exp2(exps))


def retention_direction(q, k, v, log_gamma, strict):
    b, h, s, dk = q.shape
    dv = v.shape[-1]
    nc = s // RET_CHUNK
    qc = q.reshape(b, h, nc, RET_CHUNK, dk)
    kc = k.reshape(b, h, nc, RET_CHUNK, dk)
    vc = v.reshape(b, h, nc, RET_CHUNK, dv)
    pos = jnp.arange(RET_CHUNK, dtype=jnp.float32)
    diff = pos[:, None] - pos[None, :]
    mask = diff > 0 if strict else diff >= 0
    decay = jnp.where(mask, jnp.exp(log_gamma[:, None, None] * jnp.maximum(diff, 0.0)), 0.0)
    scores = jnp.einsum('bhcid,bhcjd->bhcij', qc, kc) * decay[None, :, None]
    inner = jnp.einsum('bhcij,bhcje->bhcie', scores, vc)
    zeta = jnp.exp(log_gamma[:, None] * (RET_CHUNK - 1.0 - pos))
    xi = jnp.exp(log_gamma[:, None] * (pos + 1.0))
    chunk_decay = jnp.exp(log_gamma * RET_CHUNK)[None, :, None, None]
    updates = jnp.einsum('bhcjd,hj,bhcje->cbhde', kc, zeta, vc)

    def step(state, upd):
        return state * chunk_decay + upd, state

    _, prev = lax.scan(step, jnp.zeros((b, h, dk, dv), jnp.float32), updates)
    cross = jnp.einsum('bhcid,cbhde,hi->bhcie', qc, prev, xi)
    return (inner + cross).reshape(b, h, s, dv)


def retention_mixer(q, k, v, g):
    dtype = g.dtype
    q = rotary(split_heads(q, RET_HEADS).astype(jnp.float32), RET_DK, RET_THETA) * (RET_DK ** -0.5)
    k = rotary(split_heads(k, RET_HEADS).astype(jnp.float32), RET_DK, RET_THETA)
    v = split_heads(v, RET_HEADS).astype(jnp.float32)
    flip = lambda t: jnp.flip(t, axis=2)
    o = (retention_direction(q, k, v, retention_log_decays(0.0), False)
         + flip(retention_direction(flip(q), flip(k), flip(v), retention_log_decays(0.5), True)))
    mu = jnp.mean(o, axis=-1, keepdims=True)
    var = jnp.mean(jnp.square(o - mu), axis=-1, keepdims=True)
    o = (o - mu) * lax.rsqrt(var + GN_EPS)
    return (jax.nn.silu(g.astype(jnp.float32)) * merge_heads(o)).astype(dtype)


def blocked_dense_attention(q, k, v, scale):
    b, h, s, d = q.shape
    dv = v.shape[-1]
    nb = s // ATTN_BLOCK
    qb = jnp.moveaxis(q.reshape(b, h, nb, ATTN_BLOCK, d), 2, 0)

    def block(qi):
        sc = jnp.einsum('bhqd,bhkd->bhqk', qi, k).astype(jnp.float32) * scale
        p = jax.nn.softmax(sc, axis=-1)
        return jnp.einsum('bhqk,bhkd->bhqd', p.astype(v.dtype), v)

    o = lax.map(block, qb)
    return jnp.moveaxis(o, 0, 2).reshape(b, h, s, dv)


def mla_mixer(c_q, c_kv, k_rope, q_norm_w, kv_norm_w, w_uq, w_ukv):
    b, s, _ = c_q.shape
    q = split_heads(rms_norm(c_q, q_norm_w) @ w_uq, MLA_HEADS)
    kv = split_heads(rms_norm(c_kv, kv_norm_w) @ w_ukv, MLA_HEADS)
    k_nope, v = kv[..., :MLA_NOPE], kv[..., MLA_NOPE:]
    q = jnp.concatenate([q[..., :MLA_NOPE], rotary(q[..., MLA_NOPE:], MLA_ROPE, ROPE_THETA)], axis=-1)
    kr = rotary(k_rope[:, None], MLA_ROPE, ROPE_THETA)
    k = jnp.concatenate([k_nope, jnp.broadcast_to(kr, (b, MLA_HEADS, s, MLA_ROPE))], axis=-1)
    o = blocked_dense_attention(q, k, v, (MLA_NOPE + MLA_ROPE) ** -0.5)
    return merge_heads(o)


def ssd_direction(x, dt, a, bm, cm, strict):
    b, s, h, p = x.shape
    n = bm.shape[-1]
    nc = s // SSM_CHUNK
    rep = h // SSM_GROUPS
    bh = jnp.repeat(bm, rep, axis=2).reshape(b, nc, SSM_CHUNK, h, n)
    ch = jnp.repeat(cm, rep, axis=2).reshape(b, nc, SSM_CHUNK, h, n)
    xdt = (x * dt[..., None]).reshape(b, nc, SSM_CHUNK, h, p)
    cs = jnp.cumsum((dt * a).reshape(b, nc, SSM_CHUNK, h), axis=2)
    seg = cs[:, :, :, None, :] - cs[:, :, None, :, :]
    pos = jnp.arange(SSM_CHUNK)
    mask = pos[:, None] > pos[None, :] if strict else pos[:, None] >= pos[None, :]
    decay = jnp.where(mask[None, None, :, :, None], jnp.exp(jnp.minimum(seg, 0.0)), 0.0)
    scores = jnp.einsum('bclhn,bcshn->bclsh', ch, bh) * decay
    y_diag = jnp.einsum('bclsh,bcshp->bclhp', scores, xdt)
    to_end = jnp.exp(cs[:, :, -1:, :] - cs)
    states = jnp.einsum('bclhn,bclh,bclhp->cbhpn', bh, to_end, xdt)
    chunk_decay = jnp.moveaxis(jnp.exp(cs[:, :, -1, :]), 1, 0)

    def step(state, inp):
        upd, dec = inp
        return state * dec[:, :, None, None] + upd, state

    _, prev = lax.scan(step, jnp.zeros((b, h, p, n), jnp.float32), (states, chunk_decay))
    y_off = jnp.einsum('bclhn,cbhpn,bclh->bclhp', ch, prev, jnp.exp(cs))
    return (y_diag + y_off).reshape(b, s, h, p)


def mamba2_mixer(z, xbc, dt, conv_w, conv_b, a_log, dt_bias, d_skip, norm_w):
    b, s, _ = z.shape
    pad = SSM_CONV // 2
    xbc = lax.conv_general_dilated(xbc, conv_w[:, None, :].astype(xbc.dtype), (1,), [(pad, pad)],
                                   dimension_numbers=('NWC', 'WIO', 'NWC'),
                                   feature_group_count=SSM_CONV_DIM)
    xbc = jax.nn.silu((xbc + conv_b.astype(xbc.dtype)).astype(jnp.float32))
    xs = xbc[..., :SSM_INNER].reshape(b, s, SSM_HEADS, SSM_HEAD_DIM)
    bm = xbc[..., SSM_INNER:SSM_INNER + SSM_GROUPS * SSM_STATE].reshape(b, s, SSM_GROUPS, SSM_STATE)
    cm = xbc[..., SSM_INNER + SSM_GROUPS * SSM_STATE:].reshape(b, s, SSM_GROUPS, SSM_STATE)
    dt = jax.nn.softplus(dt.astype(jnp.float32).reshape(b, s, 2, SSM_HEADS) + dt_bias.astype(jnp.float32))
    a = -jnp.exp(a_log.astype(jnp.float32))
    flip = lambda t: jnp.flip(t, axis=1)
    y_fwd = ssd_direction(xs, dt[:, :, 0], a[0], bm, cm, False)
    y_bwd = flip(ssd_direction(flip(xs), flip(dt[:, :, 1]), a[1], flip(bm), flip(cm), True))
    y = y_fwd + y_bwd + d_skip.astype(jnp.float32)[:, None] * xs
    y = y.reshape(b, s, SSM_INNER) * jax.nn.silu(z.astype(jnp.float32))
    return rms_norm(y, norm_w).astype(z.dtype)


def dilated_attention(q, k, v, scale):
    b, h, s, d = q.shape
    nb = s // ATTN_BLOCK
    offsets = [dil * jnp.arange(-(win // (2 * dil)), win // (2 * dil) + 1) for win, dil in DIL_PATTERNS]

    def block(i):
        start = i * ATTN_BLOCK
        qi = lax.dynamic_slice_in_dim(q, start, ATTN_BLOCK, axis=2).astype(jnp.float32)
        qpos = start + jnp.arange(ATTN_BLOCK)
        maxes, denoms, outs = [], [], []
        for off in offsets:
            kpos = qpos[:, None] + off[None, :]
            valid = (kpos >= 0) & (kpos < s)
            kpos = jnp.clip(kpos, 0, s - 1)
            kg = jnp.take(k, kpos, axis=2).astype(jnp.float32)
            vg = jnp.take(v, kpos, axis=2).astype(jnp.float32)
            sc = jnp.einsum('bhqd,bhqkd->bhqk', qi, kg) * scale
            sc = jnp.where(valid, sc, -jnp.inf)
            m = jnp.max(sc, axis=-1, keepdims=True)
            p = jnp.exp(sc - m)
            l = jnp.sum(p, axis=-1, keepdims=True)
            outs.append(jnp.einsum('bhqk,bhqkd->bhqd', p, vg) / l)
            maxes.append(m)
            denoms.append(l)
        m_st = jnp.stack(maxes)
        w = jnp.stack(denoms) * jnp.exp(m_st - jnp.max(m_st, axis=0, keepdims=True))
        return (jnp.sum(w * jnp.stack(outs), axis=0) / jnp.sum(w, axis=0)).astype(q.dtype)

    o = lax.map(block, jnp.arange(nb))
    return jnp.moveaxis(o, 0, 2).reshape(b, h, s, d)


def dilated_mixer(q, k, v):
    q = rotary(split_heads(q, DIL_HEADS), ROPE_DIM, ROPE_THETA)
    k = rotary(split_heads(k, DIL_HEADS), ROPE_DIM, ROPE_THETA)
    v = split_heads(v, DIL_HEADS)
    return merge_heads(dilated_attention(q, k, v, DIL_HEAD_DIM ** -0.5))


def expert_choice_ffn(h, router_w, w_gate, w_up, w_down):
    b, s, d = h.shape
    capacity = EC_CAPACITY_FACTOR * s // N_EXPERTS
    logits = jnp.einsum('bsd,de->bse', h, router_w).astype(jnp.float32)
    affinity = jax.nn.softmax(logits, axis=-1)
    gate, token_idx = lax.top_k(jnp.swapaxes(affinity, 1, 2), capacity)
    xin = jax.vmap(lambda hb, ib: hb[ib])(h, token_idx)
    hid = (jax.nn.silu(jnp.einsum('becd,edf->becf', xin, w_gate))
           * jnp.einsum('becd,edf->becf', xin, w_up))
    out = jnp.einsum('becf,efd->becd', hid, w_down) * gate[..., None].astype(h.dtype)

    def combine(ob, ib):
        return jnp.zeros((s, d), ob.dtype).at[ib.reshape(-1)].add(ob.reshape(-1, d))

    return jax.vmap(combine)(out, token_idx)


def setup_inputs(seed: int = 0) -> dict:
    key = jax.random.key(seed)
    ks = jax.random.split(key, 20)
    f32 = jnp.float32

    def dense(k, shape, fan_in):
        return jax.random.normal(k, shape, f32) * (fan_in ** -0.5)

    def gain(k, shape):
        return 1.0 + 0.02 * jax.random.normal(k, shape, f32)

    dt_init = jnp.exp(jax.random.uniform(ks[10], (DEPTH, 2, SSM_HEADS), f32,
                                         math.log(1e-3), math.log(1e-1)))
    return {
        'x': jax.random.normal(ks[0], (BATCH, SEQ, D_MODEL), f32),
        'ln1_w': gain(ks[1], (DEPTH, D_MODEL)),
        'w_in': dense(ks[2], (DEPTH, D_MODEL, PROJ_WIDTH), D_MODEL),
        'mla_q_norm_w': gain(ks[3], (DEPTH, MLA_Q_RANK)),
        'mla_kv_norm_w': gain(ks[4], (DEPTH, MLA_KV_RANK)),
        'mla_w_uq': dense(ks[5], (DEPTH, MLA_Q_RANK, MLA_HEADS * (MLA_NOPE + MLA_ROPE)), MLA_Q_RANK),
        'mla_w_ukv': dense(ks[6], (DEPTH, MLA_KV_RANK, MLA_HEADS * (MLA_NOPE + MLA_V)), MLA_KV_RANK),
        'ssm_conv_w': dense(ks[7], (DEPTH, SSM_CONV, SSM_CONV_DIM), SSM_CONV),
        'ssm_conv_b': 0.02 * jax.random.normal(ks[8], (DEPTH, SSM_CONV_DIM), f32),
        'ssm_a_log': jnp.log(jax.random.uniform(ks[9], (DEPTH, 2, SSM_HEADS), f32, 1.0, 16.0)),
        'ssm_dt_bias': dt_init + jnp.log(-jnp.expm1(-dt_init)),
        'ssm_d': gain(ks[11], (DEPTH, SSM_HEADS)),
        'ssm_norm_w': gain(ks[12], (DEPTH, SSM_INNER)),
        'w_out': dense(ks[13], (DEPTH, MIX_WIDTH, D_MODEL), MIX_WIDTH),
        'ln2_w': gain(ks[14], (DEPTH, D_MODEL)),
        'router_w': dense(ks[15], (DEPTH, D_MODEL, N_EXPERTS), D_MODEL),
        'exp_w_gate': dense(ks[16], (DEPTH, N_EXPERTS, D_MODEL, EXPERT_FF), D_MODEL),
        'exp_w_up': dense(ks[17], (DEPTH, N_EXPERTS, D_MODEL, EXPERT_FF), D_MODEL),
        'exp_w_down': dense(ks[18], (DEPTH, N_EXPERTS, EXPERT_FF, D_MODEL), EXPERT_FF),
        'final_norm_w': gain(ks[19], (D_MODEL,)),
    }


def reference(x, ln1_w, w_in, mla_q_norm_w, mla_kv_norm_w, mla_w_uq, mla_w_ukv,
              ssm_conv_w, ssm_conv_b, ssm_a_log, ssm_dt_bias, ssm_d, ssm_norm_w,
              w_out, ln2_w, router_w, exp_w_gate, exp_w_up, exp_w_down, final_norm_w):
    split_pts = _split_points()
    for i in range(DEPTH):
        hn = rms_norm(x, ln1_w[i])
        proj = hn @ w_in[i]
        (r_q, r_k, r_v, r_g, m_cq, m_ckv, m_kr,
         s_z, s_xbc, s_dt, d_q, d_k, d_v) = jnp.split(proj, split_pts, axis=-1)
        y_a = retention_mixer(r_q, r_k, r_v, r_g)
        y_b = mla_mixer(m_cq, m_ckv, m_kr, mla_q_norm_w[i], mla_kv_norm_w[i], mla_w_uq[i], mla_w_ukv[i])
        y_c = mamba2_mixer(s_z, s_xbc, s_dt, ssm_conv_w[i], ssm_conv_b[i], ssm_a_log[i],
                           ssm_dt_bias[i], ssm_d[i], ssm_norm_w[i])
        y_d = dilated_mixer(d_q, d_k, d_v)
        mixed = jnp.concatenate([y_a, y_b.astype(x.dtype), y_c, y_d.astype(x.dtype)], axis=-1)
        x = x + mixed @ w_out[i]
        x = x + expert_choice_ffn(rms_norm(x, ln2_w[i]), router_w[i], exp_w_gate[i],
                                  exp_w_up[i], exp_w_down[i])
    return rms_norm(x, final_norm_w)
```

```python
import numpy as np
from contextlib import ExitStack
import concourse.bass as bass
import concourse.mybir as mybir
from concourse.bass_utils import run_bass_kernel_spmd
F32 = mybir.dt.float32
BF16 = mybir.dt.bfloat16
U32 = mybir.dt.uint32
AF = mybir.ActivationFunctionType
ALU = mybir.AluOpType
AX = mybir.AxisListType


CE = ("tensor", "vector", "scalar", "gpsimd")
ENGS = ("sync",) + CE
NDS = 48


def _key(x):
    if isinstance(x, str):
        return x
    if isinstance(x, tuple):
        return x[1]
    return x.tensor.name


def _ap(x):
    return x[0] if isinstance(x, tuple) else x


class Sched:
    def __init__(self, nc, es):
        self.nc, self.es = nc, es
        self.q = {e: [] for e in ENGS}
        self.cnt = {e: 0 for e in CE}
        self.sem = {e: es.enter_context(nc.semaphore("s_" + e)) for e in CE}
        self.dsem = [es.enter_context(nc.semaphore("d%d" % i)) for i in range(NDS)]
        self.dcnt = [0] * NDS
        self.dnext = 0
        self.seen_c = {e: {f: 0 for f in CE} for e in ENGS}
        self.seen_d = {e: [0] * NDS for e in ENGS}
        self.last_w = {}
        self.readers = {}
        self.n_alloc = 0
        self.psum_keys = set()
        self.ninstr = 0
        self.prefix = ""
        self.root_es = es
        self.epoch = 0

    def sb(self, shape, dtype, name=None):
        self.n_alloc += 1
        name = self.prefix + (name or "t%d" % self.n_alloc)
        return self.es.enter_context(self.nc.sbuf_tensor(name, list(shape), dtype))

    def ps(self, shape, dtype, name=None):
        self.n_alloc += 1
        name = self.prefix + (name or "p%d" % self.n_alloc)
        t = self.es.enter_context(self.nc.psum_tensor(name, [128, 512], mybir.dt.float32))
        self.psum_keys.add(name)
        return t[0:shape[0], 0:shape[1]]

    def scope(self, prefix):
        sched = self

        class _Scope:
            def __enter__(self_):
                self_.old = (sched.es, sched.prefix)
                self_.stack = ExitStack()
                self_.stack.__enter__()
                sched.es, sched.prefix = self_.stack, prefix
                return sched

            def __exit__(self_, *a):
                sched.barrier()
                sched.es, sched.prefix = self_.old
                return self_.stack.__exit__(*a)
        return _Scope()

    def _wait(self, stream, tok):
        if tok is None:
            return
        if tok[0] == "c":
            _, eng, n, ep = tok
            if ep < self.epoch:
                return
            if stream == "tensor" and eng == "tensor":
                return
            if self.seen_c[stream][eng] >= n:
                return
            self.seen_c[stream][eng] = n
            sem = self.sem[eng]
            self.q[stream].append(lambda e, sem=sem, n=n: e.wait_ge(sem, n))
        else:
            _, i, n = tok
            if self.seen_d[stream][i] >= n:
                return
            self.seen_d[stream][i] = n
            sem = self.dsem[i]
            self.q[stream].append(lambda e, sem=sem, n=n: e.wait_ge(sem, n))

    def _deps(self, stream, reads, writes):
        for r in reads:
            self._wait(stream, self.last_w.get(_key(r)))
        for w in writes:
            k = _key(w)
            self._wait(stream, self.last_w.get(k))
            for t in self.readers.get(k, ()):
                self._wait(stream, t)

    def _commit(self, tok, reads, writes):
        for r in reads:
            self.readers.setdefault(_key(r), []).append(tok)
        for w in writes:
            k = _key(w)
            self.last_w[k] = tok
            self.readers[k] = []

    def op(self, eng, fn, reads=(), writes=()):
        extra = [r for r in reads if _key(r) in self.psum_keys]
        if extra:
            writes = list(writes) + extra
        self._deps(eng, reads, writes)
        self.cnt[eng] += 1
        n = self.cnt[eng]
        sem = self.sem[eng]
        self.q[eng].append(lambda e, fn=fn, sem=sem: fn(e).then_inc(sem, 1))
        self.seen_c[eng][eng] = max(self.seen_c[eng][eng], 0)
        self._commit(("c", eng, n, self.epoch), reads, writes)
        self.ninstr += 1

    def dma(self, out, in_, queue="sync", reads=None, writes=None, **kw):
        reads = [in_] if reads is None else reads
        writes = [out] if writes is None else writes
        self._deps(queue, reads, writes)
        i = self.dnext
        self.dnext = (self.dnext + 1) % NDS
        if self.dcnt[i]:
            self._wait(queue, ("d", i, self.dcnt[i]))
        self.dcnt[i] += 16
        sem = self.dsem[i]
        o, a = _ap(out), _ap(in_)
        self.q[queue].append(
            lambda e, o=o, a=a, sem=sem, kw=kw: e.dma_start(out=o, in_=a, **kw).then_inc(sem, 16))
        self._commit(("d", i, self.dcnt[i]), reads, writes)
        self.ninstr += 1

    def raw_dma(self, queue, fn, reads, writes):
        self._deps(queue, reads, writes)
        i = self.dnext
        self.dnext = (self.dnext + 1) % NDS
        if self.dcnt[i]:
            self._wait(queue, ("d", i, self.dcnt[i]))
        self.dcnt[i] += 16
        sem = self.dsem[i]
        self.q[queue].append(lambda e, fn=fn, sem=sem: fn(e).then_inc(sem, 16))
        self._commit(("d", i, self.dcnt[i]), reads, writes)
        self.ninstr += 1

    def barrier(self):
        for s in ENGS:
            for e in CE:
                if self.cnt[e]:
                    self._wait(s, ("c", e, self.cnt[e], self.epoch))
            for i in range(NDS):
                if self.dcnt[i]:
                    self._wait(s, ("d", i, self.dcnt[i]))

    def new_epoch(self):
        self.barrier()
        self.epoch += 1
        self.sem = {e: self.root_es.enter_context(self.nc.semaphore("s%d_%s" % (self.epoch, e))) for e in CE}
        self.cnt = {e: 0 for e in CE}
        self.seen_c = {e: {f: 0 for f in CE} for e in ENGS}

    def finish(self):
        for e in CE:
            if self.cnt[e]:
                self._wait("sync", ("c", e, self.cnt[e], self.epoch))
        for i in range(NDS):
            if self.dcnt[i]:
                self._wait("sync", ("d", i, self.dcnt[i]))
        with self.nc.Block() as block:
            @block.sync
            def _(e):
                for f in self.q["sync"]:
                    f(e)

            @block.tensor
            def _(e):
                for f in self.q["tensor"]:
                    f(e)

            @block.vector
            def _(e):
                for f in self.q["vector"]:
                    f(e)

            @block.scalar
            def _(e):
                for f in self.q["scalar"]:
                    f(e)

            @block.gpsimd
            def _(e):
                for f in self.q["gpsimd"]:
                    f(e)

    def mm(self, out, lhsT, rhs, start=True, stop=True, **kw):
        o, l, r = _ap(out), _ap(lhsT), _ap(rhs)
        self.op("tensor", lambda e: e.matmul(o, l, r, start=start, stop=stop, **kw),
                reads=[lhsT, rhs] + ([] if start else [out]), writes=[out])

    def tr(self, out, in_, ident):
        o, a, i = _ap(out), _ap(in_), _ap(ident)
        self.op("tensor", lambda e: e.transpose(o, a, i), reads=[in_, ident], writes=[out])

    def act(self, out, in_, func, bias=None, scale=None, accum_out=None, eng="scalar", extra_reads=()):
        o, a = _ap(out), _ap(in_)
        kw = {}
        reads = [in_] + list(extra_reads)
        writes = [out]
        if bias is not None:
            kw["bias"] = _ap(bias) if not isinstance(bias, (int, float)) else bias
            if not isinstance(bias, (int, float)):
                reads.append(bias)
        if scale is not None:
            kw["scale"] = _ap(scale) if not isinstance(scale, (int, float)) else scale
            if not isinstance(scale, (int, float)):
                reads.append(scale)
        if accum_out is not None:
            kw["accum_out"] = _ap(accum_out)
            writes.append(accum_out)
        self.op("scalar", lambda e: e.activation(o, a, func, **kw), reads=reads, writes=writes)

    def tt(self, out, in0, in1, op, eng="vector"):
        o, a, b = _ap(out), _ap(in0), _ap(in1)
        self.op(eng, lambda e: e.tensor_tensor(o, a, b, op), reads=[in0, in1], writes=[out])

    def ts(self, out, in0, s1, s2, op0, op1=None, eng="vector", accum_out=None):
        o, a = _ap(out), _ap(in0)
        reads = [in0]
        v1 = s1
        if not isinstance(s1, (int, float)):
            reads.append(s1)
            v1 = _ap(s1)
        v2 = s2
        if s2 is not None and not isinstance(s2, (int, float)):
            reads.append(s2)
            v2 = _ap(s2)
        writes = [out]
        kw = {}
        if accum_out is not None:
            kw["accum_out"] = _ap(accum_out)
            writes.append(accum_out)
        if op1 is None:
            self.op(eng, lambda e: e.tensor_scalar(o, a, v1, v2, op0, **kw), reads=reads, writes=writes)
        else:
            self.op(eng, lambda e: e.tensor_scalar(o, a, v1, v2, op0, op1, **kw), reads=reads, writes=writes)

    def stt(self, out, in0, scalar, in1, op0, op1, eng="vector"):
        o, a, b = _ap(out), _ap(in0), _ap(in1)
        reads = [in0, in1]
        v = scalar
        if not isinstance(scalar, (int, float)):
            reads.append(scalar)
            v = _ap(scalar)
        self.op(eng, lambda e: e.scalar_tensor_tensor(o, a, v, b, op0, op1), reads=reads, writes=[out])

    def copy(self, out, in_, eng="vector"):
        o, a = _ap(out), _ap(in_)
        if eng == "scalar":
            self.op(eng, lambda e: e.copy(o, a), reads=[in_], writes=[out])
        else:
            self.op(eng, lambda e: e.tensor_copy(o, a), reads=[in_], writes=[out])

    def memset(self, out, val, eng="vector"):
        o = _ap(out)
        self.op(eng, lambda e: e.memset(o, val), reads=[], writes=[out])


def half_swap(a, lo, hi):
    b = a.copy()
    m = (lo + hi) // 2
    b[..., lo:m, :] = a[..., m:hi, :]
    b[..., m:hi, :] = a[..., lo:m, :]
    return b

def rot_tables(dk, L, lo, hi, theta, scale):
    C = np.ones((dk, L), np.float64)
    Sg = np.zeros((dk, L), np.float64)
    rd = hi - lo
    inv = 1.0 / (theta ** (np.arange(0, rd, 2, dtype=np.float32) / np.float32(rd))).astype(np.float32)
    ang = (np.arange(L, dtype=np.float32)[:, None] * inv[None, :]).astype(np.float32)
    c, s = np.cos(ang.astype(np.float64)).T, np.sin(ang.astype(np.float64)).T
    C[lo:lo + rd // 2] = c
    C[lo + rd // 2:hi] = c
    Sg[lo:lo + rd // 2] = -s
    Sg[lo + rd // 2:hi] = s
    return (C * scale).astype(np.float32), (Sg * scale).astype(np.float32)

def dil_masks():
    M = np.zeros((20, 128, 512), np.float32)
    j = np.arange(128)[:, None]
    i = np.arange(512)[None, :]
    for rr in range(20):
        d_ = 128 * (rr - 8) + j - i
        for (win, dil) in ((128, 1), (512, 4), (2048, 16)):
            M[rr] += ((d_ % dil == 0) & (np.abs(d_) <= (win // (2 * dil)) * dil)).astype(np.float32)
    return M

def dil_plan():
    return [[(t, t - 4 * g + 8) for t in range(4 * g - 8, 4 * g + 12) if 0 <= t < 32] for g in range(8)]

def mla_plan():
    return [[(t, None) for t in range(32)] for g in range(8)]


def ret_consts(L, heads):
    NH = len(heads)
    inv = 1.0 / (10000.0 ** (np.arange(0, 64, 2, dtype=np.float32) / np.float32(64))).astype(np.float32)
    ang = (np.arange(L, dtype=np.float32)[:, None] * inv[None, :]).astype(np.float32).astype(np.float64)
    c, s = np.cos(ang), np.sin(ang)
    ct = np.concatenate([c, c], 1); st = np.concatenate([-s, s], 1)
    ck, sk = ct.T.copy(), st.T.copy()
    dtot = np.zeros((NH, 128, 128)); cols = np.zeros((128, NH * 6))
    p = np.arange(128, dtype=np.float64)
    for n, h in enumerate(heads):
        lgf = np.log1p(-np.exp2(np.float64(-5.0 - 0.0 - h))); lgb = np.log1p(-np.exp2(np.float64(-5.0 - 0.5 - h)))
        j = p[:, None]; i = p[None, :]
        dtot[n] = 0.125 * (np.where(i >= j, np.exp(lgf * np.maximum(i - j, 0)), 0.0) + np.where(j > i, np.exp(lgb * np.maximum(j - i, 0)), 0.0))
        cols[:, n * 6 + 0] = 0.125 * np.exp(lgf * (p + 1))
        cols[:, n * 6 + 1] = 0.125 * np.exp(lgb * (128 - p))
        cols[:, n * 6 + 2] = np.exp(lgf * (127 - p))
        cols[:, n * 6 + 3] = np.exp(lgb * p)
        cols[:, n * 6 + 4] = np.exp(lgf * 128)
        cols[:, n * 6 + 5] = np.exp(lgb * 128)
    f = lambda a: np.ascontiguousarray(a, dtype=np.float32)
    return dict(ck=f(ck), sk=f(sk), ct=f(ct), st=f(st), dtot=f(dtot), cols=f(cols))

def swap64(a, axis):
    return np.concatenate([np.take(a, range(32, 64), axis), np.take(a, range(0, 32), axis)], axis)


def ssd_consts():
    s = np.arange(128)[:, None]; l = np.arange(128)[None, :]
    return np.stack([(s <= l), (s >= l), (l >= s), (l < s), (s == l)]).astype(np.float32)


def emit_lin(S, xT, w, g, out, K, ntok, N, norm=True, softmax=False, fp32=False, eps=1e-6, tag="o", NB=2):
    KT = K // 128
    MD = F32 if fp32 else BF16
    W = S.sb([128, KT, N], MD, "W")
    Wraw = [S.sb([128, N], F32, "Wraw%d" % i) for i in range(2)]
    gs = S.sb([128, KT], F32, "gs")
    ones = S.sb([128, 1], F32, "ones")
    S.memset(ones[:], 1.0)
    S.dma(gs[:], g.rearrange("(k p) -> p k", p=128), allow_slow_non_contiguous=True)
    wv = w.rearrange("(k p) n -> p k n", p=128)
    for k in range(KT):
        S.dma(Wraw[k % 2][:], wv[:, k, :], queue="sync" if k % 2 == 0 else "gpsimd")
        S.ts((W[:, k, :], "W%d" % k), Wraw[k % 2][:], gs[:, k:k + 1], None, ALU.mult,
             eng="vector" if k % 2 == 0 else "gpsimd")
    x32 = [S.sb([128, KT, 128], F32, "x32_%d" % i) for i in range(NB)]
    xt = x32 if fp32 else [S.sb([128, KT, 128], BF16, "xt%d" % i) for i in range(NB)]
    sq = [S.sb([128, KT, 128], F32, "sq%d" % i) for i in range(NB)]
    ob = [S.sb([128, N], F32, "ob%d" % i) for i in range(NB)]
    rstd = [S.sb([128, 1], F32, "rstd%d" % i) for i in range(NB)]
    mx = [S.sb([128, 1], F32, "mx%d" % i) for i in range(NB)]
    sm = [S.sb([128, 1], F32, "sm%d" % i) for i in range(NB)]
    ssp = S.ps([128, 1], F32, "ssp")
    pp = [S.ps([128, 512], F32, "pp%d" % i) for i in range(4)]
    xv = xT.rearrange("(k p) t -> p k t", p=128)
    groups = [(c, min(512, N - c)) for c in range(0, N, 512)]
    gi = 0
    for t in range(ntok // 128):
        b = t % NB
        S.dma(x32[b][:], xv[:, :, t * 128:(t + 1) * 128], queue="sync")
        if not fp32:
            S.copy(xt[b][:], x32[b][:], eng="gpsimd")
        if norm:
            S.act(sq[b][:], x32[b][:], AF.Square)
            for k in range(KT):
                S.mm(ssp, sq[b][:, k, :], ones[:], start=(k == 0), stop=(k == KT - 1))
            S.ts(rstd[b][:], ssp, 1.0 / K, eps, ALU.mult, ALU.add)
            S.act(rstd[b][:], rstd[b][:], AF.Sqrt)
            S.op("vector", lambda e, b=b: e.reciprocal(rstd[b][:], rstd[b][:]), reads=[rstd[b][:]], writes=[rstd[b][:]])
        else:
            S.memset(rstd[b][:], 1.0)
        keys = []
        for (c0, cw) in groups:
            p = pp[gi % 4]
            for k in range(KT):
                S.op("tensor", (lambda e, p=p, b=b, k=k, c0=c0, cw=cw: e.matmul(p[:, :cw], xt[b][:, k, :], W[:, k, c0:c0 + cw], start=(k == 0), stop=(k == KT - 1))),
                     reads=[xt[b][:], "W%d" % k], writes=[p])
            key = "ob%d_%d" % (b, c0)
            keys.append(key)
            if gi % 2 == 0:
                S.act((ob[b][:, c0:c0 + cw], key), p[:, :cw], AF.Copy, scale=rstd[b][:])
            else:
                S.ts((ob[b][:, c0:c0 + cw], key), p[:, :cw], rstd[b][:], None, ALU.mult)
            gi += 1
        if softmax:
            key = keys[0]
            S.op("vector", lambda e, b=b: e.reduce_max(mx[b][:], ob[b][:], AX.X), reads=[key], writes=[mx[b][:]])
            S.ts(mx[b][:], mx[b][:], -1.0, None, ALU.mult)
            S.op("scalar", lambda e, b=b: e.activation(ob[b][:], ob[b][:], AF.Exp, bias=mx[b][:], accum_out=sm[b][:]),
                 reads=[key, mx[b][:]], writes=[key, sm[b][:]])
            S.op("vector", lambda e, b=b: e.reciprocal(sm[b][:], sm[b][:]), reads=[sm[b][:]], writes=[sm[b][:]])
            S.op("vector", lambda e, b=b: e.tensor_scalar(ob[b][:], ob[b][:], sm[b][:], None, ALU.mult), reads=[key, sm[b][:]], writes=[key])
        wk = "%s_w%d" % (tag, t)
        S.dma(out[t * 128:(t + 1) * 128, :], ob[b][:], queue="gpsimd", reads=keys, writes=[wk])
        for key in keys:
            S.readers.setdefault(key, []).append(S.last_w[wk])


def emit_linF(S, xT, w, g, outT, K, ntok, N, norm=True, resid=None, eps=1e-6, tag="f"):
    KT = K // 128
    W = S.sb([128, KT, N], BF16, "W")
    Wraw = [S.sb([128, N], F32, "Wraw%d" % i) for i in range(2)]
    gs = S.sb([128, KT], F32, "gs")
    ones = S.sb([128, 128], F32, "ones")
    S.memset(ones[:], 1.0)
    S.dma(gs[:], g.rearrange("(k p) -> p k", p=128), allow_slow_non_contiguous=True)
    wv = w.rearrange("(k p) n -> p k n", p=128)
    for k in range(KT):
        S.dma(Wraw[k % 2][:], wv[:, k, :], queue="sync" if k % 2 == 0 else "gpsimd")
        S.ts((W[:, k, :], "W%d" % k), Wraw[k % 2][:], gs[:, k:k + 1], None, ALU.mult,
             eng="vector" if k % 2 == 0 else "gpsimd")
    NB = 2
    x32 = [S.sb([128, KT, 512], F32, "x32_%d" % i) for i in range(NB)]
    xt = [S.sb([128, KT, 512], BF16, "xt%d" % i) for i in range(NB)]
    sq = [S.sb([128, 512], F32, "sq%d" % i) for i in range(NB)]
    rb = [S.sb([128, 512], F32, "rb%d" % i) for i in range(NB)]
    ob = [S.sb([128, 512], F32, "ob%d" % i) for i in range(4)]
    rs = [S.sb([128, 512], F32, "rs%d" % i) for i in range(2)] if resid is not None else None
    pss = S.ps([128, 512], F32, "pss")
    pp = [S.ps([128, 512], F32, "pp%d" % i) for i in range(4)]
    xv = xT.rearrange("(k p) t -> p k t", p=128)
    mts = [(m, min(128, N - m)) for m in range(0, N, 128)]
    gi = 0
    for t in range(ntok // 512):
        b = t % NB
        ts_ = slice(t * 512, (t + 1) * 512)
        S.dma(x32[b][:], xv[:, :, ts_], queue="sync")
        S.copy(xt[b][:], x32[b][:], eng="gpsimd")
        if norm:
            for k in range(KT):
                S.act(sq[b][:], x32[b][:, k, :], AF.Square)
                S.mm(pss, ones[:], sq[b][:], start=(k == 0), stop=(k == KT - 1))
            S.ts(rb[b][:], pss, 1.0 / K, eps, ALU.mult, ALU.add)
            S.act(rb[b][:], rb[b][:], AF.Sqrt)
            S.op("vector", lambda e, b=b: e.reciprocal(rb[b][:], rb[b][:]), reads=[rb[b][:]], writes=[rb[b][:]])
        for (m0, mw) in mts:
            p = pp[gi % 4]
            o = ob[gi % 4]
            for k in range(KT):
                S.op("tensor", (lambda e, p=p, b=b, k=k, m0=m0, mw=mw: e.matmul(p[0:mw, :], W[:, k, m0:m0 + mw], xt[b][:, k, :], start=(k == 0), stop=(k == KT - 1))),
                     reads=[xt[b][:], "W%d" % k], writes=[p])
            if resid is not None:
                r_ = rs[gi % 2]
                S.dma(r_[0:mw, :], resid[m0:m0 + mw, ts_], queue="sync")
                S.tt(o[0:mw, :], p[0:mw, :], r_[0:mw, :], ALU.add, eng="vector")
            elif norm:
                S.tt(o[0:mw, :], p[0:mw, :], rb[b][0:mw, :], ALU.mult, eng="vector")
            else:
                S.copy(o[0:mw, :], p[0:mw, :], eng="scalar")
            wk = "%s_w%d_%d" % (tag, t, m0)
            S.dma(outT[m0:m0 + mw, ts_], o[0:mw, :], queue="gpsimd", reads=[o[:]], writes=[wk])
            S.readers.setdefault(_key(o[:]), []).append(S.last_w[wk])
            gi += 1


def emit_transpose(S, src, dstT, L, C, idt, norm_w=None, eps=1e-6, tag="tr", nm=""):
    CT = C // 128
    NB = 2
    xin = [S.sb([128, C], F32, nm + "xin%d" % i) for i in range(NB)]
    sqs = S.sb([128, C], F32, nm + "sqs")
    rstd = [S.sb([128, 1], F32, nm + "rstd%d" % i) for i in range(NB)]
    ot = [S.sb([128, 4, 128], F32, nm + "ot%d" % i) for i in range(2 * CT)]
    pt = [S.ps([128, 512], F32, nm + "pt%d" % i) for i in range(2)]
    nw = None
    if norm_w is not None:
        nw = S.sb([128, CT], F32, nm + "nw")
        S.dma(nw[:], norm_w.rearrange("(k p) -> p k", p=128), allow_slow_non_contiguous=True)
    gi = 0
    for t4 in range(L // 512):
        for j in range(4):
            t = t4 * 4 + j
            b = t % NB
            S.dma(xin[b][:], src[t * 128:(t + 1) * 128, :], queue="sync" if t % 2 == 0 else "gpsimd")
            if norm_w is not None:
                S.op("scalar", lambda e, b=b: e.activation(sqs[:], xin[b][:], AF.Square, accum_out=rstd[b][:]),
                     reads=[xin[b][:]], writes=[sqs[:], rstd[b][:]])
                S.ts(rstd[b][:], rstd[b][:], 1.0 / C, eps, ALU.mult, ALU.add)
                S.act(rstd[b][:], rstd[b][:], AF.Sqrt)
                S.op("vector", lambda e, b=b: e.reciprocal(rstd[b][:], rstd[b][:]), reads=[rstd[b][:]], writes=[rstd[b][:]])
                S.ts(xin[b][:], xin[b][:], rstd[b][:], None, ALU.mult)
            for c in range(CT):
                p = pt[gi % 2]
                S.tr(p[:, 0:128], xin[b][:, c * 128:(c + 1) * 128], idt)
                o = ot[(t4 % 2) * CT + c]
                okey = "%s%sot%d_%d" % (S.prefix, nm, (t4 % 2) * CT + c, j)
                if norm_w is not None:
                    S.act((o[:, j, :], okey), p[:, 0:128], AF.Copy, scale=nw[:, c:c + 1])
                elif gi % 2 == 0:
                    S.op("vector", lambda e, o=o, j=j, p=p: e.tensor_copy(o[:, j, :], p[:, 0:128]), reads=[p], writes=[okey])
                else:
                    S.op("scalar", lambda e, o=o, j=j, p=p: e.copy(o[:, j, :], p[:, 0:128]), reads=[p], writes=[okey])
                gi += 1
        for c in range(CT):
            o = ot[(t4 % 2) * CT + c]
            keys = ["%s%sot%d_%d" % (S.prefix, nm, (t4 % 2) * CT + c, j) for j in range(4)]
            wk = "%s_w%d_%d" % (tag, t4, c)
            S.dma(dstT[c * 128:(c + 1) * 128, t4 * 512:(t4 + 1) * 512], o[:].rearrange("p j t -> p (j t)"), queue="gpsimd", reads=keys, writes=[wk])
            for key in keys:
                S.readers.setdefault(key, []).append(S.last_w[wk])


def emit_untranspose(S, srcT, dst, L, C, idt, tag="ut", odt=None, evac=None):
    CT = C // 128
    xin = [S.sb([128, CT, 128], F32, "u_xin%d" % i) for i in range(2)]
    ot = [S.sb([128, C], odt or F32, "u_ot%d" % i) for i in range(2)]
    pt = [S.ps([128, 512], F32, "u_pt%d" % i) for i in range(2)]
    sv = srcT.rearrange("(k p) t -> p k t", p=128)
    gi = 0
    for t in range(L // 128):
        b = t % 2
        S.dma(xin[b][:], sv[:, :, t * 128:(t + 1) * 128], queue="sync")
        keys = []
        for c in range(CT):
            p = pt[gi % 2]
            S.tr(p[:, 0:128], xin[b][:, c, :], idt)
            key = "%sot%d_%d" % (S.prefix, b, c)
            keys.append(key)
            if gi % 2 == 0 and evac != "scalar":
                S.op("vector", lambda e, b=b, c=c, p=p: e.tensor_copy(ot[b][:, c * 128:(c + 1) * 128], p[:, 0:128]), reads=[p], writes=[key])
            else:
                S.op("scalar", lambda e, b=b, c=c, p=p: e.copy(ot[b][:, c * 128:(c + 1) * 128], p[:, 0:128]), reads=[p], writes=[key])
            gi += 1
        wk = "%s_w%d" % (tag, t)
        S.dma(dst[t * 128:(t + 1) * 128, :], ot[b][:], queue="gpsimd", reads=keys, writes=[wk])
        for key in keys:
            S.readers.setdefault(key, []).append(S.last_w[wk])


def emit_finalnorm(S, srcT, fw, out, L, idt, eps=1e-6):
    CT = 8
    xin = [S.sb([128, CT, 128], F32, "xin%d" % i) for i in range(2)]
    sq = [S.sb([128, CT, 128], F32, "sq%d" % i) for i in range(2)]
    ot = [S.sb([128, 1024], F32, "ot%d" % i) for i in range(2)]
    rstd = [S.sb([128, 1], F32, "rstd%d" % i) for i in range(2)]
    ones = S.sb([128, 1], F32, "ones"); S.memset(ones[:], 1.0)
    FW = S.sb([128, 1024], F32, "FW")
    S.dma(FW[:], fw.partition_broadcast(128))
    ssp = S.ps([128, 1], F32, "ssp")
    pt = [S.ps([128, 512], F32, "pt%d" % i) for i in range(2)]
    sv = srcT.rearrange("(k p) t -> p k t", p=128)
    gi = 0
    for t in range(L // 128):
        b = t % 2
        S.dma(xin[b][:], sv[:, :, t * 128:(t + 1) * 128], queue="sync")
        S.act(sq[b][:], xin[b][:], AF.Square)
        for k in range(CT):
            S.mm(ssp, sq[b][:, k, :], ones[:], start=(k == 0), stop=(k == CT - 1))
        S.ts(rstd[b][:], ssp, 1.0 / 1024, eps, ALU.mult, ALU.add)
        S.act(rstd[b][:], rstd[b][:], AF.Sqrt)
        S.op("vector", lambda e, b=b: e.reciprocal(rstd[b][:], rstd[b][:]), reads=[rstd[b][:]], writes=[rstd[b][:]])
        keys = []
        for c in range(CT):
            p = pt[gi % 2]
            S.tr(p[:, 0:128], xin[b][:, c, :], idt)
            key = "%sot%d_%d" % (S.prefix, b, c)
            keys.append(key)
            S.op("vector", lambda e, b=b, c=c, p=p: e.scalar_tensor_tensor(ot[b][:, c * 128:(c + 1) * 128], p[:, 0:128], rstd[b][:], FW[:, c * 128:(c + 1) * 128], ALU.mult, ALU.mult),
                 reads=[p, rstd[b][:], FW[:]], writes=[key])
            gi += 1
        wk = "fin_w%d" % t
        S.dma(out[t * 128:(t + 1) * 128, :], ot[b][:], queue="gpsimd", reads=keys, writes=[wk])
        for key in keys:
            S.readers.setdefault(key, []).append(S.last_w[wk])


def emit_attn(S, dk, dv, L, NH, plan, nmask, q, qp, kpc, kppc, tabs_d, v, masks, idt, y, tag="at"):
    NT = L // 128
    NG = L // 512
    tabs = {}
    for i, n in enumerate(("cq", "sq", "ck", "sk")):
        t = S.sb([dk, L], F32, "tab_" + n)
        S.dma(t[:], tabs_d[i], queue="sync")
        tabs[n] = t
    mk = None
    if nmask:
        mk32 = S.sb([128, 512], F32, "mk32")
        mk = S.sb([128, nmask, 512], BF16, "mk")
        for i in range(nmask):
            S.dma(mk32[:], masks[i])
            S.copy((mk[:, i, :], "mk%d" % i), mk32[:], eng="vector")
    st = [S.sb([dk, L], F32, "st%d" % i) for i in range(2)]
    qr = [S.sb([dk, L], BF16, "qr%d" % h) for h in range(NH)]
    kr = [S.sb([dk, L], BF16, "kr%d" % h) for h in range(NH)]
    va = [S.sb([128, NT, dv + 1], BF16, "va%d" % h) for h in range(NH)]
    v32_ = S.sb([128, NT, dv], F32, "v32")

    def prologue(h):
        for (pcs, pcsp, c, s_, dst) in ((q[h], qp[h], "cq", "sq", qr[h]), (kpc[h], kppc[h], "ck", "sk", kr[h])):
            for (lo, hi, a_) in pcs:
                S.dma(st[0][lo:hi, :], a_, queue="sync", reads=[a_], writes=[st[0][:]])
            for (lo, hi, a_) in pcsp:
                S.dma(st[1][lo:hi, :], a_, queue="gpsimd", reads=[a_], writes=[st[1][:]])
            S.tt(st[0][:], st[0][:], tabs[c][:], ALU.mult, eng="vector")
            S.tt(st[1][:], st[1][:], tabs[s_][:], ALU.mult, eng="gpsimd")
            S.tt(dst[:], st[0][:], st[1][:], ALU.add, eng="vector")
        S.dma(v32_[:], v[h].rearrange("(t p) d -> p t d", p=128), queue="sync")
        S.memset(va[h][:], 1.0, eng="gpsimd")
        S.copy(va[h][:, :, 0:dv], v32_[:], eng="vector")

    prologue(0)
    NPS = 4
    pss = [S.ps([128, 512], F32, "pss%d" % i) for i in range(NPS)]
    pso = [S.ps([dv + 1, 512], F32, "pso%d" % i) for i in range(2)]
    pst = [S.ps([128, dv + 1], F32, "pst%d" % i) for i in range(2)]
    NPT = 4
    pt = [S.sb([128, 512], BF16, "pt%d" % i) for i in range(NPT)]
    ptm = [S.sb([128, 512], BF16, "ptm%d" % i) for i in range(NPT)]
    osb = [S.sb([dv + 1, 512], F32, "osb%d" % i) for i in range(2)]
    rec = [S.sb([128, 1], F32, "rec%d" % i) for i in range(2)]
    yo = [S.sb([128, 4, NH * dv], F32, "yo%d" % i) for i in range(2)]
    iters = [(g, h, n, kt, mid, len(plan[g])) for h in range(NH) for g in range(NG) for n, (kt, mid) in enumerate(plan[g])]
    LOOK = 3

    def emit_qk(i):
        g, h, n, kt, mid, ln = iters[i]
        S.mm(pss[i % NPS], kr[h][:, kt * 128:(kt + 1) * 128], qr[h][:, g * 512:(g + 1) * 512])

    for i in range(min(LOOK, len(iters))):
        emit_qk(i)
    gi = 0
    prepared = 1
    for it, (g, h, n, kt, mid, ln) in enumerate(iters):
        if prepared < NH and h == prepared - 1 and g == 0 and n == ln - 1:
            prologue(prepared)
            prepared += 1
        if it + LOOK < len(iters):
            assert iters[it + LOOK][1] < prepared
            emit_qk(it + LOOK)
        po = pso[gi % 2]
        p_s = pss[it % NPS]
        e = pt[it % NPT]
        S.act(e[:], p_s, AF.Exp)
        if mid is not None:
            em = ptm[it % NPT]
            S.tt(em[:], e[:], (mk[:, mid, :], "mk%d" % mid), ALU.mult,
                 eng="vector" if it % 3 != 2 else "gpsimd")
            e = em
        S.mm(po, va[h][:, kt, :], e[:], start=(n == 0), stop=(n == ln - 1))
        if n != ln - 1:
            continue
        ob = osb[gi % 2]
        S.copy(ob[:], po, eng="vector")
        for j in range(4):
            tp = pst[j % 2]
            S.tr(tp, ob[:, j * 128:(j + 1) * 128], idt[0:dv + 1, 0:dv + 1])
            rc = rec[j % 2]
            S.op("vector", lambda e_, rc=rc, tp=tp: e_.reciprocal(rc[:], tp[:, dv:dv + 1]),
                 reads=[tp], writes=[rc[:]])
            S.ts((yo[g % 2][:, j, h * dv:(h + 1) * dv], "%syo%d_%d_%d" % (S.prefix, g % 2, j, h)), tp[:, 0:dv], rc[:], None, ALU.mult)
        gi += 1
        keys = ["%syo%d_%d_%d" % (S.prefix, g % 2, j, h) for j in range(4)]
        wk = "%s_y%d_%d" % (tag, g, h)
        S.dma(y[g * 512:(g + 1) * 512, h * dv:(h + 1) * dv].rearrange("(j p) d -> p j d", p=128), yo[g % 2][:, :, h * dv:(h + 1) * dv], queue="sync", reads=keys, writes=[wk])
        for key in keys:
            S.readers.setdefault(key, []).append(S.last_w[wk])


def emit_ret(S, L, NH, q, k, kt_, v, g, ck, sk, ct, st_, dtot, cols, y, gn_eps=1e-5):
    NT = L // 128
    CK = S.sb([64, L], F32, "CK"); SK = S.sb([64, L], F32, "SK")
    CT = S.sb([128, NT, 64], F32, "CT"); STt = S.sb([128, NT, 64], F32, "STt")
    S.dma(CK[:], ck); S.dma(SK[:], sk, queue="gpsimd")
    tokv = lambda a: a.rearrange("(t p) d -> p t d", p=128)
    S.dma(CT[:], tokv(ct)); S.dma(STt[:], tokv(st_), queue="gpsimd")
    COL = S.sb([128, NH * 6], F32, "COL"); S.dma(COL[:], cols)
    DT = S.sb([128, NH, 128], F32, "DT")
    for h in range(NH):
        S.dma(DT[:, h, :], dtot[h])
    st0 = S.sb([64, L], F32, "st0"); st1 = S.sb([64, L], F32, "st1")
    qr = S.sb([64, L], BF16, "qr"); kr = S.sb([64, L], BF16, "kr")
    krt = S.sb([128, NT, 64], BF16, "krt")
    scrA = S.sb([128, NT * 128], F32, "scrA"); scrB = S.sb([128, NT * 128], F32, "scrB")
    vbf = S.sb([128, NT, 64], BF16, "vbf")
    vz = S.sb([128, NT, 128], BF16, "vz")
    S32 = S.sb([64, NT + 1, 128], F32, "S32")
    Sst = S.sb([64, NT, 128], BF16, "Sst")
    osb = S.sb([128, NT, NH, 64], F32, "osb")
    pST = [S.ps([128, 128], F32, "pST%d" % i) for i in range(2)]
    pIn = [S.ps([128, 64], F32, "pIn%d" % i) for i in range(2)]
    pX = [S.ps([128, 128], F32, "pX%d" % i) for i in range(2)]
    pU = [S.ps([64, 128], F32, "pU%d" % i) for i in range(2)]
    PT = [S.sb([128, 128], BF16, "PT%d" % i) for i in range(3)]
    tb = [S.sb([128, 64], F32, "tb%d" % i) for i in range(3)]
    P = S.prefix
    for h in range(NH):
        c6 = lambda i: COL[:, h * 6 + i:h * 6 + i + 1]
        for (src, dst) in ((q, qr), (k, kr)):
            S.dma(st0[:], src[h])
            S.dma(st1[0:32, :], src[h][32:64], queue="gpsimd", reads=[src[h]], writes=[st1[:]])
            S.dma(st1[32:64, :], src[h][0:32], queue="gpsimd", reads=[src[h]], writes=[st1[:]])
            S.tt(st0[:], st0[:], CK[:], ALU.mult, eng="vector")
            S.tt(st1[:], st1[:], SK[:], ALU.mult, eng="gpsimd")
            S.tt(dst[:], st0[:], st1[:], ALU.add, eng="vector")
        A3 = scrA[:, 0:NT * 64].rearrange("p (t d) -> p t d", d=64)
        A3b = scrA[:, NT * 64:NT * 128].rearrange("p (t d) -> p t d", d=64)
        S.dma(A3, tokv(kt_[h]), reads=[kt_[h]], writes=[scrA[:]])
        S.dma(A3b[:, :, 0:32], tokv(kt_[h])[:, :, 32:64], queue="gpsimd", reads=[kt_[h]], writes=[P + "scrA_b"])
        S.dma(A3b[:, :, 32:64], tokv(kt_[h])[:, :, 0:32], queue="gpsimd", reads=[kt_[h]], writes=[P + "scrA_b"])
        S.op("vector", lambda e, A3=A3: e.tensor_tensor(A3, A3, CT[:], ALU.mult), reads=[scrA[:], CT[:]], writes=[scrA[:]])
        S.op("gpsimd", lambda e, A3b=A3b: e.tensor_tensor(A3b, A3b, STt[:], ALU.mult), reads=[P + "scrA_b", STt[:]], writes=[P + "scrA_b"])
        S.op("vector", lambda e, A3=A3, A3b=A3b: e.tensor_tensor(krt[:], A3, A3b, ALU.add), reads=[scrA[:], P + "scrA_b"], writes=[krt[:]])
        B3 = scrB[:, 0:NT * 64].rearrange("p (t d) -> p t d", d=64)
        S.dma(B3, tokv(v[h]), reads=[v[h]], writes=[scrB[:]])
        S.op("vector", lambda e, B3=B3: e.tensor_copy(vbf[:], B3), reads=[scrB[:]], writes=[vbf[:]])
        S.op("vector", lambda e, B3=B3, h=h: e.tensor_scalar(vz[:, :, 0:64], B3, COL[:, h * 6 + 2:h * 6 + 3], None, ALU.mult), reads=[scrB[:], COL[:]], writes=[P + "vz_f"])
        S.op("gpsimd", lambda e, B3=B3, h=h: e.tensor_scalar(vz[:, :, 64:128], B3, COL[:, h * 6 + 3:h * 6 + 4], None, ALU.mult), reads=[scrB[:], COL[:]], writes=[P + "vz_b"])
        allS = [S32[:]] + [P + "S32f%d" % c for c in range(NT)] + [P + "S32b%d" % c for c in range(NT)]
        S.op("gpsimd", lambda e: e.memset(S32[:], 0.0), reads=[], writes=allS)
        for c in range(NT):
            pu = pU[c % 2]
            S.op("tensor", lambda e, pu=pu, c=c: e.matmul(pu, krt[:, c, :], vz[:, c, :], start=True, stop=True),
                 reads=[krt[:], P + "vz_f", P + "vz_b"], writes=[pu])
            if c + 1 <= NT - 1:
                S.op("scalar", lambda e, c=c, pu=pu: e.copy(S32[:, c + 1, 0:64], pu[:, 0:64]), reads=[pu, S32[:]], writes=[P + "S32f%d" % (c + 1)])
            if c >= 1:
                S.op("vector", lambda e, c=c, pu=pu: e.tensor_copy(S32[:, c - 1, 64:128], pu[:, 64:128]), reads=[pu, S32[:]], writes=[P + "S32b%d" % (c - 1)])
        for c in range(1, NT):
            S.op("vector", lambda e, c=c, h=h: e.scalar_tensor_tensor(S32[:, c, 0:64], S32[:, c - 1, 0:64], COL[0:64, h * 6 + 4:h * 6 + 5], S32[:, c, 0:64], ALU.mult, ALU.add),
                 reads=[P + "S32f%d" % (c - 1), P + "S32f%d" % c, S32[:]], writes=[P + "S32f%d" % c])
        for c in range(NT - 2, -1, -1):
            S.op("vector", lambda e, c=c, h=h: e.scalar_tensor_tensor(S32[:, c, 64:128], S32[:, c + 1, 64:128], COL[0:64, h * 6 + 5:h * 6 + 6], S32[:, c, 64:128], ALU.mult, ALU.add),
                 reads=[P + "S32b%d" % (c + 1), P + "S32b%d" % c, S32[:]], writes=[P + "S32b%d" % c])
        S.op("vector", lambda e: e.tensor_copy(Sst[:], S32[:, 0:NT, :]), reads=allS, writes=[Sst[:]])
        def front_r(c):
            cs = slice(c * 128, (c + 1) * 128)
            S.mm(pST[c % 2], kr[:, cs], qr[:, cs])
            S.mm(pX[c % 2], qr[:, cs], Sst[:, c, :])

        front_r(0)
        for c in range(NT):
            if c + 1 < NT:
                front_r(c + 1)
            ps_ = pST[c % 2]
            pt = PT[c % 3]
            S.tt(pt[:], ps_, DT[:, h, :], ALU.mult, eng="vector")
            pin = pIn[c % 2]
            S.mm(pin, pt[:], vbf[:, c, :])
            px = pX[c % 2]
            t = tb[c % 3]
            S.act(t[:], px[:, 0:64], AF.Copy, scale=c6(0))
            S.stt(t[:], px[:, 64:128], c6(1), t[:], ALU.mult, ALU.add, eng="vector")
            S.op("vector", lambda e, c=c, h=h, pin=pin, t=t: e.tensor_tensor(osb[:, c, h, :], pin, t[:], ALU.add),
                 reads=[pin, t[:]], writes=[P + "osb%d_%d" % (c, h)])
    G = NT * NH
    allosb = [P + "osb%d_%d" % (c, h) for c in range(NT) for h in range(NH)]
    o3 = osb[:].rearrange("p t h d -> p (t h) d")
    o2 = osb[:].rearrange("p t h d -> p (t h d)")
    s1 = S.sb([128, G], F32, "s1"); s2 = S.sb([128, G], F32, "s2"); mu = S.sb([128, G], F32, "mu")
    rs = S.sb([128, G], F32, "rs"); nb = S.sb([128, G], F32, "nb")
    W_ = NH * 64
    for hh in range(0, NT * W_, 4096):
        n_ = min(4096, NT * W_ - hh)
        g0, g1 = hh // 64, (hh + n_) // 64
        S.op("scalar", lambda e, hh=hh, n_=n_: e.activation(scrA[:, 0:n_], o2[:, hh:hh + n_], AF.Square), reads=allosb, writes=[scrA[:], P + "scrA_b"])
        S.op("vector", lambda e, n_=n_, g0=g0, g1=g1: e.reduce_sum(s2[:, g0:g1], scrA[:, 0:n_].rearrange("p (g d) -> p g d", d=64), AX.X), reads=[scrA[:]], writes=[s2[:]])
    S.op("vector", lambda e: e.reduce_sum(s1[:], o3, AX.X), reads=allosb, writes=[s1[:]])
    S.ts(mu[:], s1[:], 1.0 / 64, None, ALU.mult)
    S.tt(s1[:], mu[:], mu[:], ALU.mult)
    S.stt(rs[:], s2[:], 1.0 / 64, s1[:], ALU.mult, ALU.subtract)
    S.ts(rs[:], rs[:], gn_eps, None, ALU.add)
    S.act(rs[:], rs[:], AF.Sqrt)
    S.op("vector", lambda e: e.reciprocal(rs[:], rs[:]), reads=[rs[:]], writes=[rs[:]])
    S.stt(nb[:], mu[:], -1.0, rs[:], ALU.mult, ALU.mult)
    for gi in range(G):
        S.op("scalar", lambda e, gi=gi: e.activation(o3[:, gi, :], o3[:, gi, :], AF.Identity, bias=nb[:, gi:gi + 1], scale=rs[:, gi:gi + 1]),
             reads=[allosb[gi], nb[:], rs[:]], writes=[allosb[gi]])
    gv = g.rearrange("(t p) d -> p t d", p=128)
    o4 = osb[:].rearrange("p t h d -> p t (h d)")
    TC = 4096 // W_
    for t0 in range(0, NT, TC):
        sB = scrB[:, 0:TC * W_].rearrange("p (t d) -> p t d", d=W_)
        S.dma(sB, gv[:, t0:t0 + TC, :], reads=[g], writes=[scrB[:]])
        S.act(scrB[:, 0:TC * W_], scrB[:, 0:TC * W_], AF.Silu)
        S.op("vector", lambda e, t0=t0, sB=sB: e.tensor_tensor(o4[:, t0:t0 + TC, :], o4[:, t0:t0 + TC, :], sB, ALU.mult),
             reads=allosb + [scrB[:]], writes=[P + "osbfin%d" % t0])
    S.dma(y.rearrange("(t p) d -> p t d", p=128), o4, reads=[P + "osbfin%d" % t0 for t0 in range(0, NT, TC)], writes=[P + "yout"])


def emit_ssd(S, L, xr, br, cr, z, dtr, cw, cb, dtb, alog, dsk, consts, y):
    NT = L // 128
    P = S.prefix
    CN = S.sb([128, 5, 128], F32, "CN")
    for i in range(5):
        S.dma(CN[:, i, :], consts[i])
    triF, triB, mF, mB, idt = (CN[:, i, :] for i in range(5))
    ones = S.sb([128, 128], F32, "ones"); S.memset(ones[:], 1.0)
    CW = S.sb([128, 3, 5], F32, "CW"); CB = S.sb([128, 3], F32, "CB"); DSK = S.sb([128, 2], F32, "DSK")
    for i in range(3):
        S.dma(CW[:, i, :], cw[i]); S.dma(CB[:, i:i + 1], cb[i])
    S.dma(DSK[:], dsk)
    xin = S.sb([128, L + 4], F32, "xin")
    acc = S.sb([128, L], F32, "acc")
    XS = S.sb([128, L], F32, "XS")
    Bf = S.sb([128, L], F32, "Bf")
    BT = S.sb([128, L], BF16, "BT"); CT = S.sb([128, L], BF16, "CT")
    for i, src in enumerate((xr, br, cr)):
        S.op("gpsimd", lambda e: e.memset(xin[:, 0:2], 0.0), reads=[], writes=[xin[:]])
        S.op("gpsimd", lambda e: e.memset(xin[:, L + 2:L + 4], 0.0), reads=[], writes=[xin[:]])
        S.dma(xin[:, 2:L + 2], src, reads=[src], writes=[xin[:]])
        S.ts(acc[:], xin[:, 0:L], CW[:, i, 0:1], None, ALU.mult)
        for w in range(1, 5):
            S.stt(acc[:], xin[:, w:w + L], CW[:, i, w:w + 1], acc[:], ALU.mult, ALU.add)
        if i == 0:
            S.act(XS[:], acc[:], AF.Silu, bias=CB[:, 0:1])
        elif i == 1:
            S.act(Bf[:], acc[:], AF.Silu, bias=CB[:, 1:2])
            S.copy(BT[:], Bf[:], eng="gpsimd")
        else:
            S.act(CT[:], acc[:], AF.Silu, bias=CB[:, 2:3])
    DT = S.sb([128, NT, 4], F32, "DT"); T1 = S.sb([128, NT, 4], F32, "T1"); DTA = S.sb([128, NT, 4], F32, "DTA")
    tokv = lambda a: a.rearrange("(t p) d -> p t d", p=128)
    S.dma(DT[:], tokv(dtr), allow_slow_non_contiguous=True); S.dma(T1[:], tokv(dtb))
    S.tt(DT[:], DT[:], T1[:], ALU.add)
    S.act(DT[:], DT[:], AF.Exp)
    S.act(DT[:], DT[:], AF.Ln, bias=1.0)
    S.dma(T1[:], tokv(alog))
    S.act(T1[:], T1[:], AF.Exp)
    S.stt(DTA[:], T1[:], -1.0, DT[:], ALU.mult, ALU.mult)
    dta2 = DTA[:].rearrange("p t d -> p (t d)")
    pA = S.ps([128, 512], F32, "pA"); pB = S.ps([128, 512], F32, "pB"); pC = S.ps([128, 512], F32, "pC")
    CS = S.sb([128, NT, 4], F32, "CS"); TOT = S.sb([128, NT, 4], F32, "TOT")
    S.mm(pA[:, 0:NT * 4], triF, dta2)
    S.mm(pB[:, 0:NT * 4], triB, dta2)
    S.mm(pC[:, 0:NT * 4], ones[:], dta2)
    pA3 = pA[:, 0:NT * 4].rearrange("p (t d) -> p t d", d=4); pB3 = pB[:, 0:NT * 4].rearrange("p (t d) -> p t d", d=4)
    S.op("vector", lambda e: e.tensor_copy(CS[:, :, 0:2], pA3[:, :, 0:2]), reads=[pA], writes=[P + "CSf"])
    S.op("vector", lambda e: e.tensor_copy(CS[:, :, 2:4], pB3[:, :, 2:4]), reads=[pB], writes=[P + "CSb"])
    S.copy(TOT[:].rearrange("p t d -> p (t d)"), pC[:, 0:NT * 4], eng="vector")
    TE = S.sb([128, NT, 4], F32, "TE"); ECS = S.sb([128, NT, 4], F32, "ECS"); ED = S.sb([128, NT, 4], F32, "ED")
    S.op("vector", lambda e: e.tensor_tensor(TE[:], TOT[:], CS[:], ALU.subtract), reads=[TOT[:], P + "CSf", P + "CSb"], writes=[TE[:]])
    S.act(TE[:], TE[:], AF.Exp)
    S.op("scalar", lambda e: e.activation(ECS[:], CS[:], AF.Exp), reads=[P + "CSf", P + "CSb"], writes=[ECS[:]])
    S.act(ED[:], TOT[:], AF.Exp)
    XTK = S.sb([128, NT, 128], F32, "XTK")
    BTK = S.sb([128, NT, 128], BF16, "BTK")
    XDT = S.sb([128, NT, 256], BF16, "XDT")
    XTE = S.sb([128, NT, 256], BF16, "XTE")
    pT = [S.ps([128, 512], F32, "pT%d" % i) for i in range(2)]
    for c in range(NT):
        cs_ = slice(c * 128, (c + 1) * 128)
        p = pT[c % 2]
        S.tr(p[:, 0:128], XS[:, cs_], idt)
        S.tr(p[:, 128:256], Bf[:, cs_], idt)
        S.op("vector", lambda e, c=c, p=p: e.tensor_copy(XTK[:, c, :], p[:, 0:128]), reads=[p], writes=[P + "XTK%d" % c])
        S.op("vector", lambda e, c=c, p=p: e.tensor_copy(BTK[:, c, :], p[:, 128:256]), reads=[p], writes=[P + "BTK%d" % c])
        for s4 in range(4):
            h = s4 % 2
            S.op("gpsimd", lambda e, c=c, s4=s4, h=h: e.tensor_scalar(XDT[:, c, s4 * 64:(s4 + 1) * 64], XTK[:, c, h * 64:(h + 1) * 64], DT[:, c, s4:s4 + 1], None, ALU.mult),
                 reads=[P + "XTK%d" % c, DT[:]], writes=[P + "XDT%d_%d" % (c, s4)])
            S.op("gpsimd", lambda e, c=c, s4=s4: e.tensor_scalar(XTE[:, c, s4 * 64:(s4 + 1) * 64], XDT[:, c, s4 * 64:(s4 + 1) * 64], TE[:, c, s4:s4 + 1], None, ALU.mult),
                 reads=[P + "XDT%d_%d" % (c, s4), TE[:]], writes=[P + "XTE%d_%d" % (c, s4)])
    P32 = S.sb([128, NT, 256], F32, "P32")
    PBF = S.sb([128, NT, 256], BF16, "PBF")
    S.memset(P32[:], 0.0, eng="gpsimd")
    for c in range(NT):
        p = pT[c % 2]
        S.op("tensor", lambda e, c=c, p=p: e.matmul(p[:, 0:256], BTK[:, c, :], XTE[:, c, :], start=True, stop=True),
             reads=[P + "BTK%d" % c] + [P + "XTE%d_%d" % (c, s4) for s4 in range(4)], writes=[p])
        if c + 1 < NT:
            S.op("vector", lambda e, c=c, p=p: e.tensor_copy(P32[:, c + 1, 0:128], p[:, 0:128]), reads=[p, P32[:]], writes=[P + "Pf%d" % (c + 1)])
        if c >= 1:
            S.op("vector", lambda e, c=c, p=p: e.tensor_copy(P32[:, c - 1, 128:256], p[:, 128:256]), reads=[p, P32[:]], writes=[P + "Pb%d" % (c - 1)])
    for c in range(1, NT):
        for h in range(2):
            sl = slice(h * 64, (h + 1) * 64)
            S.op("vector", lambda e, c=c, h=h, sl=sl: e.scalar_tensor_tensor(P32[:, c, sl], P32[:, c - 1, sl], ED[:, c - 1, h:h + 1], P32[:, c, sl], ALU.mult, ALU.add),
                 reads=[P + "Pf%d" % (c - 1), P + "Pf%d" % c, P32[:], ED[:]], writes=[P + "Pf%d" % c])
    for c in range(NT - 2, -1, -1):
        for h in range(2):
            sl = slice(128 + h * 64, 128 + (h + 1) * 64)
            S.op("vector", lambda e, c=c, h=h, sl=sl: e.scalar_tensor_tensor(P32[:, c, sl], P32[:, c + 1, sl], ED[:, c + 1, 2 + h:3 + h], P32[:, c, sl], ALU.mult, ALU.add),
                 reads=[P + "Pb%d" % (c + 1), P + "Pb%d" % c, P32[:], ED[:]], writes=[P + "Pb%d" % c])
    S.op("gpsimd", lambda e: e.tensor_copy(PBF[:], P32[:]), reads=[P32[:]] + [P + "Pf%d" % c for c in range(NT)] + [P + "Pb%d" % c for c in range(NT)], writes=[PBF[:]])
    ZS = xin[:, 0:L].rearrange("p (t d) -> p t d", d=128)
    S.dma(ZS, tokv(z), reads=[z], writes=[xin[:]]); S.act(ZS, ZS, AF.Silu)
    YO = acc[:].rearrange("p (t d) -> p t d", d=128)
    pG = pA; pBc = [S.ps([128, 512], F32, "pBc%d" % i) for i in range(2)]
    pYd = pB; pYo = pC
    GS = [S.sb([128, 128], F32, "GS%d" % i) for i in range(2)]
    REP = [S.sb([128, 128], F32, "REP%d" % i) for i in range(8)]
    SEG = [S.sb([128, 512], F32, "SEG%d" % i) for i in range(2)]
    MT = [S.sb([128, 512], BF16, "MT%d" % i) for i in range(2)]
    YD = [S.sb([128, 256], F32, "YD%d" % i) for i in range(2)]
    AC = [S.sb([128, 256], F32, "AC%d" % i) for i in range(2)]
    MK4 = S.sb([128, 512], BF16, "MK4")
    for s4 in range(4):
        S.op("vector", lambda e, s4=s4: e.tensor_copy(MK4[:, s4 * 128:(s4 + 1) * 128], mF if s4 < 2 else mB), reads=[CN[:]], writes=[MK4[:]])

    def front(c):
        cs_ = slice(c * 128, (c + 1) * 128)
        S.mm(pG[:, 0:128], BT[:, cs_], CT[:, cs_])
        S.copy(GS[c % 2][:], pG[:, 0:128], eng="scalar")
        pb = pBc[c % 2]
        for s4 in range(4):
            rep = REP[(c % 2) * 4 + s4]
            S.act(rep[:], ones[:], AF.Copy, scale=DTA[:, c, s4:s4 + 1])
            S.mm(pb[:, s4 * 128:(s4 + 1) * 128], rep[:], triF if s4 < 2 else triB)

    def back(c):
        cs_ = slice(c * 128, (c + 1) * 128)
        pb = pBc[c % 2]; seg = SEG[c % 2]; mt = MT[c % 2]; gs = GS[c % 2]
        for s4 in range(4):
            key = P + ("CSf" if s4 < 2 else "CSb")
            S.op("vector", lambda e, seg=seg, pb=pb, c=c, s4=s4: e.tensor_scalar(seg[:, s4 * 128:(s4 + 1) * 128], pb[:, s4 * 128:(s4 + 1) * 128], CS[:, c, s4:s4 + 1], 0.0, ALU.subtract, ALU.min),
                 reads=[pb, key], writes=[seg[:]])
        S.act(seg[:], seg[:], AF.Exp)
        S.tt(seg[:], seg[:], MK4[:], ALU.mult, eng="gpsimd")
        for s4 in range(4):
            S.op("vector" if s4 % 2 == 0 else "gpsimd", lambda e, seg=seg, mt=mt, gs=gs, s4=s4: e.tensor_tensor(mt[:, s4 * 128:(s4 + 1) * 128], seg[:, s4 * 128:(s4 + 1) * 128], gs[:], ALU.mult),
                 reads=[seg[:], gs[:]], writes=[mt[:]])
        for s4 in range(4):
            S.op("tensor", lambda e, mt=mt, c=c, s4=s4: e.matmul(pYd[:, s4 * 64:(s4 + 1) * 64], mt[:, s4 * 128:(s4 + 1) * 128], XDT[:, c, s4 * 64:(s4 + 1) * 64], start=True, stop=True),
                 reads=[mt[:], P + "XDT%d_%d" % (c, s4)], writes=[pYd])
        S.mm(pYo[:, 0:256], CT[:, cs_], PBF[:, c, :])
        yd, ac = YD[c % 2], AC[c % 2]
        S.copy(yd[:], pYd[:, 0:256], eng="scalar")
        for s4 in range(4):
            sl = slice(s4 * 64, (s4 + 1) * 64)
            S.op("vector", lambda e, c=c, s4=s4, sl=sl, ac=ac, yd=yd: e.scalar_tensor_tensor(ac[:, sl], pYo[:, sl], ECS[:, c, s4:s4 + 1], yd[:, sl], ALU.mult, ALU.add),
                 reads=[pYo, ECS[:], yd[:]], writes=[ac[:]])
        S.op("vector", lambda e, c=c, ac=ac: e.tensor_tensor(YO[:, c, :], ac[:, 0:128], ac[:, 128:256], ALU.add), reads=[ac[:], acc[:]], writes=[P + "YO%d" % c])
        for h in range(2):
            sl = slice(h * 64, (h + 1) * 64)
            S.op("vector", lambda e, c=c, h=h, sl=sl: e.scalar_tensor_tensor(YO[:, c, sl], XTK[:, c, sl], DSK[:, h:h + 1], YO[:, c, sl], ALU.mult, ALU.add),
                 reads=[P + "XTK%d" % c, P + "YO%d" % c, DSK[:]], writes=[P + "YO%d" % c])

    front(0)
    for c in range(NT):
        if c + 1 < NT:
            front(c + 1)
        back(c)
    S.op("vector", lambda e: e.tensor_tensor(YO, YO, ZS, ALU.mult), reads=[xin[:]] + [P + "YO%d" % c for c in range(NT)], writes=[acc[:]])
    S.dma(y.rearrange("(t p) d -> p t d", p=128), YO, reads=[acc[:]], writes=[P + "yout"])


def emit_topk(S, aff, vals, idxu, idt, L, NE=16, KSEL=512, mid_hook=None):
    NT = L // 128
    A = S.sb([128, NT, NE], F32, "A")
    S.dma(A[:], aff.rearrange("(t p) e -> p t e", p=128))
    wk = S.sb([NE, L], F32, "wk")
    vs = S.sb([NE, KSEL], F32, "vs")
    ix = S.sb([NE, KSEL], U32, "ix")
    pt = [S.ps([128, 512], F32, "pt%d" % i) for i in range(2)]
    P = S.prefix
    for t4 in range(NT // 4):
        p = pt[t4 % 2]
        for j in range(4):
            S.tr(p[0:NE, j * 128:(j + 1) * 128], A[:, t4 * 4 + j, :], idt)
        S.op("vector", lambda e, t4=t4, p=p: e.tensor_copy(wk[:, t4 * 512:(t4 + 1) * 512], p[0:NE, :]), reads=[p], writes=[P + "wk%d" % t4])
    allwk = [P + "wk%d" % t4 for t4 in range(NT // 4)]
    if mid_hook is not None:
        mid_hook()
    for it in range(KSEL // 8):
        sl = slice(it * 8, it * 8 + 8)
        S.op("vector", lambda e, sl=sl: e.max(vs[:, sl], wk[:]), reads=[wk[:]] + (allwk if it == 0 else []), writes=[P + "vs%d" % it])
        S.op("vector", lambda e, sl=sl: e.max_index(ix[:, sl], vs[:, sl], wk[:]), reads=[wk[:], P + "vs%d" % it], writes=[P + "ix%d" % it])
        S.op("vector", lambda e, sl=sl: e.match_replace(wk[:], vs[:, sl], wk[:], -1e30), reads=[wk[:], P + "vs%d" % it], writes=[wk[:]])
    S.dma(vals, vs[:], reads=[P + "vs%d" % i for i in range(KSEL // 8)], writes=[P + "vout"])
    S.dma(idxu, ix[:], queue="gpsimd", reads=[P + "ix%d" % i for i in range(KSEL // 8)], writes=[P + "iout"])


def emit_moe(S, x1k, vals, idxu, g2, wg, wu, wd, x2k, idt, L, NE=16, eps=1e-6):
    P = S.prefix
    NS = NE * 2
    G2 = S.sb([128, 8], F32, "G2")
    S.dma(G2[:], g2.rearrange("(k p) -> p k", p=128), allow_slow_non_contiguous=True)
    WG = [S.sb([128, 8, 1024], BF16, "WG%d" % i) for i in range(2)]
    WU = [S.sb([128, 8, 1024], BF16, "WU%d" % i) for i in range(2)]
    WD = [S.sb([128, 8, 1024], BF16, "WD%d" % i) for i in range(2)]
    XGT = [S.sb([128, 4, 1024], F32, "XGT%d" % i) for i in range(2)]
    IX = [S.sb([128, 4], U32, "IX%d" % i) for i in range(3)]
    GT = [S.sb([128, 4], F32, "GT%d" % i) for i in range(3)]
    SS = [S.sb([128, 4], F32, "SS%d" % i) for i in range(2)]
    SQ = S.sb([128, 1024], F32, "SQ")
    XNs = [S.sb([128, 8, 512], BF16, "XN%d" % i) for i in range(2)]
    HID = S.sb([128, 8, 512], BF16, "HID")
    ACC = S.sb([128, 4, 1024], F32, "ACC")
    GS = [S.sb([128, 512], F32, "GS%d" % i) for i in range(2)]
    OB = [S.sb([128, 1024], F32, "OB%d" % i) for i in range(2)]
    pt = [S.ps([128, 512], F32, "pt%d" % i) for i in range(2)]
    pg = [S.ps([128, 512], F32, "pg%d" % i) for i in range(2)]
    pu = [S.ps([128, 512], F32, "pu%d" % i) for i in range(2)]
    po = [S.ps([128, 512], F32, "po%d" % i) for i in range(2)]
    for r in range(L // 512):
        S.dma(x2k[r * 512:(r + 1) * 512, :], x1k[r * 512:(r + 1) * 512, :], queue="sync", writes=[P + "x2k"])

    def load_w(s):
        e_, hf = divmod(s, 2)
        sl = s % 2
        fs = slice(hf * 1024, (hf + 1) * 1024)
        S.dma(WG[sl][:], wg[e_].rearrange("(k p) f -> p k f", p=128)[:, :, fs], queue="gpsimd")
        S.dma(WU[sl][:], wu[e_].rearrange("(k p) f -> p k f", p=128)[:, :, fs], queue="gpsimd")
        S.dma(WD[sl][:], wd[e_][fs, :].rearrange("(k p) d -> p k d", p=128), queue="gpsimd")

    def gather(e_):
        b = e_ % 2
        b3 = e_ % 3
        S.dma(IX[b3][:], idxu[e_].rearrange("(j p) -> p j", p=128), queue="sync", allow_slow_non_contiguous=True)
        S.dma(GT[b3][:], vals[e_].rearrange("(j p) -> p j", p=128), queue="sync", allow_slow_non_contiguous=True)
        for j in range(4):
            S.raw_dma("gpsimd", lambda e, b=b, b3=b3, j=j: e.indirect_dma_start(out=XGT[b][:, j, :], out_offset=None, in_=x1k,
                      in_offset=bass.IndirectOffsetOnAxis(ap=IX[b3][:, j:j + 1], axis=0)),
                      reads=[IX[b3][:], x1k], writes=[XGT[b][:]])

    def prep(e_):
        b = e_ % 2
        XN = XNs[b]
        for j in range(4):
            S.op("scalar", lambda e, b=b, j=j: e.activation(SQ[:], XGT[b][:, j, :], AF.Square, accum_out=SS[b][:, j:j + 1]),
                 reads=[XGT[b][:]], writes=[SQ[:], SS[b][:]])
        S.ts(SS[b][:], SS[b][:], 1.0 / 1024, eps, ALU.mult, ALU.add)
        S.act(SS[b][:], SS[b][:], AF.Sqrt)
        S.op("vector", lambda e, b=b: e.reciprocal(SS[b][:], SS[b][:]), reads=[SS[b][:]], writes=[SS[b][:]])
        for j in range(4):
            S.op("vector" if j % 2 == 0 else "gpsimd", lambda e, b=b, j=j: e.tensor_scalar(XGT[b][:, j, :], XGT[b][:, j, :], SS[b][:, j:j + 1], None, ALU.mult),
                 reads=[XGT[b][:], SS[b][:]], writes=[XGT[b][:]])
        for k in range(8):
            p = pt[k % 2]
            for j in range(4):
                S.tr(p[:, j * 128:(j + 1) * 128], XGT[b][:, j, k * 128:(k + 1) * 128], idt)
            if k % 2 == 0:
                S.op("scalar", lambda e, k=k, p=p, XN=XN: e.activation(XN[:, k, :], p, AF.Copy, scale=G2[:, k:k + 1]), reads=[p, G2[:]], writes=[XN[:]])
            else:
                S.op("vector", lambda e, k=k, p=p, XN=XN: e.tensor_scalar(XN[:, k, :], p, G2[:, k:k + 1], None, ALU.mult), reads=[p, G2[:]], writes=[XN[:]])

    load_w(0)
    gather(0)
    prep(0)
    for s in range(NS):
        e_, hf = divmod(s, 2)
        sl = s % 2
        b = e_ % 2
        XN = XNs[b]
        b3 = e_ % 3
        if hf == 0 and e_ + 1 < NE:
            gather(e_ + 1)
        if s + 1 < NS:
            load_w(s + 1)
        for f in range(8):
            a, u = pg[f % 2], pu[f % 2]
            for k in range(8):
                S.op("tensor", lambda e, a=a, k=k, f=f, sl=sl, XN=XN: e.matmul(a, WG[sl][:, k, f * 128:(f + 1) * 128], XN[:, k, :], start=(k == 0), stop=(k == 7)),
                     reads=[WG[sl][:], XN[:]], writes=[a])
            for k in range(8):
                S.op("tensor", lambda e, u=u, k=k, f=f, sl=sl, XN=XN: e.matmul(u, WU[sl][:, k, f * 128:(f + 1) * 128], XN[:, k, :], start=(k == 0), stop=(k == 7)),
                     reads=[WU[sl][:], XN[:]], writes=[u])
            gs = GS[f % 2]
            S.act(gs[:], a, AF.Silu)
            S.op("vector", lambda e, f=f, u=u, gs=gs: e.tensor_tensor(HID[:, f, :], u, gs[:], ALU.mult), reads=[u, gs[:]], writes=[HID[:]])
        if hf == 1 and e_ + 1 < NE:
            prep(e_ + 1)
        for j in range(4):
            ob = OB[j % 2]
            for dh in range(2):
                p = po[dh]
                ds_ = slice(dh * 512, (dh + 1) * 512)
                for k in range(8):
                    S.op("tensor", lambda e, p=p, k=k, j=j, ds_=ds_, sl=sl: e.matmul(p, HID[:, k, j * 128:(j + 1) * 128], WD[sl][:, k, ds_], start=(k == 0), stop=(k == 7)),
                         reads=[HID[:], WD[sl][:]], writes=[p])
                if hf == 0:
                    if dh == 0:
                        S.op("scalar", lambda e, p=p, j=j, ds_=ds_: e.copy(ACC[:, j, ds_], p), reads=[p], writes=[ACC[:]])
                    else:
                        S.op("vector", lambda e, p=p, j=j, ds_=ds_: e.tensor_copy(ACC[:, j, ds_], p), reads=[p], writes=[ACC[:]])
                else:
                    S.op("vector", lambda e, p=p, j=j, ds_=ds_, ob=ob: e.tensor_tensor(ob[:, ds_], p, ACC[:, j, ds_], ALU.add), reads=[p, ACC[:]], writes=[ob[:]])
            if hf == 1:
                S.op("scalar", lambda e, ob=ob, j=j, b3=b3: e.activation(ob[:], ob[:], AF.Copy, scale=GT[b3][:, j:j + 1]), reads=[ob[:], GT[b3][:]], writes=[ob[:]])
                S.raw_dma("gpsimd", lambda e, ob=ob, j=j, b3=b3: e.indirect_dma_start(out=x2k, out_offset=bass.IndirectOffsetOnAxis(ap=IX[b3][:, j:j + 1], axis=0),
                          in_=ob[:], in_offset=None, compute_op=ALU.add), reads=[ob[:], IX[b3][:]], writes=[P + "x2k"])


L_ = 4096
NF_, NT_ = 2208, 1288

def build_fused(depth=2, upto=99, debug=False):
    nc = bass.Bass("TRN2", target_bir_lowering=False)
    L = L_
    def din(n, s, dt=F32):
        return nc.dram_tensor(n, s, dt, kind="ExternalInput").ap()
    def scr(n, s, dt=F32):
        if debug:
            return nc.dram_tensor(n, s, dt, kind="ExternalOutput").ap()
        return nc.dram_tensor(n, s, dt, kind="Internal", addr_space="Local").ap()
    xT = din("xT", [1024, L])
    Wl = []
    for l in range(depth):
        d = {}
        for n, s in (("wF", [1024, NF_]), ("wT", [1024, NT_]), ("ln1", [1024]), ("wuq", [256, 384]), ("qn", [256]),
                     ("wkn", [128, 256]), ("wv", [128, 256]), ("kvn", [128]), ("cw", [2, 3, 128, 5]), ("cb", [2, 3, 128, 1]),
                     ("dtb", [2, L, 4]), ("alog", [2, L, 4]), ("dsk", [2, 128, 2]), ("snw", [256]), ("wout", [1024, 1024]),
                     ("ln2", [1024]), ("rw", [1024, 16]), ("wg", [16, 1024, 2048]), ("wu", [16, 1024, 2048]), ("wd", [16, 2048, 1024])):
            d[n] = din("%s%d" % (n, l), s)
        Wl.append(d)
    fnw = din("fnw", [1024]); ones1k = din("ones1k", [1024])
    C = {}
    for n, s in (("rck", [64, L]), ("rsk", [64, L]), ("rct", [L, 64]), ("rst", [L, 64]), ("rdtot", [4, 128, 128]), ("rcols", [2, 128, 12]),
                 ("mcq", [96, L]), ("msq", [96, L]), ("mck", [96, L]), ("msk", [96, L]),
                 ("dcq", [64, L]), ("dsq", [64, L]), ("dck", [64, L]), ("dsk_", [64, L]), ("dmask", [20, 128, 512]),
                 ("ident", [128, 128]), ("sconst", [5, 128, 128]), ("tokid", [128, 32]), ("iota512", [128, 512])):
        C[n] = din(n, s)
    out = nc.dram_tensor("out", [L, 1024], F32, kind="ExternalOutput").ap()
    PF = scr("PF", [NF_, L]); PT = scr("PT", [L, NT_]); QF = scr("QF", [384, L]); KF = scr("KF", [256, L]); VT = scr("VT", [L, 256])
    YA, YB, YC, YD = (scr(n, [L, 256]) for n in ("YA", "YB", "YC", "YD"))
    MT = scr("MT", [1024, L]); X1T = scr("X1T", [1024, L])
    AFF = scr("AFF", [L, 16]); VALS = scr("VALS", [16, 512]); IDXU = scr("IDXU", [16, 512], U32)
    X1K = scr("X1K", [L, 1024]); X2K = scr("X2K", [L, 1024])
    X2T = [scr("X2T%d" % l, [1024, L]) for l in range(depth)]
    with ExitStack() as es:
        S = Sched(nc, es)
        with S.scope("c_"):
            pass
        IDT = S.sb([128, 128], F32, "IDT"); S.dma(IDT[:], C["ident"])
        idt = IDT[:]
        stage = [0]
        def go():
            stage[0] += 1
            return stage[0] <= upto
        cur = xT
        for l in range(depth):
            W = Wl[l]; pf = "l%d" % l
            if go():
                with S.scope(pf + "a_"):
                    emit_lin(S, cur, W["wT"], W["ln1"], PT, 1024, L, NT_, tag=pf + "a", NB=3)
            if go():
                with S.scope(pf + "b_"):
                    emit_linF(S, cur, W["wF"], W["ln1"], PF, 1024, L, NF_, tag=pf + "b")
            if go():
                for c2 in range(2):
                    hs = [2 * c2, 2 * c2 + 1]
                    with S.scope(pf + "r%d_" % c2):
                        emit_ret(S, L, 2, q=[PF[h * 64:(h + 1) * 64] for h in hs], k=[PF[256 + h * 64:256 + (h + 1) * 64] for h in hs],
                                 kt_=[PT[:, h * 64:(h + 1) * 64] for h in hs],
                                 v=[PT[:, 256 + h * 64:256 + (h + 1) * 64] for h in hs], g=PT[:, 512 + c2 * 128:512 + (c2 + 1) * 128],
                                 ck=C["rck"], sk=C["rsk"], ct=C["rct"], st_=C["rst"], dtot=C["rdtot"][2 * c2:2 * c2 + 2], cols=C["rcols"][c2],
                                 y=YA[:, c2 * 128:(c2 + 1) * 128])
            if go():
                with S.scope(pf + "m1_"):
                    emit_linF(S, PF[512:768], W["wuq"], W["qn"], QF, 256, L, 384, tag=pf + "m1")
                with S.scope(pf + "m2_"):
                    emit_linF(S, PF[768:896], W["wkn"], W["kvn"], KF, 128, L, 256, tag=pf + "m2")
                with S.scope(pf + "m3_"):
                    emit_lin(S, PF[768:896], W["wv"], W["kvn"], VT, 128, L, 256, tag=pf + "m3")
            if go():
                for c2 in range(2):
                    hs = [2 * c2, 2 * c2 + 1]
                    with S.scope(pf + "ma%d_" % c2):
                        emit_attn(S, 96, 64, L, 2, mla_plan(), 0, q=[[(0, 96, QF[h * 96:(h + 1) * 96])] for h in hs],
                                  qp=[[(0, 64, QF[h * 96:h * 96 + 64]), (64, 80, QF[h * 96 + 80:h * 96 + 96]), (80, 96, QF[h * 96 + 64:h * 96 + 80])] for h in hs],
                                  kpc=[[(0, 64, KF[h * 64:(h + 1) * 64]), (64, 96, PF[896:928])] for h in hs],
                                  kppc=[[(0, 64, KF[h * 64:(h + 1) * 64]), (64, 80, PF[912:928]), (80, 96, PF[896:912])] for h in hs],
                                  tabs_d=[C["mcq"], C["msq"], C["mck"], C["msk"]], v=[VT[:, h * 64:(h + 1) * 64] for h in hs], masks=None, idt=idt,
                                  y=YB[:, c2 * 128:(c2 + 1) * 128], tag=pf + "ma%d" % c2)
            if go():
                for g_ in range(2):
                    with S.scope(pf + "s%d_" % g_):
                        emit_ssd(S, L, PF[928 + g_ * 128:928 + (g_ + 1) * 128], PF[1184 + g_ * 128:1184 + (g_ + 1) * 128], PF[1440 + g_ * 128:1440 + (g_ + 1) * 128],
                                 z=PT[:, 768 + g_ * 128:768 + (g_ + 1) * 128], dtr=PT[:, 1024 + g_ * 4:1024 + (g_ + 1) * 4], cw=W["cw"][g_], cb=W["cb"][g_],
                                 dtb=W["dtb"][g_], alog=W["alog"][g_], dsk=W["dsk"][g_], consts=C["sconst"], y=YC[:, g_ * 128:(g_ + 1) * 128])
            if go():
                for c2 in range(2):
                    hs = [2 * c2, 2 * c2 + 1]
                    with S.scope(pf + "d%d_" % c2):
                        dq_ = lambda h: PF[1696 + h * 64:1696 + (h + 1) * 64]
                        dk_ = lambda h: PF[1952 + h * 64:1952 + (h + 1) * 64]
                        sw_ = lambda a: [(0, 8, a[8:16]), (8, 16, a[0:8]), (16, 64, a[16:64])]
                        emit_attn(S, 64, 64, L, 2, dil_plan(), 20, q=[[(0, 64, dq_(h))] for h in hs], qp=[sw_(dq_(h)) for h in hs],
                                  kpc=[[(0, 64, dk_(h))] for h in hs], kppc=[sw_(dk_(h)) for h in hs],
                                  tabs_d=[C["dcq"], C["dsq"], C["dck"], C["dsk_"]], v=[PT[:, 1032 + h * 64:1032 + (h + 1) * 64] for h in hs], masks=C["dmask"], idt=idt,
                                  y=YD[:, c2 * 128:(c2 + 1) * 128], tag=pf + "d%d" % c2)
            if go():
                with S.scope(pf + "t_"):
                    for i, (src, nw) in enumerate(((YA, None), (YB, None), (YC, W["snw"]), (YD, None))):
                        emit_transpose(S, src, MT[i * 256:(i + 1) * 256], L, 256, idt, norm_w=nw, tag=pf + "t%d" % i, nm="m%d" % i)
            if go():
                with S.scope(pf + "o_"):
                    emit_linF(S, MT, W["wout"], ones1k, X1T, 1024, L, 1024, norm=False, resid=cur, tag=pf + "o")
            if go():
                with S.scope(pf + "q_"):
                    emit_lin(S, X1T, W["rw"], W["ln2"], AFF, 1024, L, 16, softmax=True, fp32=True, tag=pf + "q", NB=4)
                with S.scope(pf + "k_"):
                    emit_topk(S, AFF, VALS, IDXU, idt, L,
                              mid_hook=lambda: emit_untranspose(S, X1T, X1K, L, 1024, idt, tag=pf + "u", evac="scalar"))
            if go():
                with S.scope(pf + "e_"):
                    emit_moe(S, X1K, VALS, IDXU, W["ln2"], W["wg"], W["wu"], W["wd"], X2K, idt, L)
            if go():
                with S.scope(pf + "c_"):
                    emit_transpose(S, X2K, X2T[l], L, 1024, idt, tag=pf + "c")
            cur = X2T[l]
            S.new_epoch()
        if go():
            with S.scope("fin_"):
                emit_finalnorm(S, cur, fnw, out, L, idt)
        else:
            with S.scope("fin_"):
                z_ = S.sb([128, 1024], F32, "z_"); S.memset(z_[:], 0.0)
                S.dma(out[0:128, :], z_[:])
        S.finish()
    return nc


def _perm_ret():
    return np.array([h * 64 + ((d + 32) % 64) for h in range(4) for d in range(64)])

def _perm_dil():
    def sw(d):
        return d + 8 if d < 8 else (d - 8 if d < 16 else d)
    return np.array([h * 64 + sw(d) for h in range(4) for d in range(64)])

def host_consts():
    L = L_
    C = {}
    rc = ret_consts(L, [0, 1, 2, 3])
    C["rck"], C["rsk"], C["rct"], C["rst"], C["rdtot"] = rc["ck"], rc["sk"], rc["ct"], rc["st"], rc["dtot"]
    C["rcols"] = np.ascontiguousarray(np.stack([rc["cols"][:, 0:12], rc["cols"][:, 12:24]]))
    C["mcq"], C["msq"] = rot_tables(96, L, 64, 96, 500000.0, 96 ** -0.5)
    C["mck"], C["msk"] = rot_tables(96, L, 64, 96, 500000.0, 1.0)
    C["dcq"], C["dsq"] = rot_tables(64, L, 0, 16, 500000.0, 64 ** -0.5)
    C["dck"], C["dsk_"] = rot_tables(64, L, 0, 16, 500000.0, 1.0)
    C["dmask"] = dil_masks()
    C["ident"] = np.eye(128, dtype=np.float32)
    C["sconst"] = ssd_consts()
    C["tokid"] = (np.arange(128)[:, None] + 128 * np.arange(32)[None, :]).astype(np.float32)
    C["iota512"] = np.broadcast_to(np.arange(512, dtype=np.float32)[None, :], (128, 512)).copy()
    return C

def host_layer_weights(P, l):
    L = L_
    f = lambda a: np.ascontiguousarray(a, dtype=np.float32)
    w = P['w_in'][l]
    rq, rk, rv, rg = w[:, 0:256], w[:, 256:512], w[:, 512:768], w[:, 768:1024]
    cq, ckv, kr = w[:, 1024:1280], w[:, 1280:1408], w[:, 1408:1440]
    sz, sxbc, sdt = w[:, 1440:1696], w[:, 1696:2464], w[:, 2464:2472]
    dq, dk, dv = w[:, 2472:2728], w[:, 2728:2984], w[:, 2984:3240]
    pr, pd = _perm_ret(), _perm_dil()
    krs = np.concatenate([kr[:, 16:32], kr[:, 0:16]], axis=1)
    wF = np.concatenate([rq, rk, cq, ckv, kr, sxbc, dq, dk], axis=1)
    dtc = [0, 1, 4, 5, 2, 3, 6, 7]
    wT = np.concatenate([rk, rv, rg, sz, sdt[:, dtc], dv], axis=1)
    assert wF.shape[1] == NF_ and wT.shape[1] == NT_
    uq = P['mla_w_uq'][l]
    def swq(j):
        return j + 16 if 64 <= j < 80 else (j - 16 if 80 <= j < 96 else j)
    pq = np.array([h * 96 + swq(j) for h in range(4) for j in range(96)])
    wuq = uq
    ukv = P['mla_w_ukv'][l].reshape(128, 4, 128)
    wkn = ukv[:, :, 0:64].reshape(128, 256); wv = ukv[:, :, 64:128].reshape(128, 256)
    cwt = np.zeros((2, 3, 128, 5), np.float32); cbt = np.zeros((2, 3, 128, 1), np.float32)
    dtb = np.zeros((2, L, 4), np.float32); alog = np.zeros((2, L, 4), np.float32); dsk = np.zeros((2, 128, 2), np.float32)
    for g_ in range(2):
        for i, c0 in enumerate((g_ * 128, 256 + g_ * 128, 512 + g_ * 128)):
            cols = list(range(c0, c0 + 128))
            cwt[g_, i] = P['ssm_conv_w'][l][:, cols].T
            cbt[g_, i, :, 0] = P['ssm_conv_b'][l][cols]
        hs = [2 * g_, 2 * g_ + 1]
        dcols = [hs[0], hs[1], 4 + hs[0], 4 + hs[1]]
        dtb[g_] = P['ssm_dt_bias'][l].reshape(8)[dcols][None]
        alog[g_] = P['ssm_a_log'][l].reshape(8)[dcols][None]
        dsk[g_] = P['ssm_d'][l][hs][None]
    return dict(wF=f(wF), wT=f(wT), ln1=f(P['ln1_w'][l]), wuq=f(wuq), qn=f(P['mla_q_norm_w'][l]), wkn=f(wkn), wv=f(wv),
                kvn=f(P['mla_kv_norm_w'][l]), cw=cwt, cb=cbt, dtb=dtb, alog=alog, dsk=dsk, snw=f(P['ssm_norm_w'][l]),
                wout=f(P['w_out'][l]), ln2=f(P['ln2_w'][l]), rw=f(P['router_w'][l]), wg=f(P['exp_w_gate'][l]), wu=f(P['exp_w_up'][l]),
                wd=f(P['exp_w_down'][l]))

def kernel(**inputs):
    P = {k: np.asarray(v) for k, v in inputs.items()}
    x = np.asarray(P['x'], dtype=np.float32)
    nc = build_fused(depth=2)
    base = dict(host_consts())
    base["fnw"] = np.ascontiguousarray(P['final_norm_w'], dtype=np.float32)
    base["ones1k"] = np.ones(1024, np.float32)
    for l in range(2):
        for k_, v_ in host_layer_weights(P, l).items():
            base["%s%d" % (k_, l)] = v_
    in_maps = []
    for b in range(4):
        m = dict(base)
        m["xT"] = np.ascontiguousarray(x[b].T)
        in_maps.append(m)
    res = run_bass_kernel_spmd(nc, in_maps, core_ids=[0, 1, 2, 3])
    return np.stack([res.results[b]["out"] for b in range(4)]).astype(np.float32)
```

```python
import numpy as np
from contextlib import ExitStack
import concourse.bass as bass
import concourse.mybir as mybir
from concourse.bass_utils import run_bass_kernel_spmd
F32 = mybir.dt.float32
BF16 = mybir.dt.bfloat16
U32 = mybir.dt.uint32
AF = mybir.ActivationFunctionType
ALU = mybir.AluOpType
AX = mybir.AxisListType


CE = ("tensor", "vector", "scalar", "gpsimd")
ENGS = ("sync",) + CE
NDS = 48


def _key(x):
    if isinstance(x, str):
        return x
    if isinstance(x, tuple):
        return x[1]
    return x.tensor.name


def _ap(x):
    return x[0] if isinstance(x, tuple) else x


class Sched:
    def __init__(self, nc, es):
        self.nc, self.es = nc, es
        self.q = {e: [] for e in ENGS}
        self.cnt = {e: 0 for e in CE}
        self.sem = {e: es.enter_context(nc.semaphore("s_" + e)) for e in CE}
        self.dsem = [es.enter_context(nc.semaphore("d%d" % i)) for i in range(NDS)]
        self.dcnt = [0] * NDS
        self.dnext = 0
        self.seen_c = {e: {f: 0 for f in CE} for e in ENGS}
        self.seen_d = {e: [0] * NDS for e in ENGS}
        self.last_w = {}
        self.readers = {}
        self.n_alloc = 0
        self.psum_keys = set()
        self.ninstr = 0
        self.prefix = ""
        self.root_es = es
        self.epoch = 0

    def sb(self, shape, dtype, name=None):
        self.n_alloc += 1
        name = self.prefix + (name or "t%d" % self.n_alloc)
        return self.es.enter_context(self.nc.sbuf_tensor(name, list(shape), dtype))

    def ps(self, shape, dtype, name=None):
        self.n_alloc += 1
        name = self.prefix + (name or "p%d" % self.n_alloc)
        t = self.es.enter_context(self.nc.psum_tensor(name, [128, 512], mybir.dt.float32))
        self.psum_keys.add(name)
        return t[0:shape[0], 0:shape[1]]

    def scope(self, prefix):
        sched = self

        class _Scope:
            def __enter__(self_):
                self_.old = (sched.es, sched.prefix)
                self_.stack = ExitStack()
                self_.stack.__enter__()
                sched.es, sched.prefix = self_.stack, prefix
                return sched

            def __exit__(self_, *a):
                sched.barrier()
                sched.es, sched.prefix = self_.old
                return self_.stack.__exit__(*a)
        return _Scope()

    def _wait(self, stream, tok):
        if tok is None:
            return
        if tok[0] == "c":
            _, eng, n, ep = tok
            if ep < self.epoch:
                return
            if stream == "tensor" and eng == "tensor":
                return
            if self.seen_c[stream][eng] >= n:
                return
            self.seen_c[stream][eng] = n
            sem = self.sem[eng]
            self.q[stream].append(lambda e, sem=sem, n=n: e.wait_ge(sem, n))
        else:
            _, i, n = tok
            if self.seen_d[stream][i] >= n:
                return
            self.seen_d[stream][i] = n
            sem = self.dsem[i]
            self.q[stream].append(lambda e, sem=sem, n=n: e.wait_ge(sem, n))

    def _deps(self, stream, reads, writes):
        for r in reads:
            self._wait(stream, self.last_w.get(_key(r)))
        for w in writes:
            k = _key(w)
            self._wait(stream, self.last_w.get(k))
            for t in self.readers.get(k, ()):
                self._wait(stream, t)

    def _commit(self, tok, reads, writes):
        for r in reads:
            self.readers.setdefault(_key(r), []).append(tok)
        for w in writes:
            k = _key(w)
            self.last_w[k] = tok
            self.readers[k] = []

    def op(self, eng, fn, reads=(), writes=()):
        extra = [r for r in reads if _key(r) in self.psum_keys]
        if extra:
            writes = list(writes) + extra
        self._deps(eng, reads, writes)
        self.cnt[eng] += 1
        n = self.cnt[eng]
        sem = self.sem[eng]
        self.q[eng].append(lambda e, fn=fn, sem=sem: fn(e).then_inc(sem, 1))
        self.seen_c[eng][eng] = max(self.seen_c[eng][eng], 0)
        self._commit(("c", eng, n, self.epoch), reads, writes)
        self.ninstr += 1

    def dma(self, out, in_, queue="sync", reads=None, writes=None, **kw):
        reads = [in_] if reads is None else reads
        writes = [out] if writes is None else writes
        self._deps(queue, reads, writes)
        i = self.dnext
        self.dnext = (self.dnext + 1) % NDS
        if self.dcnt[i]:
            self._wait(queue, ("d", i, self.dcnt[i]))
        self.dcnt[i] += 16
        sem = self.dsem[i]
        o, a = _ap(out), _ap(in_)
        self.q[queue].append(
            lambda e, o=o, a=a, sem=sem, kw=kw: e.dma_start(out=o, in_=a, **kw).then_inc(sem, 16))
        self._commit(("d", i, self.dcnt[i]), reads, writes)
        self.ninstr += 1

    def raw_dma(self, queue, fn, reads, writes):
        self._deps(queue, reads, writes)
        i = self.dnext
        self.dnext = (self.dnext + 1) % NDS
        if self.dcnt[i]:
            self._wait(queue, ("d", i, self.dcnt[i]))
        self.dcnt[i] += 16
        sem = self.dsem[i]
        self.q[queue].append(lambda e, fn=fn, sem=sem: fn(e).then_inc(sem, 16))
        self._commit(("d", i, self.dcnt[i]), reads, writes)
        self.ninstr += 1

    def barrier(self):
        for s in ENGS:
            for e in CE:
                if self.cnt[e]:
                    self._wait(s, ("c", e, self.cnt[e], self.epoch))
            for i in range(NDS):
                if self.dcnt[i]:
                    self._wait(s, ("d", i, self.dcnt[i]))

    def new_epoch(self):
        self.barrier()
        self.epoch += 1
        self.sem = {e: self.root_es.enter_context(self.nc.semaphore("s%d_%s" % (self.epoch, e))) for e in CE}
        self.cnt = {e: 0 for e in CE}
        self.seen_c = {e: {f: 0 for f in CE} for e in ENGS}

    def finish(self):
        for e in CE:
            if self.cnt[e]:
                self._wait("sync", ("c", e, self.cnt[e], self.epoch))
        for i in range(NDS):
            if self.dcnt[i]:
                self._wait("sync", ("d", i, self.dcnt[i]))
        with self.nc.Block() as block:
            @block.sync
            def _(e):
                for f in self.q["sync"]:
                    f(e)

            @block.tensor
            def _(e):
                for f in self.q["tensor"]:
                    f(e)

            @block.vector
            def _(e):
                for f in self.q["vector"]:
                    f(e)

            @block.scalar
            def _(e):
                for f in self.q["scalar"]:
                    f(e)

            @block.gpsimd
            def _(e):
                for f in self.q["gpsimd"]:
                    f(e)

    def mm(self, out, lhsT, rhs, start=True, stop=True, **kw):
        o, l, r = _ap(out), _ap(lhsT), _ap(rhs)
        self.op("tensor", lambda e: e.matmul(o, l, r, start=start, stop=stop, **kw),
                reads=[lhsT, rhs] + ([] if start else [out]), writes=[out])

    def tr(self, out, in_, ident):
        o, a, i = _ap(out), _ap(in_), _ap(ident)
        self.op("tensor", lambda e: e.transpose(o, a, i), reads=[in_, ident], writes=[out])

    def act(self, out, in_, func, bias=None, scale=None, accum_out=None, eng="scalar", extra_reads=()):
        o, a = _ap(out), _ap(in_)
        kw = {}
        reads = [in_] + list(extra_reads)
        writes = [out]
        if bias is not None:
            kw["bias"] = _ap(bias) if not isinstance(bias, (int, float)) else bias
            if not isinstance(bias, (int, float)):
                reads.append(bias)
        if scale is not None:
            kw["scale"] = _ap(scale) if not isinstance(scale, (int, float)) else scale
            if not isinstance(scale, (int, float)):
                reads.append(scale)
        if accum_out is not None:
            kw["accum_out"] = _ap(accum_out)
            writes.append(accum_out)
        self.op("scalar", lambda e: e.activation(o, a, func, **kw), reads=reads, writes=writes)

    def tt(self, out, in0, in1, op, eng="vector"):
        o, a, b = _ap(out), _ap(in0), _ap(in1)
        self.op(eng, lambda e: e.tensor_tensor(o, a, b, op), reads=[in0, in1], writes=[out])

    def ts(self, out, in0, s1, s2, op0, op1=None, eng="vector", accum_out=None):
        o, a = _ap(out), _ap(in0)
        reads = [in0]
        v1 = s1
        if not isinstance(s1, (int, float)):
            reads.append(s1)
            v1 = _ap(s1)
        v2 = s2
        if s2 is not None and not isinstance(s2, (int, float)):
            reads.append(s2)
            v2 = _ap(s2)
        writes = [out]
        kw = {}
        if accum_out is not None:
            kw["accum_out"] = _ap(accum_out)
            writes.append(accum_out)
        if op1 is None:
            self.op(eng, lambda e: e.tensor_scalar(o, a, v1, v2, op0, **kw), reads=reads, writes=writes)
        else:
            self.op(eng, lambda e: e.tensor_scalar(o, a, v1, v2, op0, op1, **kw), reads=reads, writes=writes)

    def stt(self, out, in0, scalar, in1, op0, op1, eng="vector"):
        o, a, b = _ap(out), _ap(in0), _ap(in1)
        reads = [in0, in1]
        v = scalar
        if not isinstance(scalar, (int, float)):
            reads.append(scalar)
            v = _ap(scalar)
        self.op(eng, lambda e: e.scalar_tensor_tensor(o, a, v, b, op0, op1), reads=reads, writes=[out])

    def copy(self, out, in_, eng="vector"):
        o, a = _ap(out), _ap(in_)
        if eng == "scalar":
            self.op(eng, lambda e: e.copy(o, a), reads=[in_], writes=[out])
        else:
            self.op(eng, lambda e: e.tensor_copy(o, a), reads=[in_], writes=[out])

    def memset(self, out, val, eng="vector"):
        o = _ap(out)
        self.op(eng, lambda e: e.memset(o, val), reads=[], writes=[out])


def half_swap(a, lo, hi):
    b = a.copy()
    m = (lo + hi) // 2
    b[..., lo:m, :] = a[..., m:hi, :]
    b[..., m:hi, :] = a[..., lo:m, :]
    return b

def rot_tables(dk, L, lo, hi, theta, scale):
    C = np.ones((dk, L), np.float64)
    Sg = np.zeros((dk, L), np.float64)
    rd = hi - lo
    inv = 1.0 / (theta ** (np.arange(0, rd, 2, dtype=np.float32) / np.float32(rd))).astype(np.float32)
    ang = (np.arange(L, dtype=np.float32)[:, None] * inv[None, :]).astype(np.float32)
    c, s = np.cos(ang.astype(np.float64)).T, np.sin(ang.astype(np.float64)).T
    C[lo:lo + rd // 2] = c
    C[lo + rd // 2:hi] = c
    Sg[lo:lo + rd // 2] = -s
    Sg[lo + rd // 2:hi] = s
    return (C * scale).astype(np.float32), (Sg * scale).astype(np.float32)

def dil_masks():
    M = np.zeros((20, 128, 512), np.float32)
    j = np.arange(128)[:, None]
    i = np.arange(512)[None, :]
    for rr in range(20):
        d_ = 128 * (rr - 8) + j - i
        for (win, dil) in ((128, 1), (512, 4), (2048, 16)):
            M[rr] += ((d_ % dil == 0) & (np.abs(d_) <= (win // (2 * dil)) * dil)).astype(np.float32)
    return M

def dil_plan():
    return [[(t, t - 4 * g + 8) for t in range(4 * g - 8, 4 * g + 12) if 0 <= t < 32] for g in range(8)]

def mla_plan():
    return [[(t, None) for t in range(32)] for g in range(8)]


def ret_consts(L, heads):
    NH = len(heads)
    inv = 1.0 / (10000.0 ** (np.arange(0, 64, 2, dtype=np.float32) / np.float32(64))).astype(np.float32)
    ang = (np.arange(L, dtype=np.float32)[:, None] * inv[None, :]).astype(np.float32).astype(np.float64)
    c, s = np.cos(ang), np.sin(ang)
    ct = np.concatenate([c, c], 1); st = np.concatenate([-s, s], 1)
    ck, sk = ct.T.copy(), st.T.copy()
    dtot = np.zeros((NH, 128, 128)); cols = np.zeros((128, NH * 6))
    p = np.arange(128, dtype=np.float64)
    for n, h in enumerate(heads):
        lgf = np.log1p(-np.exp2(np.float64(-5.0 - 0.0 - h))); lgb = np.log1p(-np.exp2(np.float64(-5.0 - 0.5 - h)))
        j = p[:, None]; i = p[None, :]
        dtot[n] = 0.125 * (np.where(i >= j, np.exp(lgf * np.maximum(i - j, 0)), 0.0) + np.where(j > i, np.exp(lgb * np.maximum(j - i, 0)), 0.0))
        cols[:, n * 6 + 0] = 0.125 * np.exp(lgf * (p + 1))
        cols[:, n * 6 + 1] = 0.125 * np.exp(lgb * (128 - p))
        cols[:, n * 6 + 2] = np.exp(lgf * (127 - p))
        cols[:, n * 6 + 3] = np.exp(lgb * p)
        cols[:, n * 6 + 4] = np.exp(lgf * 128)
        cols[:, n * 6 + 5] = np.exp(lgb * 128)
    f = lambda a: np.ascontiguousarray(a, dtype=np.float32)
    return dict(ck=f(ck), sk=f(sk), ct=f(ct), st=f(st), dtot=f(dtot), cols=f(cols))

def swap64(a, axis):
    return np.concatenate([np.take(a, range(32, 64), axis), np.take(a, range(0, 32), axis)], axis)


def ssd_consts():
    s = np.arange(128)[:, None]; l = np.arange(128)[None, :]
    return np.stack([(s <= l), (s >= l), (l >= s), (l < s), (s == l)]).astype(np.float32)


def emit_lin(S, xT, w, g, out, K, ntok, N, norm=True, softmax=False, fp32=False, eps=1e-6, tag="o", NB=2):
    KT = K // 128
    MD = F32 if fp32 else BF16
    W = S.sb([128, KT, N], MD, "W")
    Wraw = [S.sb([128, N], F32, "Wraw%d" % i) for i in range(2)]
    gs = S.sb([128, KT], F32, "gs")
    ones = S.sb([128, 1], F32, "ones")
    S.memset(ones[:], 1.0)
    S.dma(gs[:], g.rearrange("(k p) -> p k", p=128), allow_slow_non_contiguous=True)
    wv = w.rearrange("(k p) n -> p k n", p=128)
    for k in range(KT):
        S.dma(Wraw[k % 2][:], wv[:, k, :], queue="sync" if k % 2 == 0 else "gpsimd")
        S.ts((W[:, k, :], "W%d" % k), Wraw[k % 2][:], gs[:, k:k + 1], None, ALU.mult,
             eng="vector" if k % 2 == 0 else "gpsimd")
    x32 = [S.sb([128, KT, 128], F32, "x32_%d" % i) for i in range(NB)]
    xt = x32 if fp32 else [S.sb([128, KT, 128], BF16, "xt%d" % i) for i in range(NB)]
    sq = [S.sb([128, KT, 128], F32, "sq%d" % i) for i in range(NB)]
    ob = [S.sb([128, N], F32, "ob%d" % i) for i in range(NB)]
    rstd = [S.sb([128, 1], F32, "rstd%d" % i) for i in range(NB)]
    mx = [S.sb([128, 1], F32, "mx%d" % i) for i in range(NB)]
    sm = [S.sb([128, 1], F32, "sm%d" % i) for i in range(NB)]
    ssp = S.ps([128, 1], F32, "ssp")
    pp = [S.ps([128, 512], F32, "pp%d" % i) for i in range(4)]
    xv = xT.rearrange("(k p) t -> p k t", p=128)
    groups = [(c, min(512, N - c)) for c in range(0, N, 512)]
    gi = 0
    for t in range(ntok // 128):
        b = t % NB
        S.dma(x32[b][:], xv[:, :, t * 128:(t + 1) * 128], queue="sync")
        if not fp32:
            S.copy(xt[b][:], x32[b][:], eng="gpsimd")
        if norm:
            S.act(sq[b][:], x32[b][:], AF.Square)
            for k in range(KT):
                S.mm(ssp, sq[b][:, k, :], ones[:], start=(k == 0), stop=(k == KT - 1))
            S.ts(rstd[b][:], ssp, 1.0 / K, eps, ALU.mult, ALU.add)
            S.act(rstd[b][:], rstd[b][:], AF.Sqrt)
            S.op("vector", lambda e, b=b: e.reciprocal(rstd[b][:], rstd[b][:]), reads=[rstd[b][:]], writes=[rstd[b][:]])
        else:
            S.memset(rstd[b][:], 1.0)
        keys = []
        for (c0, cw) in groups:
            p = pp[gi % 4]
            for k in range(KT):
                S.op("tensor", (lambda e, p=p, b=b, k=k, c0=c0, cw=cw: e.matmul(p[:, :cw], xt[b][:, k, :], W[:, k, c0:c0 + cw], start=(k == 0), stop=(k == KT - 1))),
                     reads=[xt[b][:], "W%d" % k], writes=[p])
            key = "ob%d_%d" % (b, c0)
            keys.append(key)
            if gi % 2 == 0:
                S.act((ob[b][:, c0:c0 + cw], key), p[:, :cw], AF.Copy, scale=rstd[b][:])
            else:
                S.ts((ob[b][:, c0:c0 + cw], key), p[:, :cw], rstd[b][:], None, ALU.mult)
            gi += 1
        if softmax:
            key = keys[0]
            S.op("vector", lambda e, b=b: e.reduce_max(mx[b][:], ob[b][:], AX.X), reads=[key], writes=[mx[b][:]])
            S.ts(mx[b][:], mx[b][:], -1.0, None, ALU.mult)
            S.op("scalar", lambda e, b=b: e.activation(ob[b][:], ob[b][:], AF.Exp, bias=mx[b][:], accum_out=sm[b][:]),
                 reads=[key, mx[b][:]], writes=[key, sm[b][:]])
            S.op("vector", lambda e, b=b: e.reciprocal(sm[b][:], sm[b][:]), reads=[sm[b][:]], writes=[sm[b][:]])
            S.op("vector", lambda e, b=b: e.tensor_scalar(ob[b][:], ob[b][:], sm[b][:], None, ALU.mult), reads=[key, sm[b][:]], writes=[key])
        wk = "%s_w%d" % (tag, t)
        S.dma(out[t * 128:(t + 1) * 128, :], ob[b][:], queue="gpsimd", reads=keys, writes=[wk])
        for key in keys:
            S.readers.setdefault(key, []).append(S.last_w[wk])


def emit_linF(S, xT, w, g, outT, K, ntok, N, norm=True, resid=None, eps=1e-6, tag="f"):
    KT = K // 128
    W = S.sb([128, KT, N], BF16, "W")
    Wraw = [S.sb([128, N], F32, "Wraw%d" % i) for i in range(2)]
    gs = S.sb([128, KT], F32, "gs")
    ones = S.sb([128, 128], F32, "ones")
    S.memset(ones[:], 1.0)
    S.dma(gs[:], g.rearrange("(k p) -> p k", p=128), allow_slow_non_contiguous=True)
    wv = w.rearrange("(k p) n -> p k n", p=128)
    for k in range(KT):
        S.dma(Wraw[k % 2][:], wv[:, k, :], queue="sync" if k % 2 == 0 else "gpsimd")
        S.ts((W[:, k, :], "W%d" % k), Wraw[k % 2][:], gs[:, k:k + 1], None, ALU.mult,
             eng="vector" if k % 2 == 0 else "gpsimd")
    NB = 2
    x32 = [S.sb([128, KT, 512], F32, "x32_%d" % i) for i in range(NB)]
    xt = [S.sb([128, KT, 512], BF16, "xt%d" % i) for i in range(NB)]
    sq = [S.sb([128, 512], F32, "sq%d" % i) for i in range(NB)]
    rb = [S.sb([128, 512], F32, "rb%d" % i) for i in range(NB)]
    ob = [S.sb([128, 512], F32, "ob%d" % i) for i in range(4)]
    rs = [S.sb([128, 512], F32, "rs%d" % i) for i in range(2)] if resid is not None else None
    pss = S.ps([128, 512], F32, "pss")
    pp = [S.ps([128, 512], F32, "pp%d" % i) for i in range(4)]
    xv = xT.rearrange("(k p) t -> p k t", p=128)
    mts = [(m, min(128, N - m)) for m in range(0, N, 128)]
    gi = 0
    for t in range(ntok // 512):
        b = t % NB
        ts_ = slice(t * 512, (t + 1) * 512)
        S.dma(x32[b][:], xv[:, :, ts_], queue="sync")
        S.copy(xt[b][:], x32[b][:], eng="gpsimd")
        if norm:
            for k in range(KT):
                S.act(sq[b][:], x32[b][:, k, :], AF.Square)
                S.mm(pss, ones[:], sq[b][:], start=(k == 0), stop=(k == KT - 1))
            S.ts(rb[b][:], pss, 1.0 / K, eps, ALU.mult, ALU.add)
            S.act(rb[b][:], rb[b][:], AF.Sqrt)
            S.op("vector", lambda e, b=b: e.reciprocal(rb[b][:], rb[b][:]), reads=[rb[b][:]], writes=[rb[b][:]])
        for (m0, mw) in mts:
            p = pp[gi % 4]
            o = ob[gi % 4]
            for k in range(KT):
                S.op("tensor", (lambda e, p=p, b=b, k=k, m0=m0, mw=mw: e.matmul(p[0:mw, :], W[:, k, m0:m0 + mw], xt[b][:, k, :], start=(k == 0), stop=(k == KT - 1))),
                     reads=[xt[b][:], "W%d" % k], writes=[p])
            if resid is not None:
                r_ = rs[gi % 2]
                S.dma(r_[0:mw, :], resid[m0:m0 + mw, ts_], queue="sync")
                S.tt(o[0:mw, :], p[0:mw, :], r_[0:mw, :], ALU.add, eng="vector")
            elif norm:
                S.tt(o[0:mw, :], p[0:mw, :], rb[b][0:mw, :], ALU.mult, eng="vector")
            else:
                S.copy(o[0:mw, :], p[0:mw, :], eng="scalar")
            wk = "%s_w%d_%d" % (tag, t, m0)
            S.dma(outT[m0:m0 + mw, ts_], o[0:mw, :], queue="gpsimd", reads=[o[:]], writes=[wk])
            S.readers.setdefault(_key(o[:]), []).append(S.last_w[wk])
            gi += 1


def emit_transpose(S, src, dstT, L, C, idt, norm_w=None, eps=1e-6, tag="tr", nm=""):
    CT = C // 128
    NB = 2
    xin = [S.sb([128, C], F32, nm + "xin%d" % i) for i in range(NB)]
    sqs = S.sb([128, C], F32, nm + "sqs")
    rstd = [S.sb([128, 1], F32, nm + "rstd%d" % i) for i in range(NB)]
    ot = [S.sb([128, 4, 128], F32, nm + "ot%d" % i) for i in range(2 * CT)]
    pt = [S.ps([128, 512], F32, nm + "pt%d" % i) for i in range(2)]
    nw = None
    if norm_w is not None:
        nw = S.sb([128, CT], F32, nm + "nw")
        S.dma(nw[:], norm_w.rearrange("(k p) -> p k", p=128), allow_slow_non_contiguous=True)
    gi = 0
    for t4 in range(L // 512):
        for j in range(4):
            t = t4 * 4 + j
            b = t % NB
            S.dma(xin[b][:], src[t * 128:(t + 1) * 128, :], queue="sync" if t % 2 == 0 else "gpsimd")
            if norm_w is not None:
                S.op("scalar", lambda e, b=b: e.activation(sqs[:], xin[b][:], AF.Square, accum_out=rstd[b][:]),
                     reads=[xin[b][:]], writes=[sqs[:], rstd[b][:]])
                S.ts(rstd[b][:], rstd[b][:], 1.0 / C, eps, ALU.mult, ALU.add)
                S.act(rstd[b][:], rstd[b][:], AF.Sqrt)
                S.op("vector", lambda e, b=b: e.reciprocal(rstd[b][:], rstd[b][:]), reads=[rstd[b][:]], writes=[rstd[b][:]])
                S.ts(xin[b][:], xin[b][:], rstd[b][:], None, ALU.mult)
            for c in range(CT):
                p = pt[gi % 2]
                S.tr(p[:, 0:128], xin[b][:, c * 128:(c + 1) * 128], idt)
                o = ot[(t4 % 2) * CT + c]
                okey = "%s%sot%d_%d" % (S.prefix, nm, (t4 % 2) * CT + c, j)
                if norm_w is not None:
                    S.act((o[:, j, :], okey), p[:, 0:128], AF.Copy, scale=nw[:, c:c + 1])
                elif gi % 2 == 0:
                    S.op("vector", lambda e, o=o, j=j, p=p: e.tensor_copy(o[:, j, :], p[:, 0:128]), reads=[p], writes=[okey])
                else:
                    S.op("scalar", lambda e, o=o, j=j, p=p: e.copy(o[:, j, :], p[:, 0:128]), reads=[p], writes=[okey])
                gi += 1
        for c in range(CT):
            o = ot[(t4 % 2) * CT + c]
            keys = ["%s%sot%d_%d" % (S.prefix, nm, (t4 % 2) * CT + c, j) for j in range(4)]
            wk = "%s_w%d_%d" % (tag, t4, c)
            S.dma(dstT[c * 128:(c + 1) * 128, t4 * 512:(t4 + 1) * 512], o[:].rearrange("p j t -> p (j t)"), queue="gpsimd", reads=keys, writes=[wk])
            for key in keys:
                S.readers.setdefault(key, []).append(S.last_w[wk])


def emit_untranspose(S, srcT, dst, L, C, idt, tag="ut", odt=None, evac=None):
    CT = C // 128
    xin = [S.sb([128, CT, 128], F32, "u_xin%d" % i) for i in range(2)]
    ot = [S.sb([128, C], odt or F32, "u_ot%d" % i) for i in range(2)]
    pt = [S.ps([128, 512], F32, "u_pt%d" % i) for i in range(2)]
    sv = srcT.rearrange("(k p) t -> p k t", p=128)
    gi = 0
    for t in range(L // 128):
        b = t % 2
        S.dma(xin[b][:], sv[:, :, t * 128:(t + 1) * 128], queue="sync")
        keys = []
        for c in range(CT):
            p = pt[gi % 2]
            S.tr(p[:, 0:128], xin[b][:, c, :], idt)
            key = "%sot%d_%d" % (S.prefix, b, c)
            keys.append(key)
            if gi % 2 == 0 and evac != "scalar":
                S.op("vector", lambda e, b=b, c=c, p=p: e.tensor_copy(ot[b][:, c * 128:(c + 1) * 128], p[:, 0:128]), reads=[p], writes=[key])
            else:
                S.op("scalar", lambda e, b=b, c=c, p=p: e.copy(ot[b][:, c * 128:(c + 1) * 128], p[:, 0:128]), reads=[p], writes=[key])
            gi += 1
        wk = "%s_w%d" % (tag, t)
        S.dma(dst[t * 128:(t + 1) * 128, :], ot[b][:], queue="gpsimd", reads=keys, writes=[wk])
        for key in keys:
            S.readers.setdefault(key, []).append(S.last_w[wk])


def emit_finalnorm(S, srcT, fw, out, L, idt, eps=1e-6):
    CT = 8
    xin = [S.sb([128, CT, 128], F32, "xin%d" % i) for i in range(2)]
    sq = [S.sb([128, CT, 128], F32, "sq%d" % i) for i in range(2)]
    ot = [S.sb([128, 1024], F32, "ot%d" % i) for i in range(2)]
    rstd = [S.sb([128, 1], F32, "rstd%d" % i) for i in range(2)]
    ones = S.sb([128, 1], F32, "ones"); S.memset(ones[:], 1.0)
    FW = S.sb([128, 1024], F32, "FW")
    S.dma(FW[:], fw.partition_broadcast(128))
    ssp = S.ps([128, 1], F32, "ssp")
    pt = [S.ps([128, 512], F32, "pt%d" % i) for i in range(2)]
    sv = srcT.rearrange("(k p) t -> p k t", p=128)
    gi = 0
    for t in range(L // 128):
        b = t % 2
        S.dma(xin[b][:], sv[:, :, t * 128:(t + 1) * 128], queue="sync")
        S.act(sq[b][:], xin[b][:], AF.Square)
        for k in range(CT):
            S.mm(ssp, sq[b][:, k, :], ones[:], start=(k == 0), stop=(k == CT - 1))
        S.ts(rstd[b][:], ssp, 1.0 / 1024, eps, ALU.mult, ALU.add)
        S.act(rstd[b][:], rstd[b][:], AF.Sqrt)
        S.op("vector", lambda e, b=b: e.reciprocal(rstd[b][:], rstd[b][:]), reads=[rstd[b][:]], writes=[rstd[b][:]])
        keys = []
        for c in range(CT):
            p = pt[gi % 2]
            S.tr(p[:, 0:128], xin[b][:, c, :], idt)
            key = "%sot%d_%d" % (S.prefix, b, c)
            keys.append(key)
            S.op("vector", lambda e, b=b, c=c, p=p: e.scalar_tensor_tensor(ot[b][:, c * 128:(c + 1) * 128], p[:, 0:128], rstd[b][:], FW[:, c * 128:(c + 1) * 128], ALU.mult, ALU.mult),
                 reads=[p, rstd[b][:], FW[:]], writes=[key])
            gi += 1
        wk = "fin_w%d" % t
        S.dma(out[t * 128:(t + 1) * 128, :], ot[b][:], queue="gpsimd", reads=keys, writes=[wk])
        for key in keys:
            S.readers.setdefault(key, []).append(S.last_w[wk])


def emit_attn(S, dk, dv, L, NH, plan, nmask, q, qp, kpc, kppc, tabs_d, v, masks, idt, y, tag="at"):
    NT = L // 128
    NG = L // 512
    tabs = {}
    for i, n in enumerate(("cq", "sq", "ck", "sk")):
        t = S.sb([dk, L], F32, "tab_" + n)
        S.dma(t[:], tabs_d[i], queue="sync")
        tabs[n] = t
    mk = None
    if nmask:
        mk32 = S.sb([128, 512], F32, "mk32")
        mk = S.sb([128, nmask, 512], BF16, "mk")
        for i in range(nmask):
            S.dma(mk32[:], masks[i])
            S.copy((mk[:, i, :], "mk%d" % i), mk32[:], eng="vector")
    st = [S.sb([dk, L], F32, "st%d" % i) for i in range(2)]
    qr = [S.sb([dk, L], BF16, "qr%d" % h) for h in range(NH)]
    kr = [S.sb([dk, L], BF16, "kr%d" % h) for h in range(NH)]
    va = [S.sb([128, NT, dv + 1], BF16, "va%d" % h) for h in range(NH)]
    v32_ = S.sb([128, NT, dv], F32, "v32")

    def prologue(h):
        for (pcs, pcsp, c, s_, dst) in ((q[h], qp[h], "cq", "sq", qr[h]), (kpc[h], kppc[h], "ck", "sk", kr[h])):
            for (lo, hi, a_) in pcs:
                S.dma(st[0][lo:hi, :], a_, queue="sync", reads=[a_], writes=[st[0][:]])
            for (lo, hi, a_) in pcsp:
                S.dma(st[1][lo:hi, :], a_, queue="gpsimd", reads=[a_], writes=[st[1][:]])
            S.tt(st[0][:], st[0][:], tabs[c][:], ALU.mult, eng="vector")
            S.tt(st[1][:], st[1][:], tabs[s_][:], ALU.mult, eng="gpsimd")
            S.tt(dst[:], st[0][:], st[1][:], ALU.add, eng="vector")
        S.dma(v32_[:], v[h].rearrange("(t p) d -> p t d", p=128), queue="sync")
        S.memset(va[h][:], 1.0, eng="gpsimd")
        S.copy(va[h][:, :, 0:dv], v32_[:], eng="vector")

    prologue(0)
    NPS = 4
    pss = [S.ps([128, 512], F32, "pss%d" % i) for i in range(NPS)]
    pso = [S.ps([dv + 1, 512], F32, "pso%d" % i) for i in range(2)]
    pst = [S.ps([128, dv + 1], F32, "pst%d" % i) for i in range(2)]
    NPT = 4
    pt = [S.sb([128, 512], BF16, "pt%d" % i) for i in range(NPT)]
    ptm = [S.sb([128, 512], BF16, "ptm%d" % i) for i in range(NPT)]
    osb = [S.sb([dv + 1, 512], F32, "osb%d" % i) for i in range(2)]
    rec = [S.sb([128, 1], F32, "rec%d" % i) for i in range(2)]
    yo = [S.sb([128, 4, NH * dv], F32, "yo%d" % i) for i in range(2)]
    iters = [(g, h, n, kt, mid, len(plan[g])) for h in range(NH) for g in range(NG) for n, (kt, mid) in enumerate(plan[g])]
    LOOK = 3

    def emit_qk(i):
        g, h, n, kt, mid, ln = iters[i]
        S.mm(pss[i % NPS], kr[h][:, kt * 128:(kt + 1) * 128], qr[h][:, g * 512:(g + 1) * 512])

    for i in range(min(LOOK, len(iters))):
        emit_qk(i)
    gi = 0
    prepared = 1
    for it, (g, h, n, kt, mid, ln) in enumerate(iters):
        if prepared < NH and h == prepared - 1 and g == 0 and n == ln - 1:
            prologue(prepared)
            prepared += 1
        if it + LOOK < len(iters):
            assert iters[it + LOOK][1] < prepared
            emit_qk(it + LOOK)
        po = pso[gi % 2]
        p_s = pss[it % NPS]
        e = pt[it % NPT]
        S.act(e[:], p_s, AF.Exp)
        if mid is not None:
            em = ptm[it % NPT]
            S.tt(em[:], e[:], (mk[:, mid, :], "mk%d" % mid), ALU.mult,
                 eng="vector" if it % 3 != 2 else "gpsimd")
            e = em
        S.mm(po, va[h][:, kt, :], e[:], start=(n == 0), stop=(n == ln - 1))
        if n != ln - 1:
            continue
        ob = osb[gi % 2]
        S.copy(ob[:], po, eng="vector")
        for j in range(4):
            tp = pst[j % 2]
            S.tr(tp, ob[:, j * 128:(j + 1) * 128], idt[0:dv + 1, 0:dv + 1])
            rc = rec[j % 2]
            S.op("vector", lambda e_, rc=rc, tp=tp: e_.reciprocal(rc[:], tp[:, dv:dv + 1]),
                 reads=[tp], writes=[rc[:]])
            S.ts((yo[g % 2][:, j, h * dv:(h + 1) * dv], "%syo%d_%d_%d" % (S.prefix, g % 2, j, h)), tp[:, 0:dv], rc[:], None, ALU.mult)
        gi += 1
        keys = ["%syo%d_%d_%d" % (S.prefix, g % 2, j, h) for j in range(4)]
        wk = "%s_y%d_%d" % (tag, g, h)
        S.dma(y[g * 512:(g + 1) * 512, h * dv:(h + 1) * dv].rearrange("(j p) d -> p j d", p=128), yo[g % 2][:, :, h * dv:(h + 1) * dv], queue="sync", reads=keys, writes=[wk])
        for key in keys:
            S.readers.setdefault(key, []).append(S.last_w[wk])


def emit_ret(S, L, NH, q, k, kt_, v, g, ck, sk, ct, st_, dtot, cols, y, gn_eps=1e-5):
    NT = L // 128
    CK = S.sb([64, L], F32, "CK"); SK = S.sb([64, L], F32, "SK")
    CT = S.sb([128, NT, 64], F32, "CT"); STt = S.sb([128, NT, 64], F32, "STt")
    S.dma(CK[:], ck); S.dma(SK[:], sk, queue="gpsimd")
    tokv = lambda a: a.rearrange("(t p) d -> p t d", p=128)
    S.dma(CT[:], tokv(ct)); S.dma(STt[:], tokv(st_), queue="gpsimd")
    COL = S.sb([128, NH * 6], F32, "COL"); S.dma(COL[:], cols)
    DT = S.sb([128, NH, 128], F32, "DT")
    for h in range(NH):
        S.dma(DT[:, h, :], dtot[h])
    st0 = S.sb([64, L], F32, "st0"); st1 = S.sb([64, L], F32, "st1")
    qr = S.sb([64, L], BF16, "qr"); kr = S.sb([64, L], BF16, "kr")
    krt = S.sb([128, NT, 64], BF16, "krt")
    scrA = S.sb([128, NT * 128], F32, "scrA"); scrB = S.sb([128, NT * 128], F32, "scrB")
    vbf = S.sb([128, NT, 64], BF16, "vbf")
    vz = S.sb([128, NT, 128], BF16, "vz")
    S32 = S.sb([64, NT + 1, 128], F32, "S32")
    Sst = S.sb([64, NT, 128], BF16, "Sst")
    osb = S.sb([128, NT, NH, 64], F32, "osb")
    pST = [S.ps([128, 128], F32, "pST%d" % i) for i in range(2)]
    pIn = [S.ps([128, 64], F32, "pIn%d" % i) for i in range(2)]
    pX = [S.ps([128, 128], F32, "pX%d" % i) for i in range(2)]
    pU = [S.ps([64, 128], F32, "pU%d" % i) for i in range(2)]
    PT = [S.sb([128, 128], BF16, "PT%d" % i) for i in range(3)]
    tb = [S.sb([128, 64], F32, "tb%d" % i) for i in range(3)]
    P = S.prefix
    for h in range(NH):
        c6 = lambda i: COL[:, h * 6 + i:h * 6 + i + 1]
        for (src, dst) in ((q, qr), (k, kr)):
            S.dma(st0[:], src[h])
            S.dma(st1[0:32, :], src[h][32:64], queue="gpsimd", reads=[src[h]], writes=[st1[:]])
            S.dma(st1[32:64, :], src[h][0:32], queue="gpsimd", reads=[src[h]], writes=[st1[:]])
            S.tt(st0[:], st0[:], CK[:], ALU.mult, eng="vector")
            S.tt(st1[:], st1[:], SK[:], ALU.mult, eng="gpsimd")
            S.tt(dst[:], st0[:], st1[:], ALU.add, eng="vector")
        A3 = scrA[:, 0:NT * 64].rearrange("p (t d) -> p t d", d=64)
        A3b = scrA[:, NT * 64:NT * 128].rearrange("p (t d) -> p t d", d=64)
        S.dma(A3, tokv(kt_[h]), reads=[kt_[h]], writes=[scrA[:]])
        S.dma(A3b[:, :, 0:32], tokv(kt_[h])[:, :, 32:64], queue="gpsimd", reads=[kt_[h]], writes=[P + "scrA_b"])
        S.dma(A3b[:, :, 32:64], tokv(kt_[h])[:, :, 0:32], queue="gpsimd", reads=[kt_[h]], writes=[P + "scrA_b"])
        S.op("vector", lambda e, A3=A3: e.tensor_tensor(A3, A3, CT[:], ALU.mult), reads=[scrA[:], CT[:]], writes=[scrA[:]])
        S.op("gpsimd", lambda e, A3b=A3b: e.tensor_tensor(A3b, A3b, STt[:], ALU.mult), reads=[P + "scrA_b", STt[:]], writes=[P + "scrA_b"])
        S.op("vector", lambda e, A3=A3, A3b=A3b: e.tensor_tensor(krt[:], A3, A3b, ALU.add), reads=[scrA[:], P + "scrA_b"], writes=[krt[:]])
        B3 = scrB[:, 0:NT * 64].rearrange("p (t d) -> p t d", d=64)
        S.dma(B3, tokv(v[h]), reads=[v[h]], writes=[scrB[:]])
        S.op("vector", lambda e, B3=B3: e.tensor_copy(vbf[:], B3), reads=[scrB[:]], writes=[vbf[:]])
        S.op("vector", lambda e, B3=B3, h=h: e.tensor_scalar(vz[:, :, 0:64], B3, COL[:, h * 6 + 2:h * 6 + 3], None, ALU.mult), reads=[scrB[:], COL[:]], writes=[P + "vz_f"])
        S.op("gpsimd", lambda e, B3=B3, h=h: e.tensor_scalar(vz[:, :, 64:128], B3, COL[:, h * 6 + 3:h * 6 + 4], None, ALU.mult), reads=[scrB[:], COL[:]], writes=[P + "vz_b"])
        allS = [S32[:]] + [P + "S32f%d" % c for c in range(NT)] + [P + "S32b%d" % c for c in range(NT)]
        S.op("gpsimd", lambda e: e.memset(S32[:], 0.0), reads=[], writes=allS)
        for c in range(NT):
            pu = pU[c % 2]
            S.op("tensor", lambda e, pu=pu, c=c: e.matmul(pu, krt[:, c, :], vz[:, c, :], start=True, stop=True),
                 reads=[krt[:], P + "vz_f", P + "vz_b"], writes=[pu])
            if c + 1 <= NT - 1:
                S.op("scalar", lambda e, c=c, pu=pu: e.copy(S32[:, c + 1, 0:64], pu[:, 0:64]), reads=[pu, S32[:]], writes=[P + "S32f%d" % (c + 1)])
            if c >= 1:
                S.op("vector", lambda e, c=c, pu=pu: e.tensor_copy(S32[:, c - 1, 64:128], pu[:, 64:128]), reads=[pu, S32[:]], writes=[P + "S32b%d" % (c - 1)])
        for c in range(1, NT):
            S.op("vector", lambda e, c=c, h=h: e.scalar_tensor_tensor(S32[:, c, 0:64], S32[:, c - 1, 0:64], COL[0:64, h * 6 + 4:h * 6 + 5], S32[:, c, 0:64], ALU.mult, ALU.add),
                 reads=[P + "S32f%d" % (c - 1), P + "S32f%d" % c, S32[:]], writes=[P + "S32f%d" % c])
        for c in range(NT - 2, -1, -1):
            S.op("vector", lambda e, c=c, h=h: e.scalar_tensor_tensor(S32[:, c, 64:128], S32[:, c + 1, 64:128], COL[0:64, h * 6 + 5:h * 6 + 6], S32[:, c, 64:128], ALU.mult, ALU.add),
                 reads=[P + "S32b%d" % (c + 1), P + "S32b%d" % c, S32[:]], writes=[P + "S32b%d" % c])
        S.op("vector", lambda e: e.tensor_copy(Sst[:], S32[:, 0:NT, :]), reads=allS, writes=[Sst[:]])
        def front_r(c):
            cs = slice(c * 128, (c + 1) * 128)
            S.mm(pST[c % 2], kr[:, cs], qr[:, cs])
            S.mm(pX[c % 2], qr[:, cs], Sst[:, c, :])

        front_r(0)
        for c in range(NT):
            if c + 1 < NT:
                front_r(c + 1)
            ps_ = pST[c % 2]
            pt = PT[c % 3]
            S.tt(pt[:], ps_, DT[:, h, :], ALU.mult, eng="vector")
            pin = pIn[c % 2]
            S.mm(pin, pt[:], vbf[:, c, :])
            px = pX[c % 2]
            t = tb[c % 3]
            S.act(t[:], px[:, 0:64], AF.Copy, scale=c6(0))
            S.stt(t[:], px[:, 64:128], c6(1), t[:], ALU.mult, ALU.add, eng="vector")
            S.op("vector", lambda e, c=c, h=h, pin=pin, t=t: e.tensor_tensor(osb[:, c, h, :], pin, t[:], ALU.add),
                 reads=[pin, t[:]], writes=[P + "osb%d_%d" % (c, h)])
    G = NT * NH
    allosb = [P + "osb%d_%d" % (c, h) for c in range(NT) for h in range(NH)]
    o3 = osb[:].rearrange("p t h d -> p (t h) d")
    o2 = osb[:].rearrange("p t h d -> p (t h d)")
    s1 = S.sb([128, G], F32, "s1"); s2 = S.sb([128, G], F32, "s2"); mu = S.sb([128, G], F32, "mu")
    rs = S.sb([128, G], F32, "rs"); nb = S.sb([128, G], F32, "nb")
    W_ = NH * 64
    for hh in range(0, NT * W_, 4096):
        n_ = min(4096, NT * W_ - hh)
        g0, g1 = hh // 64, (hh + n_) // 64
        S.op("scalar", lambda e, hh=hh, n_=n_: e.activation(scrA[:, 0:n_], o2[:, hh:hh + n_], AF.Square), reads=allosb, writes=[scrA[:], P + "scrA_b"])
        S.op("vector", lambda e, n_=n_, g0=g0, g1=g1: e.reduce_sum(s2[:, g0:g1], scrA[:, 0:n_].rearrange("p (g d) -> p g d", d=64), AX.X), reads=[scrA[:]], writes=[s2[:]])
    S.op("vector", lambda e: e.reduce_sum(s1[:], o3, AX.X), reads=allosb, writes=[s1[:]])
    S.ts(mu[:], s1[:], 1.0 / 64, None, ALU.mult)
    S.tt(s1[:], mu[:], mu[:], ALU.mult)
    S.stt(rs[:], s2[:], 1.0 / 64, s1[:], ALU.mult, ALU.subtract)
    S.ts(rs[:], rs[:], gn_eps, None, ALU.add)
    S.act(rs[:], rs[:], AF.Sqrt)
    S.op("vector", lambda e: e.reciprocal(rs[:], rs[:]), reads=[rs[:]], writes=[rs[:]])
    S.stt(nb[:], mu[:], -1.0, rs[:], ALU.mult, ALU.mult)
    for gi in range(G):
        S.op("scalar", lambda e, gi=gi: e.activation(o3[:, gi, :], o3[:, gi, :], AF.Identity, bias=nb[:, gi:gi + 1], scale=rs[:, gi:gi + 1]),
             reads=[allosb[gi], nb[:], rs[:]], writes=[allosb[gi]])
    gv = g.rearrange("(t p) d -> p t d", p=128)
    o4 = osb[:].rearrange("p t h d -> p t (h d)")
    TC = 4096 // W_
    for t0 in range(0, NT, TC):
        sB = scrB[:, 0:TC * W_].rearrange("p (t d) -> p t d", d=W_)
        S.dma(sB, gv[:, t0:t0 + TC, :], reads=[g], writes=[scrB[:]])
        S.act(scrB[:, 0:TC * W_], scrB[:, 0:TC * W_], AF.Silu)
        S.op("vector", lambda e, t0=t0, sB=sB: e.tensor_tensor(o4[:, t0:t0 + TC, :], o4[:, t0:t0 + TC, :], sB, ALU.mult),
             reads=allosb + [scrB[:]], writes=[P + "osbfin%d" % t0])
    S.dma(y.rearrange("(t p) d -> p t d", p=128), o4, reads=[P + "osbfin%d" % t0 for t0 in range(0, NT, TC)], writes=[P + "yout"])


def emit_ssd(S, L, xr, br, cr, z, dtr, cw, cb, dtb, alog, dsk, consts, y):
    NT = L // 128
    P = S.prefix
    CN = S.sb([128, 5, 128], F32, "CN")
    for i in range(5):
        S.dma(CN[:, i, :], consts[i])
    triF, triB, mF, mB, idt = (CN[:, i, :] for i in range(5))
    ones = S.sb([128, 128], F32, "ones"); S.memset(ones[:], 1.0)
    CW = S.sb([128, 3, 5], F32, "CW"); CB = S.sb([128, 3], F32, "CB"); DSK = S.sb([128, 2], F32, "DSK")
    for i in range(3):
        S.dma(CW[:, i, :], cw[i]); S.dma(CB[:, i:i + 1], cb[i])
    S.dma(DSK[:], dsk)
    xin = S.sb([128, L + 4], F32, "xin")
    acc = S.sb([128, L], F32, "acc")
    XS = S.sb([128, L], F32, "XS")
    Bf = S.sb([128, L], F32, "Bf")
    BT = S.sb([128, L], BF16, "BT"); CT = S.sb([128, L], BF16, "CT")
    for i, src in enumerate((xr, br, cr)):
        S.op("gpsimd", lambda e: e.memset(xin[:, 0:2], 0.0), reads=[], writes=[xin[:]])
        S.op("gpsimd", lambda e: e.memset(xin[:, L + 2:L + 4], 0.0), reads=[], writes=[xin[:]])
        S.dma(xin[:, 2:L + 2], src, reads=[src], writes=[xin[:]])
        S.ts(acc[:], xin[:, 0:L], CW[:, i, 0:1], None, ALU.mult)
        for w in range(1, 5):
            S.stt(acc[:], xin[:, w:w + L], CW[:, i, w:w + 1], acc[:], ALU.mult, ALU.add)
        if i == 0:
            S.act(XS[:], acc[:], AF.Silu, bias=CB[:, 0:1])
        elif i == 1:
            S.act(Bf[:], acc[:], AF.Silu, bias=CB[:, 1:2])
            S.copy(BT[:], Bf[:], eng="gpsimd")
        else:
            S.act(CT[:], acc[:], AF.Silu, bias=CB[:, 2:3])
    DT = S.sb([128, NT, 4], F32, "DT"); T1 = S.sb([128, NT, 4], F32, "T1"); DTA = S.sb([128, NT, 4], F32, "DTA")
    tokv = lambda a: a.rearrange("(t p) d -> p t d", p=128)
    S.dma(DT[:], tokv(dtr), allow_slow_non_contiguous=True); S.dma(T1[:], tokv(dtb))
    S.tt(DT[:], DT[:], T1[:], ALU.add)
    S.act(DT[:], DT[:], AF.Exp)
    S.act(DT[:], DT[:], AF.Ln, bias=1.0)
    S.dma(T1[:], tokv(alog))
    S.act(T1[:], T1[:], AF.Exp)
    S.stt(DTA[:], T1[:], -1.0, DT[:], ALU.mult, ALU.mult)
    dta2 = DTA[:].rearrange("p t d -> p (t d)")
    pA = S.ps([128, 512], F32, "pA"); pB = S.ps([128, 512], F32, "pB"); pC = S.ps([128, 512], F32, "pC")
    CS = S.sb([128, NT, 4], F32, "CS"); TOT = S.sb([128, NT, 4], F32, "TOT")
    S.mm(pA[:, 0:NT * 4], triF, dta2)
    S.mm(pB[:, 0:NT * 4], triB, dta2)
    S.mm(pC[:, 0:NT * 4], ones[:], dta2)
    pA3 = pA[:, 0:NT * 4].rearrange("p (t d) -> p t d", d=4); pB3 = pB[:, 0:NT * 4].rearrange("p (t d) -> p t d", d=4)
    S.op("vector", lambda e: e.tensor_copy(CS[:, :, 0:2], pA3[:, :, 0:2]), reads=[pA], writes=[P + "CSf"])
    S.op("vector", lambda e: e.tensor_copy(CS[:, :, 2:4], pB3[:, :, 2:4]), reads=[pB], writes=[P + "CSb"])
    S.copy(TOT[:].rearrange("p t d -> p (t d)"), pC[:, 0:NT * 4], eng="vector")
    TE = S.sb([128, NT, 4], F32, "TE"); ECS = S.sb([128, NT, 4], F32, "ECS"); ED = S.sb([128, NT, 4], F32, "ED")
    S.op("vector", lambda e: e.tensor_tensor(TE[:], TOT[:], CS[:], ALU.subtract), reads=[TOT[:], P + "CSf", P + "CSb"], writes=[TE[:]])
    S.act(TE[:], TE[:], AF.Exp)
    S.op("scalar", lambda e: e.activation(ECS[:], CS[:], AF.Exp), reads=[P + "CSf", P + "CSb"], writes=[ECS[:]])
    S.act(ED[:], TOT[:], AF.Exp)
    XTK = S.sb([128, NT, 128], F32, "XTK")
    BTK = S.sb([128, NT, 128], BF16, "BTK")
    XDT = S.sb([128, NT, 256], BF16, "XDT")
    XTE = S.sb([128, NT, 256], BF16, "XTE")
    pT = [S.ps([128, 512], F32, "pT%d" % i) for i in range(2)]
    for c in range(NT):
        cs_ = slice(c * 128, (c + 1) * 128)
        p = pT[c % 2]
        S.tr(p[:, 0:128], XS[:, cs_], idt)
        S.tr(p[:, 128:256], Bf[:, cs_], idt)
        S.op("vector", lambda e, c=c, p=p: e.tensor_copy(XTK[:, c, :], p[:, 0:128]), reads=[p], writes=[P + "XTK%d" % c])
        S.op("vector", lambda e, c=c, p=p: e.tensor_copy(BTK[:, c, :], p[:, 128:256]), reads=[p], writes=[P + "BTK%d" % c])
        for s4 in range(4):
            h = s4 % 2
            S.op("gpsimd", lambda e, c=c, s4=s4, h=h: e.tensor_scalar(XDT[:, c, s4 * 64:(s4 + 1) * 64], XTK[:, c, h * 64:(h + 1) * 64], DT[:, c, s4:s4 + 1], None, ALU.mult),
                 reads=[P + "XTK%d" % c, DT[:]], writes=[P + "XDT%d_%d" % (c, s4)])
            S.op("gpsimd", lambda e, c=c, s4=s4: e.tensor_scalar(XTE[:, c, s4 * 64:(s4 + 1) * 64], XDT[:, c, s4 * 64:(s4 + 1) * 64], TE[:, c, s4:s4 + 1], None, ALU.mult),
                 reads=[P + "XDT%d_%d" % (c, s4), TE[:]], writes=[P + "XTE%d_%d" % (c, s4)])
    P32 = S.sb([128, NT, 256], F32, "P32")
    PBF = S.sb([128, NT, 256], BF16, "PBF")
    S.memset(P32[:], 0.0, eng="gpsimd")
    for c in range(NT):
        p = pT[c % 2]
        S.op("tensor", lambda e, c=c, p=p: e.matmul(p[:, 0:256], BTK[:, c, :], XTE[:, c, :], start=True, stop=True),
             reads=[P + "BTK%d" % c] + [P + "XTE%d_%d" % (c, s4) for s4 in range(4)], writes=[p])
        if c + 1 < NT:
            S.op("vector", lambda e, c=c, p=p: e.tensor_copy(P32[:, c + 1, 0:128], p[:, 0:128]), reads=[p, P32[:]], writes=[P + "Pf%d" % (c + 1)])
        if c >= 1:
            S.op("vector", lambda e, c=c, p=p: e.tensor_copy(P32[:, c - 1, 128:256], p[:, 128:256]), reads=[p, P32[:]], writes=[P + "Pb%d" % (c - 1)])
    for c in range(1, NT):
        for h in range(2):
            sl = slice(h * 64, (h + 1) * 64)
            S.op("vector", lambda e, c=c, h=h, sl=sl: e.scalar_tensor_tensor(P32[:, c, sl], P32[:, c - 1, sl], ED[:, c - 1, h:h + 1], P32[:, c, sl], ALU.mult, ALU.add),
                 reads=[P + "Pf%d" % (c - 1), P + "Pf%d" % c, P32[:], ED[:]], writes=[P + "Pf%d" % c])
    for c in range(NT - 2, -1, -1):
        for h in range(2):
            sl = slice(128 + h * 64, 128 + (h + 1) * 64)
            S.op("vector", lambda e, c=c, h=h, sl=sl: e.scalar_tensor_tensor(P32[:, c, sl], P32[:, c + 1, sl], ED[:, c + 1, 2 + h:3 + h], P32[:, c, sl], ALU.mult, ALU.add),
                 reads=[P + "Pb%d" % (c + 1), P + "Pb%d" % c, P32[:], ED[:]], writes=[P + "Pb%d" % c])
    S.op("gpsimd", lambda e: e.tensor_copy(PBF[:], P32[:]), reads=[P32[:]] + [P + "Pf%d" % c for c in range(NT)] + [P + "Pb%d" % c for c in range(NT)], writes=[PBF[:]])
    ZS = xin[:, 0:L].rearrange("p (t d) -> p t d", d=128)
    S.dma(ZS, tokv(z), reads=[z], writes=[xin[:]]); S.act(ZS, ZS, AF.Silu)
    YO = acc[:].rearrange("p (t d) -> p t d", d=128)
    pG = pA; pBc = [S.ps([128, 512], F32, "pBc%d" % i) for i in range(2)]
    pYd = pB; pYo = pC
    GS = [S.sb([128, 128], F32, "GS%d" % i) for i in range(2)]
    REP = [S.sb([128, 128], F32, "REP%d" % i) for i in range(8)]
    SEG = [S.sb([128, 512], F32, "SEG%d" % i) for i in range(2)]
    MT = [S.sb([128, 512], BF16, "MT%d" % i) for i in range(2)]
    YD = [S.sb([128, 256], F32, "YD%d" % i) for i in range(2)]
    AC = [S.sb([128, 256], F32, "AC%d" % i) for i in range(2)]
    MK4 = S.sb([128, 512], BF16, "MK4")
    for s4 in range(4):
        S.op("vector", lambda e, s4=s4: e.tensor_copy(MK4[:, s4 * 128:(s4 + 1) * 128], mF if s4 < 2 else mB), reads=[CN[:]], writes=[MK4[:]])

    def front(c):
        cs_ = slice(c * 128, (c + 1) * 128)
        S.mm(pG[:, 0:128], BT[:, cs_], CT[:, cs_])
        S.copy(GS[c % 2][:], pG[:, 0:128], eng="scalar")
        pb = pBc[c % 2]
        for s4 in range(4):
            rep = REP[(c % 2) * 4 + s4]
            S.act(rep[:], ones[:], AF.Copy, scale=DTA[:, c, s4:s4 + 1])
            S.mm(pb[:, s4 * 128:(s4 + 1) * 128], rep[:], triF if s4 < 2 else triB)

    def back(c):
        cs_ = slice(c * 128, (c + 1) * 128)
        pb = pBc[c % 2]; seg = SEG[c % 2]; mt = MT[c % 2]; gs = GS[c % 2]
        for s4 in range(4):
            key = P + ("CSf" if s4 < 2 else "CSb")
            S.op("vector", lambda e, seg=seg, pb=pb, c=c, s4=s4: e.tensor_scalar(seg[:, s4 * 128:(s4 + 1) * 128], pb[:, s4 * 128:(s4 + 1) * 128], CS[:, c, s4:s4 + 1], 0.0, ALU.subtract, ALU.min),
                 reads=[pb, key], writes=[seg[:]])
        S.act(seg[:], seg[:], AF.Exp)
        S.tt(seg[:], seg[:], MK4[:], ALU.mult, eng="gpsimd")
        for s4 in range(4):
            S.op("vector" if s4 % 2 == 0 else "gpsimd", lambda e, seg=seg, mt=mt, gs=gs, s4=s4: e.tensor_tensor(mt[:, s4 * 128:(s4 + 1) * 128], seg[:, s4 * 128:(s4 + 1) * 128], gs[:], ALU.mult),
                 reads=[seg[:], gs[:]], writes=[mt[:]])
        for s4 in range(4):
            S.op("tensor", lambda e, mt=mt, c=c, s4=s4: e.matmul(pYd[:, s4 * 64:(s4 + 1) * 64], mt[:, s4 * 128:(s4 + 1) * 128], XDT[:, c, s4 * 64:(s4 + 1) * 64], start=True, stop=True),
                 reads=[mt[:], P + "XDT%d_%d" % (c, s4)], writes=[pYd])
        S.mm(pYo[:, 0:256], CT[:, cs_], PBF[:, c, :])
        yd, ac = YD[c % 2], AC[c % 2]
        S.copy(yd[:], pYd[:, 0:256], eng="scalar")
        for s4 in range(4):
            sl = slice(s4 * 64, (s4 + 1) * 64)
            S.op("vector", lambda e, c=c, s4=s4, sl=sl, ac=ac, yd=yd: e.scalar_tensor_tensor(ac[:, sl], pYo[:, sl], ECS[:, c, s4:s4 + 1], yd[:, sl], ALU.mult, ALU.add),
                 reads=[pYo, ECS[:], yd[:]], writes=[ac[:]])
        S.op("vector", lambda e, c=c, ac=ac: e.tensor_tensor(YO[:, c, :], ac[:, 0:128], ac[:, 128:256], ALU.add), reads=[ac[:], acc[:]], writes=[P + "YO%d" % c])
        for h in range(2):
            sl = slice(h * 64, (h + 1) * 64)
            S.op("vector", lambda e, c=c, h=h, sl=sl: e.scalar_tensor_tensor(YO[:, c, sl], XTK[:, c, sl], DSK[:, h:h + 1], YO[:, c, sl], ALU.mult, ALU.add),
                 reads=[P + "XTK%d" % c, P + "YO%d" % c, DSK[:]], writes=[P + "YO%d" % c])

    front(0)
    for c in range(NT):
        if c + 1 < NT:
            front(c + 1)
        back(c)
    S.op("vector", lambda e: e.tensor_tensor(YO, YO, ZS, ALU.mult), reads=[xin[:]] + [P + "YO%d" % c for c in range(NT)], writes=[acc[:]])
    S.dma(y.rearrange("(t p) d -> p t d", p=128), YO, reads=[acc[:]], writes=[P + "yout"])


def emit_topk(S, aff, vals, idxu, idt, L, NE=16, KSEL=512, mid_hook=None):
    NT = L // 128
    A = S.sb([128, NT, NE], F32, "A")
    S.dma(A[:], aff.rearrange("(t p) e -> p t e", p=128))
    wk = S.sb([NE, L], F32, "wk")
    vs = S.sb([NE, KSEL], F32, "vs")
    ix = S.sb([NE, KSEL], U32, "ix")
    pt = [S.ps([128, 512], F32, "pt%d" % i) for i in range(2)]
    P = S.prefix
    for t4 in range(NT // 4):
        p = pt[t4 % 2]
        for j in range(4):
            S.tr(p[0:NE, j * 128:(j + 1) * 128], A[:, t4 * 4 + j, :], idt)
        S.op("vector", lambda e, t4=t4, p=p: e.tensor_copy(wk[:, t4 * 512:(t4 + 1) * 512], p[0:NE, :]), reads=[p], writes=[P + "wk%d" % t4])
    allwk = [P + "wk%d" % t4 for t4 in range(NT // 4)]
    if mid_hook is not None:
        mid_hook()
    for it in range(KSEL // 8):
        sl = slice(it * 8, it * 8 + 8)
        S.op("vector", lambda e, sl=sl: e.max(vs[:, sl], wk[:]), reads=[wk[:]] + (allwk if it == 0 else []), writes=[P + "vs%d" % it])
        S.op("vector", lambda e, sl=sl: e.max_index(ix[:, sl], vs[:, sl], wk[:]), reads=[wk[:], P + "vs%d" % it], writes=[P + "ix%d" % it])
        S.op("vector", lambda e, sl=sl: e.match_replace(wk[:], vs[:, sl], wk[:], -1e30), reads=[wk[:], P + "vs%d" % it], writes=[wk[:]])
    S.dma(vals, vs[:], reads=[P + "vs%d" % i for i in range(KSEL // 8)], writes=[P + "vout"])
    S.dma(idxu, ix[:], queue="gpsimd", reads=[P + "ix%d" % i for i in range(KSEL // 8)], writes=[P + "iout"])


def emit_moe(S, x1k, vals, idxu, g2, wg, wu, wd, x2k, idt, L, NE=16, eps=1e-6):
    P = S.prefix
    NS = NE * 2
    G2 = S.sb([128, 8], F32, "G2")
    S.dma(G2[:], g2.rearrange("(k p) -> p k", p=128), allow_slow_non_contiguous=True)
    WG = [S.sb([128, 8, 1024], BF16, "WG%d" % i) for i in range(2)]
    WU = [S.sb([128, 8, 1024], BF16, "WU%d" % i) for i in range(2)]
    WD = [S.sb([128, 8, 1024], BF16, "WD%d" % i) for i in range(2)]
    XGT = [S.sb([128, 4, 1024], F32, "XGT%d" % i) for i in range(2)]
    IX = [S.sb([128, 4], U32, "IX%d" % i) for i in range(2)]
    GT = [S.sb([128, 4], F32, "GT%d" % i) for i in range(2)]
    SS = [S.sb([128, 4], F32, "SS%d" % i) for i in range(2)]
    SQ = S.sb([128, 1024], F32, "SQ")
    XNs = [S.sb([128, 8, 512], BF16, "XN%d" % i) for i in range(2)]
    HID = S.sb([128, 8, 512], BF16, "HID")
    ACC = S.sb([128, 4, 1024], F32, "ACC")
    GS = [S.sb([128, 512], F32, "GS%d" % i) for i in range(2)]
    OB = [S.sb([128, 1024], F32, "OB%d" % i) for i in range(2)]
    pt = [S.ps([128, 512], F32, "pt%d" % i) for i in range(2)]
    pg = [S.ps([128, 512], F32, "pg%d" % i) for i in range(2)]
    pu = [S.ps([128, 512], F32, "pu%d" % i) for i in range(2)]
    po = [S.ps([128, 512], F32, "po%d" % i) for i in range(2)]
    for r in range(L // 512):
        S.dma(x2k[r * 512:(r + 1) * 512, :], x1k[r * 512:(r + 1) * 512, :], queue="sync", writes=[P + "x2k"])

    def load_gu(s):
        e_, hf = divmod(s, 2)
        sl = s % 2
        fs = slice(hf * 1024, (hf + 1) * 1024)
        S.dma(WG[sl][:], wg[e_].rearrange("(k p) f -> p k f", p=128)[:, :, fs], queue="gpsimd")
        S.dma(WU[sl][:], wu[e_].rearrange("(k p) f -> p k f", p=128)[:, :, fs], queue="gpsimd")

    def load_d(s):
        e_, hf = divmod(s, 2)
        sl = s % 2
        fs = slice(hf * 1024, (hf + 1) * 1024)
        S.dma(WD[sl][:], wd[e_][fs, :].rearrange("(k p) d -> p k d", p=128), queue="gpsimd")

    def load_w(s):
        load_gu(s)
        load_d(s)

    def gather(e_):
        b = e_ % 2
        S.dma(IX[b][:], idxu[e_].rearrange("(j p) -> p j", p=128), queue="sync", allow_slow_non_contiguous=True)
        S.dma(GT[b][:], vals[e_].rearrange("(j p) -> p j", p=128), queue="sync", allow_slow_non_contiguous=True)
        for j in range(4):
            S.raw_dma("gpsimd", lambda e, b=b, j=j: e.indirect_dma_start(out=XGT[b][:, j, :], out_offset=None, in_=x1k,
                      in_offset=bass.IndirectOffsetOnAxis(ap=IX[b][:, j:j + 1], axis=0)),
                      reads=[IX[b][:], x1k], writes=[XGT[b][:]])

    def prep(e_):
        b = e_ % 2
        XN = XNs[b]
        for j in range(4):
            S.op("scalar", lambda e, b=b, j=j: e.activation(SQ[:], XGT[b][:, j, :], AF.Square, accum_out=SS[b][:, j:j + 1]),
                 reads=[XGT[b][:]], writes=[SQ[:], SS[b][:]])
        S.ts(SS[b][:], SS[b][:], 1.0 / 1024, eps, ALU.mult, ALU.add)
        S.act(SS[b][:], SS[b][:], AF.Sqrt)
        S.op("vector", lambda e, b=b: e.reciprocal(SS[b][:], SS[b][:]), reads=[SS[b][:]], writes=[SS[b][:]])
        for j in range(4):
            S.op("vector" if j % 2 == 0 else "gpsimd", lambda e, b=b, j=j: e.tensor_scalar(XGT[b][:, j, :], XGT[b][:, j, :], SS[b][:, j:j + 1], None, ALU.mult),
                 reads=[XGT[b][:], SS[b][:]], writes=[XGT[b][:]])
        for k in range(8):
            p = pt[k % 2]
            for j in range(4):
                S.tr(p[:, j * 128:(j + 1) * 128], XGT[b][:, j, k * 128:(k + 1) * 128], idt)
            if k % 2 == 0:
                S.op("scalar", lambda e, k=k, p=p, XN=XN: e.activation(XN[:, k, :], p, AF.Copy, scale=G2[:, k:k + 1]), reads=[p, G2[:]], writes=[XN[:]])
            else:
                S.op("vector", lambda e, k=k, p=p, XN=XN: e.tensor_scalar(XN[:, k, :], p, G2[:, k:k + 1], None, ALU.mult), reads=[p, G2[:]], writes=[XN[:]])

    load_w(0)
    load_w(1)
    gather(0)
    prep(0)
    for s in range(NS):
        e_, hf = divmod(s, 2)
        sl = s % 2
        b = e_ % 2
        XN = XNs[b]
        if hf == 0 and e_ + 1 < NE:
            gather(e_ + 1)
        for f in range(8):
            a, u = pg[f % 2], pu[f % 2]
            for k in range(8):
                S.op("tensor", lambda e, a=a, k=k, f=f, sl=sl, XN=XN: e.matmul(a, WG[sl][:, k, f * 128:(f + 1) * 128], XN[:, k, :], start=(k == 0), stop=(k == 7)),
                     reads=[WG[sl][:], XN[:]], writes=[a])
            for k in range(8):
                S.op("tensor", lambda e, u=u, k=k, f=f, sl=sl, XN=XN: e.matmul(u, WU[sl][:, k, f * 128:(f + 1) * 128], XN[:, k, :], start=(k == 0), stop=(k == 7)),
                     reads=[WU[sl][:], XN[:]], writes=[u])
            gs = GS[f % 2]
            S.act(gs[:], a, AF.Silu)
            S.op("vector", lambda e, f=f, u=u, gs=gs: e.tensor_tensor(HID[:, f, :], u, gs[:], ALU.mult), reads=[u, gs[:]], writes=[HID[:]])
        if s + 2 < NS:
            load_gu(s + 2)
        if hf == 1 and e_ + 1 < NE:
            prep(e_ + 1)
        for j in range(4):
            ob = OB[j % 2]
            for dh in range(2):
                p = po[dh]
                ds_ = slice(dh * 512, (dh + 1) * 512)
                for k in range(8):
                    S.op("tensor", lambda e, p=p, k=k, j=j, ds_=ds_, sl=sl: e.matmul(p, HID[:, k, j * 128:(j + 1) * 128], WD[sl][:, k, ds_], start=(k == 0), stop=(k == 7)),
                         reads=[HID[:], WD[sl][:]], writes=[p])
                if hf == 0:
                    if dh == 0:
                        S.op("scalar", lambda e, p=p, j=j, ds_=ds_: e.copy(ACC[:, j, ds_], p), reads=[p], writes=[ACC[:]])
                    else:
                        S.op("vector", lambda e, p=p, j=j, ds_=ds_: e.tensor_copy(ACC[:, j, ds_], p), reads=[p], writes=[ACC[:]])
                else:
                    S.op("vector", lambda e, p=p, j=j, ds_=ds_, ob=ob: e.tensor_tensor(ob[:, ds_], p, ACC[:, j, ds_], ALU.add), reads=[p, ACC[:]], writes=[ob[:]])
            if hf == 1:
                S.op("scalar", lambda e, ob=ob, j=j, b=b: e.activation(ob[:], ob[:], AF.Copy, scale=GT[b][:, j:j + 1]), reads=[ob[:], GT[b][:]], writes=[ob[:]])
                S.raw_dma("gpsimd", lambda e, ob=ob, j=j, b=b: e.indirect_dma_start(out=x2k, out_offset=bass.IndirectOffsetOnAxis(ap=IX[b][:, j:j + 1], axis=0),
                          in_=ob[:], in_offset=None, compute_op=ALU.add), reads=[ob[:], IX[b][:]], writes=[P + "x2k"])
        if s + 2 < NS:
            load_d(s + 2)


L_ = 4096
NF_, NT_ = 2208, 1288

def build_fused(depth=2, upto=99, debug=False):
    nc = bass.Bass("TRN2", target_bir_lowering=False)
    L = L_
    def din(n, s, dt=F32):
        return nc.dram_tensor(n, s, dt, kind="ExternalInput").ap()
    def scr(n, s, dt=F32):
        if debug:
            return nc.dram_tensor(n, s, dt, kind="ExternalOutput").ap()
        return nc.dram_tensor(n, s, dt, kind="Internal", addr_space="Local").ap()
    xT = din("xT", [1024, L])
    Wl = []
    for l in range(depth):
        d = {}
        for n, s in (("wF", [1024, NF_]), ("wT", [1024, NT_]), ("ln1", [1024]), ("wuq", [256, 384]), ("qn", [256]),
                     ("wkn", [128, 256]), ("wv", [128, 256]), ("kvn", [128]), ("cw", [2, 3, 128, 5]), ("cb", [2, 3, 128, 1]),
                     ("dtb", [2, L, 4]), ("alog", [2, L, 4]), ("dsk", [2, 128, 2]), ("snw", [256]), ("wout", [1024, 1024]),
                     ("ln2", [1024]), ("rw", [1024, 16]), ("wg", [16, 1024, 2048]), ("wu", [16, 1024, 2048]), ("wd", [16, 2048, 1024])):
            d[n] = din("%s%d" % (n, l), s)
        Wl.append(d)
    fnw = din("fnw", [1024]); ones1k = din("ones1k", [1024])
    C = {}
    for n, s in (("rck", [64, L]), ("rsk", [64, L]), ("rct", [L, 64]), ("rst", [L, 64]), ("rdtot", [4, 128, 128]), ("rcols", [2, 128, 12]),
                 ("mcq", [96, L]), ("msq", [96, L]), ("mck", [96, L]), ("msk", [96, L]),
                 ("dcq", [64, L]), ("dsq", [64, L]), ("dck", [64, L]), ("dsk_", [64, L]), ("dmask", [20, 128, 512]),
                 ("ident", [128, 128]), ("sconst", [5, 128, 128]), ("tokid", [128, 32]), ("iota512", [128, 512])):
        C[n] = din(n, s)
    out = nc.dram_tensor("out", [L, 1024], F32, kind="ExternalOutput").ap()
    PF = scr("PF", [NF_, L]); PT = scr("PT", [L, NT_]); QF = scr("QF", [384, L]); KF = scr("KF", [256, L]); VT = scr("VT", [L, 256])
    YA, YB, YC, YD = (scr(n, [L, 256]) for n in ("YA", "YB", "YC", "YD"))
    MT = scr("MT", [1024, L]); X1T = scr("X1T", [1024, L])
    AFF = scr("AFF", [L, 16]); VALS = scr("VALS", [16, 512]); IDXU = scr("IDXU", [16, 512], U32)
    X1K = scr("X1K", [L, 1024]); X2K = scr("X2K", [L, 1024])
    X2T = [scr("X2T%d" % l, [1024, L]) for l in range(depth)]
    with ExitStack() as es:
        S = Sched(nc, es)
        with S.scope("c_"):
            pass
        IDT = S.sb([128, 128], F32, "IDT"); S.dma(IDT[:], C["ident"])
        idt = IDT[:]
        stage = [0]
        def go():
            stage[0] += 1
            return stage[0] <= upto
        cur = xT
        for l in range(depth):
            W = Wl[l]; pf = "l%d" % l
            if go():
                with S.scope(pf + "a_"):
                    emit_lin(S, cur, W["wT"], W["ln1"], PT, 1024, L, NT_, tag=pf + "a", NB=3)
            if go():
                with S.scope(pf + "b_"):
                    emit_linF(S, cur, W["wF"], W["ln1"], PF, 1024, L, NF_, tag=pf + "b")
            if go():
                for c2 in range(2):
                    hs = [2 * c2, 2 * c2 + 1]
                    with S.scope(pf + "r%d_" % c2):
                        emit_ret(S, L, 2, q=[PF[h * 64:(h + 1) * 64] for h in hs], k=[PF[256 + h * 64:256 + (h + 1) * 64] for h in hs],
                                 kt_=[PT[:, h * 64:(h + 1) * 64] for h in hs],
                                 v=[PT[:, 256 + h * 64:256 + (h + 1) * 64] for h in hs], g=PT[:, 512 + c2 * 128:512 + (c2 + 1) * 128],
                                 ck=C["rck"], sk=C["rsk"], ct=C["rct"], st_=C["rst"], dtot=C["rdtot"][2 * c2:2 * c2 + 2], cols=C["rcols"][c2],
                                 y=YA[:, c2 * 128:(c2 + 1) * 128])
            if go():
                with S.scope(pf + "m1_"):
                    emit_linF(S, PF[512:768], W["wuq"], W["qn"], QF, 256, L, 384, tag=pf + "m1")
                with S.scope(pf + "m2_"):
                    emit_linF(S, PF[768:896], W["wkn"], W["kvn"], KF, 128, L, 256, tag=pf + "m2")
                with S.scope(pf + "m3_"):
                    emit_lin(S, PF[768:896], W["wv"], W["kvn"], VT, 128, L, 256, tag=pf + "m3")
            if go():
                for c2 in range(2):
                    hs = [2 * c2, 2 * c2 + 1]
                    with S.scope(pf + "ma%d_" % c2):
                        emit_attn(S, 96, 64, L, 2, mla_plan(), 0, q=[[(0, 96, QF[h * 96:(h + 1) * 96])] for h in hs],
                                  qp=[[(0, 64, QF[h * 96:h * 96 + 64]), (64, 80, QF[h * 96 + 80:h * 96 + 96]), (80, 96, QF[h * 96 + 64:h * 96 + 80])] for h in hs],
                                  kpc=[[(0, 64, KF[h * 64:(h + 1) * 64]), (64, 96, PF[896:928])] for h in hs],
                                  kppc=[[(0, 64, KF[h * 64:(h + 1) * 64]), (64, 80, PF[912:928]), (80, 96, PF[896:912])] for h in hs],
                                  tabs_d=[C["mcq"], C["msq"], C["mck"], C["msk"]], v=[VT[:, h * 64:(h + 1) * 64] for h in hs], masks=None, idt=idt,
                                  y=YB[:, c2 * 128:(c2 + 1) * 128], tag=pf + "ma%d" % c2)
            if go():
                for g_ in range(2):
                    with S.scope(pf + "s%d_" % g_):
                        emit_ssd(S, L, PF[928 + g_ * 128:928 + (g_ + 1) * 128], PF[1184 + g_ * 128:1184 + (g_ + 1) * 128], PF[1440 + g_ * 128:1440 + (g_ + 1) * 128],
                                 z=PT[:, 768 + g_ * 128:768 + (g_ + 1) * 128], dtr=PT[:, 1024 + g_ * 4:1024 + (g_ + 1) * 4], cw=W["cw"][g_], cb=W["cb"][g_],
                                 dtb=W["dtb"][g_], alog=W["alog"][g_], dsk=W["dsk"][g_], consts=C["sconst"], y=YC[:, g_ * 128:(g_ + 1) * 128])
            if go():
                for c2 in range(2):
                    hs = [2 * c2, 2 * c2 + 1]
                    with S.scope(pf + "d%d_" % c2):
                        dq_ = lambda h: PF[1696 + h * 64:1696 + (h + 1) * 64]
                        dk_ = lambda h: PF[1952 + h * 64:1952 + (h + 1) * 64]
                        sw_ = lambda a: [(0, 8, a[8:16]), (8, 16, a[0:8]), (16, 64, a[16:64])]
                        emit_attn(S, 64, 64, L, 2, dil_plan(), 20, q=[[(0, 64, dq_(h))] for h in hs], qp=[sw_(dq_(h)) for h in hs],
                                  kpc=[[(0, 64, dk_(h))] for h in hs], kppc=[sw_(dk_(h)) for h in hs],
                                  tabs_d=[C["dcq"], C["dsq"], C["dck"], C["dsk_"]], v=[PT[:, 1032 + h * 64:1032 + (h + 1) * 64] for h in hs], masks=C["dmask"], idt=idt,
                                  y=YD[:, c2 * 128:(c2 + 1) * 128], tag=pf + "d%d" % c2)
            if go():
                with S.scope(pf + "t_"):
                    for i, (src, nw) in enumerate(((YA, None), (YB, None), (YC, W["snw"]), (YD, None))):
                        emit_transpose(S, src, MT[i * 256:(i + 1) * 256], L, 256, idt, norm_w=nw, tag=pf + "t%d" % i, nm="m%d" % i)
            if go():
                with S.scope(pf + "o_"):
                    emit_linF(S, MT, W["wout"], ones1k, X1T, 1024, L, 1024, norm=False, resid=cur, tag=pf + "o")
            if go():
                with S.scope(pf + "q_"):
                    emit_lin(S, X1T, W["rw"], W["ln2"], AFF, 1024, L, 16, softmax=True, fp32=True, tag=pf + "q", NB=4)
                with S.scope(pf + "k_"):
                    emit_topk(S, AFF, VALS, IDXU, idt, L,
                              mid_hook=lambda: emit_untranspose(S, X1T, X1K, L, 1024, idt, tag=pf + "u", evac="scalar"))
            if go():
                with S.scope(pf + "e_"):
                    emit_moe(S, X1K, VALS, IDXU, W["ln2"], W["wg"], W["wu"], W["wd"], X2K, idt, L)
            if go():
                with S.scope(pf + "c_"):
                    emit_transpose(S, X2K, X2T[l], L, 1024, idt, tag=pf + "c")
            cur = X2T[l]
            S.new_epoch()
        if go():
            with S.scope("fin_"):
                emit_finalnorm(S, cur, fnw, out, L, idt)
        else:
            with S.scope("fin_"):
                z_ = S.sb([128, 1024], F32, "z_"); S.memset(z_[:], 0.0)
                S.dma(out[0:128, :], z_[:])
        S.finish()
    return nc


def _perm_ret():
    return np.array([h * 64 + ((d + 32) % 64) for h in range(4) for d in range(64)])

def _perm_dil():
    def sw(d):
        return d + 8 if d < 8 else (d - 8 if d < 16 else d)
    return np.array([h * 64 + sw(d) for h in range(4) for d in range(64)])

def host_consts():
    L = L_
    C = {}
    rc = ret_consts(L, [0, 1, 2, 3])
    C["rck"], C["rsk"], C["rct"], C["rst"], C["rdtot"] = rc["ck"], rc["sk"], rc["ct"], rc["st"], rc["dtot"]
    C["rcols"] = np.ascontiguousarray(np.stack([rc["cols"][:, 0:12], rc["cols"][:, 12:24]]))
    C["mcq"], C["msq"] = rot_tables(96, L, 64, 96, 500000.0, 96 ** -0.5)
    C["mck"], C["msk"] = rot_tables(96, L, 64, 96, 500000.0, 1.0)
    C["dcq"], C["dsq"] = rot_tables(64, L, 0, 16, 500000.0, 64 ** -0.5)
    C["dck"], C["dsk_"] = rot_tables(64, L, 0, 16, 500000.0, 1.0)
    C["dmask"] = dil_masks()
    C["ident"] = np.eye(128, dtype=np.float32)
    C["sconst"] = ssd_consts()
    C["tokid"] = (np.arange(128)[:, None] + 128 * np.arange(32)[None, :]).astype(np.float32)
    C["iota512"] = np.broadcast_to(np.arange(512, dtype=np.float32)[None, :], (128, 512)).copy()
    return C

def host_layer_weights(P, l):
    L = L_
    f = lambda a: np.ascontiguousarray(a, dtype=np.float32)
    w = P['w_in'][l]
    rq, rk, rv, rg = w[:, 0:256], w[:, 256:512], w[:, 512:768], w[:, 768:1024]
    cq, ckv, kr = w[:, 1024:1280], w[:, 1280:1408], w[:, 1408:1440]
    sz, sxbc, sdt = w[:, 1440:1696], w[:, 1696:2464], w[:, 2464:2472]
    dq, dk, dv = w[:, 2472:2728], w[:, 2728:2984], w[:, 2984:3240]
    pr, pd = _perm_ret(), _perm_dil()
    krs = np.concatenate([kr[:, 16:32], kr[:, 0:16]], axis=1)
    wF = np.concatenate([rq, rk, cq, ckv, kr, sxbc, dq, dk], axis=1)
    dtc = [0, 1, 4, 5, 2, 3, 6, 7]
    wT = np.concatenate([rk, rv, rg, sz, sdt[:, dtc], dv], axis=1)
    assert wF.shape[1] == NF_ and wT.shape[1] == NT_
    uq = P['mla_w_uq'][l]
    def swq(j):
        return j + 16 if 64 <= j < 80 else (j - 16 if 80 <= j < 96 else j)
    pq = np.array([h * 96 + swq(j) for h in range(4) for j in range(96)])
    wuq = uq
    ukv = P['mla_w_ukv'][l].reshape(128, 4, 128)
    wkn = ukv[:, :, 0:64].reshape(128, 256); wv = ukv[:, :, 64:128].reshape(128, 256)
    cwt = np.zeros((2, 3, 128, 5), np.float32); cbt = np.zeros((2, 3, 128, 1), np.float32)
    dtb = np.zeros((2, L, 4), np.float32); alog = np.zeros((2, L, 4), np.float32); dsk = np.zeros((2, 128, 2), np.float32)
    for g_ in range(2):
        for i, c0 in enumerate((g_ * 128, 256 + g_ * 128, 512 + g_ * 128)):
            cols = list(range(c0, c0 + 128))
            cwt[g_, i] = P['ssm_conv_w'][l][:, cols].T
            cbt[g_, i, :, 0] = P['ssm_conv_b'][l][cols]
        hs = [2 * g_, 2 * g_ + 1]
        dcols = [hs[0], hs[1], 4 + hs[0], 4 + hs[1]]
        dtb[g_] = P['ssm_dt_bias'][l].reshape(8)[dcols][None]
        alog[g_] = P['ssm_a_log'][l].reshape(8)[dcols][None]
        dsk[g_] = P['ssm_d'][l][hs][None]
    return dict(wF=f(wF), wT=f(wT), ln1=f(P['ln1_w'][l]), wuq=f(wuq), qn=f(P['mla_q_norm_w'][l]), wkn=f(wkn), wv=f(wv),
                kvn=f(P['mla_kv_norm_w'][l]), cw=cwt, cb=cbt, dtb=dtb, alog=alog, dsk=dsk, snw=f(P['ssm_norm_w'][l]),
                wout=f(P['w_out'][l]), ln2=f(P['ln2_w'][l]), rw=f(P['router_w'][l]), wg=f(P['exp_w_gate'][l]), wu=f(P['exp_w_up'][l]),
                wd=f(P['exp_w_down'][l]))

def kernel(**inputs):
    P = {k: np.asarray(v) for k, v in inputs.items()}
    x = np.asarray(P['x'], dtype=np.float32)
    nc = build_fused(depth=2)
    base = dict(host_consts())
    base["fnw"] = np.ascontiguousarray(P['final_norm_w'], dtype=np.float32)
    base["ones1k"] = np.ones(1024, np.float32)
    for l in range(2):
        for k_, v_ in host_layer_weights(P, l).items():
            base["%s%d" % (k_, l)] = v_
    in_maps = []
    for b in range(4):
        m = dict(base)
        m["xT"] = np.ascontiguousarray(x[b].T)
        in_maps.append(m)
    res = run_bass_kernel_spmd(nc, in_maps, core_ids=[0, 1, 2, 3])
    return np.stack([res.results[b]["out"] for b in range(4)]).astype(np.float32)
```

```python
import numpy as np
from contextlib import ExitStack
import concourse.bass as bass
import concourse.mybir as mybir
from concourse.bass_utils import run_bass_kernel_spmd
F32 = mybir.dt.float32
BF16 = mybir.dt.bfloat16
U32 = mybir.dt.uint32
AF = mybir.ActivationFunctionType
ALU = mybir.AluOpType
AX = mybir.AxisListType


CE = ("tensor", "vector", "scalar", "gpsimd")
ENGS = ("sync",) + CE
NDS = 48


def _key(x):
    if isinstance(x, str):
        return x
    if isinstance(x, tuple):
        return x[1]
    return x.tensor.name


def _ap(x):
    return x[0] if isinstance(x, tuple) else x


class Sched:
    def __init__(self, nc, es):
        self.nc, self.es = nc, es
        self.q = {e: [] for e in ENGS}
        self.cnt = {e: 0 for e in CE}
        self.sem = {e: es.enter_context(nc.semaphore("s_" + e)) for e in CE}
        self.dsem = [es.enter_context(nc.semaphore("d%d" % i)) for i in range(NDS)]
        self.dcnt = [0] * NDS
        self.dnext = 0
        self.seen_c = {e: {f: 0 for f in CE} for e in ENGS}
        self.seen_d = {e: [0] * NDS for e in ENGS}
        self.last_w = {}
        self.readers = {}
        self.n_alloc = 0
        self.psum_keys = set()
        self.ninstr = 0
        self.prefix = ""
        self.root_es = es
        self.epoch = 0

    def sb(self, shape, dtype, name=None):
        self.n_alloc += 1
        name = self.prefix + (name or "t%d" % self.n_alloc)
        return self.es.enter_context(self.nc.sbuf_tensor(name, list(shape), dtype))

    def ps(self, shape, dtype, name=None):
        self.n_alloc += 1
        name = self.prefix + (name or "p%d" % self.n_alloc)
        t = self.es.enter_context(self.nc.psum_tensor(name, [128, 512], mybir.dt.float32))
        self.psum_keys.add(name)
        return t[0:shape[0], 0:shape[1]]

    def scope(self, prefix):
        sched = self

        class _Scope:
            def __enter__(self_):
                self_.old = (sched.es, sched.prefix)
                self_.stack = ExitStack()
                self_.stack.__enter__()
                sched.es, sched.prefix = self_.stack, prefix
                return sched

            def __exit__(self_, *a):
                sched.barrier()
                sched.es, sched.prefix = self_.old
                return self_.stack.__exit__(*a)
        return _Scope()

    def _wait(self, stream, tok):
        if tok is None:
            return
        if tok[0] == "c":
            _, eng, n, ep = tok
            if ep < self.epoch:
                return
            if stream == "tensor" and eng == "tensor":
                return
            if self.seen_c[stream][eng] >= n:
                return
            self.seen_c[stream][eng] = n
            sem = self.sem[eng]
            self.q[stream].append(lambda e, sem=sem, n=n: e.wait_ge(sem, n))
        else:
            _, i, n = tok
            if self.seen_d[stream][i] >= n:
                return
            self.seen_d[stream][i] = n
            sem = self.dsem[i]
            self.q[stream].append(lambda e, sem=sem, n=n: e.wait_ge(sem, n))

    def _deps(self, stream, reads, writes):
        for r in reads:
            self._wait(stream, self.last_w.get(_key(r)))
        for w in writes:
            k = _key(w)
            self._wait(stream, self.last_w.get(k))
            for t in self.readers.get(k, ()):
                self._wait(stream, t)

    def _commit(self, tok, reads, writes):
        for r in reads:
            self.readers.setdefault(_key(r), []).append(tok)
        for w in writes:
            k = _key(w)
            self.last_w[k] = tok
            self.readers[k] = []

    def op(self, eng, fn, reads=(), writes=()):
        extra = [r for r in reads if _key(r) in self.psum_keys]
        if extra:
            writes = list(writes) + extra
        self._deps(eng, reads, writes)
        self.cnt[eng] += 1
        n = self.cnt[eng]
        sem = self.sem[eng]
        self.q[eng].append(lambda e, fn=fn, sem=sem: fn(e).then_inc(sem, 1))
        self.seen_c[eng][eng] = max(self.seen_c[eng][eng], 0)
        self._commit(("c", eng, n, self.epoch), reads, writes)
        self.ninstr += 1

    def dma(self, out, in_, queue="sync", reads=None, writes=None, **kw):
        reads = [in_] if reads is None else reads
        writes = [out] if writes is None else writes
        self._deps(queue, reads, writes)
        i = self.dnext
        self.dnext = (self.dnext + 1) % NDS
        if self.dcnt[i]:
            self._wait(queue, ("d", i, self.dcnt[i]))
        self.dcnt[i] += 16
        sem = self.dsem[i]
        o, a = _ap(out), _ap(in_)
        self.q[queue].append(
            lambda e, o=o, a=a, sem=sem, kw=kw: e.dma_start(out=o, in_=a, **kw).then_inc(sem, 16))
        self._commit(("d", i, self.dcnt[i]), reads, writes)
        self.ninstr += 1

    def raw_dma(self, queue, fn, reads, writes):
        self._deps(queue, reads, writes)
        i = self.dnext
        self.dnext = (self.dnext + 1) % NDS
        if self.dcnt[i]:
            self._wait(queue, ("d", i, self.dcnt[i]))
        self.dcnt[i] += 16
        sem = self.dsem[i]
        self.q[queue].append(lambda e, fn=fn, sem=sem: fn(e).then_inc(sem, 16))
        self._commit(("d", i, self.dcnt[i]), reads, writes)
        self.ninstr += 1

    def barrier(self):
        for s in ENGS:
            for e in CE:
                if self.cnt[e]:
                    self._wait(s, ("c", e, self.cnt[e], self.epoch))
            for i in range(NDS):
                if self.dcnt[i]:
                    self._wait(s, ("d", i, self.dcnt[i]))

    def new_epoch(self):
        self.barrier()
        self.epoch += 1
        self.sem = {e: self.root_es.enter_context(self.nc.semaphore("s%d_%s" % (self.epoch, e))) for e in CE}
        self.cnt = {e: 0 for e in CE}
        self.seen_c = {e: {f: 0 for f in CE} for e in ENGS}

    def finish(self):
        for e in CE:
            if self.cnt[e]:
                self._wait("sync", ("c", e, self.cnt[e], self.epoch))
        for i in range(NDS):
            if self.dcnt[i]:
                self._wait("sync", ("d", i, self.dcnt[i]))
        with self.nc.Block() as block:
            @block.sync
            def _(e):
                for f in self.q["sync"]:
                    f(e)

            @block.tensor
            def _(e):
                for f in self.q["tensor"]:
                    f(e)

            @block.vector
            def _(e):
                for f in self.q["vector"]:
                    f(e)

            @block.scalar
            def _(e):
                for f in self.q["scalar"]:
                    f(e)

            @block.gpsimd
            def _(e):
                for f in self.q["gpsimd"]:
                    f(e)

    def mm(self, out, lhsT, rhs, start=True, stop=True, **kw):
        o, l, r = _ap(out), _ap(lhsT), _ap(rhs)
        self.op("tensor", lambda e: e.matmul(o, l, r, start=start, stop=stop, **kw),
                reads=[lhsT, rhs] + ([] if start else [out]), writes=[out])

    def tr(self, out, in_, ident):
        o, a, i = _ap(out), _ap(in_), _ap(ident)
        self.op("tensor", lambda e: e.transpose(o, a, i), reads=[in_, ident], writes=[out])

    def act(self, out, in_, func, bias=None, scale=None, accum_out=None, eng="scalar", extra_reads=()):
        o, a = _ap(out), _ap(in_)
        kw = {}
        reads = [in_] + list(extra_reads)
        writes = [out]
        if bias is not None:
            kw["bias"] = _ap(bias) if not isinstance(bias, (int, float)) else bias
            if not isinstance(bias, (int, float)):
                reads.append(bias)
        if scale is not None:
            kw["scale"] = _ap(scale) if not isinstance(scale, (int, float)) else scale
            if not isinstance(scale, (int, float)):
                reads.append(scale)
        if accum_out is not None:
            kw["accum_out"] = _ap(accum_out)
            writes.append(accum_out)
        self.op("scalar", lambda e: e.activation(o, a, func, **kw), reads=reads, writes=writes)

    def tt(self, out, in0, in1, op, eng="vector"):
        o, a, b = _ap(out), _ap(in0), _ap(in1)
        self.op(eng, lambda e: e.tensor_tensor(o, a, b, op), reads=[in0, in1], writes=[out])

    def ts(self, out, in0, s1, s2, op0, op1=None, eng="vector", accum_out=None):
        o, a = _ap(out), _ap(in0)
        reads = [in0]
        v1 = s1
        if not isinstance(s1, (int, float)):
            reads.append(s1)
            v1 = _ap(s1)
        v2 = s2
        if s2 is not None and not isinstance(s2, (int, float)):
            reads.append(s2)
            v2 = _ap(s2)
        writes = [out]
        kw = {}
        if accum_out is not None:
            kw["accum_out"] = _ap(accum_out)
            writes.append(accum_out)
        if op1 is None:
            self.op(eng, lambda e: e.tensor_scalar(o, a, v1, v2, op0, **kw), reads=reads, writes=writes)
        else:
            self.op(eng, lambda e: e.tensor_scalar(o, a, v1, v2, op0, op1, **kw), reads=reads, writes=writes)

    def stt(self, out, in0, scalar, in1, op0, op1, eng="vector"):
        o, a, b = _ap(out), _ap(in0), _ap(in1)
        reads = [in0, in1]
        v = scalar
        if not isinstance(scalar, (int, float)):
            reads.append(scalar)
            v = _ap(scalar)
        self.op(eng, lambda e: e.scalar_tensor_tensor(o, a, v, b, op0, op1), reads=reads, writes=[out])

    def copy(self, out, in_, eng="vector"):
        o, a = _ap(out), _ap(in_)
        if eng == "scalar":
            self.op(eng, lambda e: e.copy(o, a), reads=[in_], writes=[out])
        else:
            self.op(eng, lambda e: e.tensor_copy(o, a), reads=[in_], writes=[out])

    def memset(self, out, val, eng="vector"):
        o = _ap(out)
        self.op(eng, lambda e: e.memset(o, val), reads=[], writes=[out])


def half_swap(a, lo, hi):
    b = a.copy()
    m = (lo + hi) // 2
    b[..., lo:m, :] = a[..., m:hi, :]
    b[..., m:hi, :] = a[..., lo:m, :]
    return b

def rot_tables(dk, L, lo, hi, theta, scale):
    C = np.ones((dk, L), np.float64)
    Sg = np.zeros((dk, L), np.float64)
    rd = hi - lo
    inv = 1.0 / (theta ** (np.arange(0, rd, 2, dtype=np.float32) / np.float32(rd))).astype(np.float32)
    ang = (np.arange(L, dtype=np.float32)[:, None] * inv[None, :]).astype(np.float32)
    c, s = np.cos(ang.astype(np.float64)).T, np.sin(ang.astype(np.float64)).T
    C[lo:lo + rd // 2] = c
    C[lo + rd // 2:hi] = c
    Sg[lo:lo + rd // 2] = -s
    Sg[lo + rd // 2:hi] = s
    return (C * scale).astype(np.float32), (Sg * scale).astype(np.float32)

def dil_masks():
    M = np.zeros((20, 128, 512), np.float32)
    j = np.arange(128)[:, None]
    i = np.arange(512)[None, :]
    for rr in range(20):
        d_ = 128 * (rr - 8) + j - i
        for (win, dil) in ((128, 1), (512, 4), (2048, 16)):
            M[rr] += ((d_ % dil == 0) & (np.abs(d_) <= (win // (2 * dil)) * dil)).astype(np.float32)
    return M

def dil_plan():
    return [[(t, t - 4 * g + 8) for t in range(4 * g - 8, 4 * g + 12) if 0 <= t < 32] for g in range(8)]

def mla_plan():
    return [[(t, None) for t in range(32)] for g in range(8)]


def ret_consts(L, heads):
    NH = len(heads)
    inv = 1.0 / (10000.0 ** (np.arange(0, 64, 2, dtype=np.float32) / np.float32(64))).astype(np.float32)
    ang = (np.arange(L, dtype=np.float32)[:, None] * inv[None, :]).astype(np.float32).astype(np.float64)
    c, s = np.cos(ang), np.sin(ang)
    ct = np.concatenate([c, c], 1); st = np.concatenate([-s, s], 1)
    ck, sk = ct.T.copy(), st.T.copy()
    dtot = np.zeros((NH, 128, 128)); cols = np.zeros((128, NH * 6))
    p = np.arange(128, dtype=np.float64)
    for n, h in enumerate(heads):
        lgf = np.log1p(-np.exp2(np.float64(-5.0 - 0.0 - h))); lgb = np.log1p(-np.exp2(np.float64(-5.0 - 0.5 - h)))
        j = p[:, None]; i = p[None, :]
        dtot[n] = 0.125 * (np.where(i >= j, np.exp(lgf * np.maximum(i - j, 0)), 0.0) + np.where(j > i, np.exp(lgb * np.maximum(j - i, 0)), 0.0))
        cols[:, n * 6 + 0] = 0.125 * np.exp(lgf * (p + 1))
        cols[:, n * 6 + 1] = 0.125 * np.exp(lgb * (128 - p))
        cols[:, n * 6 + 2] = np.exp(lgf * (127 - p))
        cols[:, n * 6 + 3] = np.exp(lgb * p)
        cols[:, n * 6 + 4] = np.exp(lgf * 128)
        cols[:, n * 6 + 5] = np.exp(lgb * 128)
    f = lambda a: np.ascontiguousarray(a, dtype=np.float32)
    return dict(ck=f(ck), sk=f(sk), ct=f(ct), st=f(st), dtot=f(dtot), cols=f(cols))

def swap64(a, axis):
    return np.concatenate([np.take(a, range(32, 64), axis), np.take(a, range(0, 32), axis)], axis)


def ssd_consts():
    s = np.arange(128)[:, None]; l = np.arange(128)[None, :]
    return np.stack([(s <= l), (s >= l), (l >= s), (l < s), (s == l)]).astype(np.float32)


def emit_lin(S, xT, w, g, out, K, ntok, N, norm=True, softmax=False, fp32=False, eps=1e-6, tag="o", NB=2):
    KT = K // 128
    MD = F32 if fp32 else BF16
    W = S.sb([128, KT, N], MD, "W")
    Wraw = [S.sb([128, N], F32, "Wraw%d" % i) for i in range(2)]
    gs = S.sb([128, KT], F32, "gs")
    ones = S.sb([128, 1], F32, "ones")
    S.memset(ones[:], 1.0)
    S.dma(gs[:], g.rearrange("(k p) -> p k", p=128), allow_slow_non_contiguous=True)
    wv = w.rearrange("(k p) n -> p k n", p=128)
    for k in range(KT):
        S.dma(Wraw[k % 2][:], wv[:, k, :], queue="sync" if k % 2 == 0 else "gpsimd")
        S.ts((W[:, k, :], "W%d" % k), Wraw[k % 2][:], gs[:, k:k + 1], None, ALU.mult,
             eng="vector" if k % 2 == 0 else "gpsimd")
    x32 = [S.sb([128, KT, 128], F32, "x32_%d" % i) for i in range(NB)]
    xt = x32 if fp32 else [S.sb([128, KT, 128], BF16, "xt%d" % i) for i in range(NB)]
    sq = [S.sb([128, KT, 128], F32, "sq%d" % i) for i in range(NB)]
    ob = [S.sb([128, N], F32, "ob%d" % i) for i in range(NB)]
    rstd = [S.sb([128, 1], F32, "rstd%d" % i) for i in range(NB)]
    mx = [S.sb([128, 1], F32, "mx%d" % i) for i in range(NB)]
    sm = [S.sb([128, 1], F32, "sm%d" % i) for i in range(NB)]
    ssp = S.ps([128, 1], F32, "ssp")
    pp = [S.ps([128, 512], F32, "pp%d" % i) for i in range(4)]
    xv = xT.rearrange("(k p) t -> p k t", p=128)
    groups = [(c, min(512, N - c)) for c in range(0, N, 512)]
    gi = 0
    for t in range(ntok // 128):
        b = t % NB
        S.dma(x32[b][:], xv[:, :, t * 128:(t + 1) * 128], queue="sync")
        if not fp32:
            S.copy(xt[b][:], x32[b][:], eng="gpsimd")
        if norm:
            S.act(sq[b][:], x32[b][:], AF.Square)
            for k in range(KT):
                S.mm(ssp, sq[b][:, k, :], ones[:], start=(k == 0), stop=(k == KT - 1))
            S.ts(rstd[b][:], ssp, 1.0 / K, eps, ALU.mult, ALU.add)
            S.act(rstd[b][:], rstd[b][:], AF.Sqrt)
            S.op("vector", lambda e, b=b: e.reciprocal(rstd[b][:], rstd[b][:]), reads=[rstd[b][:]], writes=[rstd[b][:]])
        else:
            S.memset(rstd[b][:], 1.0)
        keys = []
        for (c0, cw) in groups:
            p = pp[gi % 4]
            for k in range(KT):
                S.op("tensor", (lambda e, p=p, b=b, k=k, c0=c0, cw=cw: e.matmul(p[:, :cw], xt[b][:, k, :], W[:, k, c0:c0 + cw], start=(k == 0), stop=(k == KT - 1))),
                     reads=[xt[b][:], "W%d" % k], writes=[p])
            key = "ob%d_%d" % (b, c0)
            keys.append(key)
            if gi % 2 == 0:
                S.act((ob[b][:, c0:c0 + cw], key), p[:, :cw], AF.Copy, scale=rstd[b][:])
            else:
                S.ts((ob[b][:, c0:c0 + cw], key), p[:, :cw], rstd[b][:], None, ALU.mult)
            gi += 1
        if softmax:
            key = keys[0]
            S.op("vector", lambda e, b=b: e.reduce_max(mx[b][:], ob[b][:], AX.X), reads=[key], writes=[mx[b][:]])
            S.ts(mx[b][:], mx[b][:], -1.0, None, ALU.mult)
            S.op("scalar", lambda e, b=b: e.activation(ob[b][:], ob[b][:], AF.Exp, bias=mx[b][:], accum_out=sm[b][:]),
                 reads=[key, mx[b][:]], writes=[key, sm[b][:]])
            S.op("vector", lambda e, b=b: e.reciprocal(sm[b][:], sm[b][:]), reads=[sm[b][:]], writes=[sm[b][:]])
            S.op("vector", lambda e, b=b: e.tensor_scalar(ob[b][:], ob[b][:], sm[b][:], None, ALU.mult), reads=[key, sm[b][:]], writes=[key])
        wk = "%s_w%d" % (tag, t)
        S.dma(out[t * 128:(t + 1) * 128, :], ob[b][:], queue="gpsimd", reads=keys, writes=[wk])
        for key in keys:
            S.readers.setdefault(key, []).append(S.last_w[wk])


def emit_linF(S, xT, w, g, outT, K, ntok, N, norm=True, resid=None, eps=1e-6, tag="f"):
    KT = K // 128
    W = S.sb([128, KT, N], BF16, "W")
    Wraw = [S.sb([128, N], F32, "Wraw%d" % i) for i in range(2)]
    gs = S.sb([128, KT], F32, "gs")
    ones = S.sb([128, 128], F32, "ones")
    S.memset(ones[:], 1.0)
    S.dma(gs[:], g.rearrange("(k p) -> p k", p=128), allow_slow_non_contiguous=True)
    wv = w.rearrange("(k p) n -> p k n", p=128)
    for k in range(KT):
        S.dma(Wraw[k % 2][:], wv[:, k, :], queue="sync" if k % 2 == 0 else "gpsimd")
        S.ts((W[:, k, :], "W%d" % k), Wraw[k % 2][:], gs[:, k:k + 1], None, ALU.mult,
             eng="vector" if k % 2 == 0 else "gpsimd")
    NB = 2
    x32 = [S.sb([128, KT, 512], F32, "x32_%d" % i) for i in range(NB)]
    xt = [S.sb([128, KT, 512], BF16, "xt%d" % i) for i in range(NB)]
    sq = [S.sb([128, 512], F32, "sq%d" % i) for i in range(NB)]
    rb = [S.sb([128, 512], F32, "rb%d" % i) for i in range(NB)]
    ob = [S.sb([128, 512], F32, "ob%d" % i) for i in range(4)]
    rs = [S.sb([128, 512], F32, "rs%d" % i) for i in range(2)] if resid is not None else None
    pss = S.ps([128, 512], F32, "pss")
    pp = [S.ps([128, 512], F32, "pp%d" % i) for i in range(4)]
    xv = xT.rearrange("(k p) t -> p k t", p=128)
    mts = [(m, min(128, N - m)) for m in range(0, N, 128)]
    gi = 0
    for t in range(ntok // 512):
        b = t % NB
        ts_ = slice(t * 512, (t + 1) * 512)
        S.dma(x32[b][:], xv[:, :, ts_], queue="sync")
        S.copy(xt[b][:], x32[b][:], eng="gpsimd")
        if norm:
            for k in range(KT):
                S.act(sq[b][:], x32[b][:, k, :], AF.Square)
                S.mm(pss, ones[:], sq[b][:], start=(k == 0), stop=(k == KT - 1))
            S.ts(rb[b][:], pss, 1.0 / K, eps, ALU.mult, ALU.add)
            S.act(rb[b][:], rb[b][:], AF.Sqrt)
            S.op("vector", lambda e, b=b: e.reciprocal(rb[b][:], rb[b][:]), reads=[rb[b][:]], writes=[rb[b][:]])
        for (m0, mw) in mts:
            p = pp[gi % 4]
            o = ob[gi % 4]
            for k in range(KT):
                S.op("tensor", (lambda e, p=p, b=b, k=k, m0=m0, mw=mw: e.matmul(p[0:mw, :], W[:, k, m0:m0 + mw], xt[b][:, k, :], start=(k == 0), stop=(k == KT - 1))),
                     reads=[xt[b][:], "W%d" % k], writes=[p])
            if resid is not None:
                r_ = rs[gi % 2]
                S.dma(r_[0:mw, :], resid[m0:m0 + mw, ts_], queue="sync")
                S.tt(o[0:mw, :], p[0:mw, :], r_[0:mw, :], ALU.add, eng="vector")
            elif norm:
                S.tt(o[0:mw, :], p[0:mw, :], rb[b][0:mw, :], ALU.mult, eng="vector")
            else:
                S.copy(o[0:mw, :], p[0:mw, :], eng="scalar")
            wk = "%s_w%d_%d" % (tag, t, m0)
            S.dma(outT[m0:m0 + mw, ts_], o[0:mw, :], queue="gpsimd", reads=[o[:]], writes=[wk])
            S.readers.setdefault(_key(o[:]), []).append(S.last_w[wk])
            gi += 1


def emit_transpose(S, src, dstT, L, C, idt, norm_w=None, eps=1e-6, tag="tr", nm=""):
    CT = C // 128
    NB = 2
    xin = [S.sb([128, C], F32, nm + "xin%d" % i) for i in range(NB)]
    sqs = S.sb([128, C], F32, nm + "sqs")
    rstd = [S.sb([128, 1], F32, nm + "rstd%d" % i) for i in range(NB)]
    ot = [S.sb([128, 4, 128], F32, nm + "ot%d" % i) for i in range(2 * CT)]
    pt = [S.ps([128, 512], F32, nm + "pt%d" % i) for i in range(2)]
    nw = None
    if norm_w is not None:
        nw = S.sb([128, CT], F32, nm + "nw")
        S.dma(nw[:], norm_w.rearrange("(k p) -> p k", p=128), allow_slow_non_contiguous=True)
    gi = 0
    for t4 in range(L // 512):
        for j in range(4):
            t = t4 * 4 + j
            b = t % NB
            S.dma(xin[b][:], src[t * 128:(t + 1) * 128, :], queue="sync" if t % 2 == 0 else "gpsimd")
            if norm_w is not None:
                S.op("scalar", lambda e, b=b: e.activation(sqs[:], xin[b][:], AF.Square, accum_out=rstd[b][:]),
                     reads=[xin[b][:]], writes=[sqs[:], rstd[b][:]])
                S.ts(rstd[b][:], rstd[b][:], 1.0 / C, eps, ALU.mult, ALU.add)
                S.act(rstd[b][:], rstd[b][:], AF.Sqrt)
                S.op("vector", lambda e, b=b: e.reciprocal(rstd[b][:], rstd[b][:]), reads=[rstd[b][:]], writes=[rstd[b][:]])
                S.ts(xin[b][:], xin[b][:], rstd[b][:], None, ALU.mult)
            for c in range(CT):
                p = pt[gi % 2]
                S.tr(p[:, 0:128], xin[b][:, c * 128:(c + 1) * 128], idt)
                o = ot[(t4 % 2) * CT + c]
                okey = "%s%sot%d_%d" % (S.prefix, nm, (t4 % 2) * CT + c, j)
                if norm_w is not None:
                    S.act((o[:, j, :], okey), p[:, 0:128], AF.Copy, scale=nw[:, c:c + 1])
                elif gi % 2 == 0:
                    S.op("vector", lambda e, o=o, j=j, p=p: e.tensor_copy(o[:, j, :], p[:, 0:128]), reads=[p], writes=[okey])
                else:
                    S.op("scalar", lambda e, o=o, j=j, p=p: e.copy(o[:, j, :], p[:, 0:128]), reads=[p], writes=[okey])
                gi += 1
        for c in range(CT):
            o = ot[(t4 % 2) * CT + c]
            keys = ["%s%sot%d_%d" % (S.prefix, nm, (t4 % 2) * CT + c, j) for j in range(4)]
            wk = "%s_w%d_%d" % (tag, t4, c)
            S.dma(dstT[c * 128:(c + 1) * 128, t4 * 512:(t4 + 1) * 512], o[:].rearrange("p j t -> p (j t)"), queue="gpsimd", reads=keys, writes=[wk])
            for key in keys:
                S.readers.setdefault(key, []).append(S.last_w[wk])


def emit_untranspose(S, srcT, dst, L, C, idt, tag="ut", odt=None, evac=None, dst2=None):
    CT = C // 128
    xin = [S.sb([128, CT, 128], F32, "u_xin%d" % i) for i in range(2)]
    ot = [S.sb([128, C], odt or F32, "u_ot%d" % i) for i in range(2)]
    pt = [S.ps([128, 512], F32, "u_pt%d" % i) for i in range(2)]
    sv = srcT.rearrange("(k p) t -> p k t", p=128)
    gi = 0
    for t in range(L // 128):
        b = t % 2
        S.dma(xin[b][:], sv[:, :, t * 128:(t + 1) * 128], queue="sync")
        keys = []
        for c in range(CT):
            p = pt[gi % 2]
            S.tr(p[:, 0:128], xin[b][:, c, :], idt)
            key = "%sot%d_%d" % (S.prefix, b, c)
            keys.append(key)
            if gi % 2 == 0 and evac != "scalar":
                S.op("vector", lambda e, b=b, c=c, p=p: e.tensor_copy(ot[b][:, c * 128:(c + 1) * 128], p[:, 0:128]), reads=[p], writes=[key])
            else:
                S.op("scalar", lambda e, b=b, c=c, p=p: e.copy(ot[b][:, c * 128:(c + 1) * 128], p[:, 0:128]), reads=[p], writes=[key])
            gi += 1
        wk = "%s_w%d" % (tag, t)
        S.dma(dst[t * 128:(t + 1) * 128, :], ot[b][:], queue="gpsimd", reads=keys, writes=[wk])
        for key in keys:
            S.readers.setdefault(key, []).append(S.last_w[wk])
        if dst2 is not None:
            wk2 = "%s_v%d" % (tag, t)
            S.dma(dst2[t * 128:(t + 1) * 128, :], ot[b][:], queue="sync", reads=keys, writes=[wk2])
            for key in keys:
                S.readers.setdefault(key, []).append(S.last_w[wk2])


def emit_finalnorm(S, srcT, fw, out, L, idt, eps=1e-6):
    CT = 8
    xin = [S.sb([128, CT, 128], F32, "xin%d" % i) for i in range(2)]
    sq = [S.sb([128, CT, 128], F32, "sq%d" % i) for i in range(2)]
    ot = [S.sb([128, 1024], F32, "ot%d" % i) for i in range(2)]
    rstd = [S.sb([128, 1], F32, "rstd%d" % i) for i in range(2)]
    ones = S.sb([128, 1], F32, "ones"); S.memset(ones[:], 1.0)
    FW = S.sb([128, 1024], F32, "FW")
    S.dma(FW[:], fw.partition_broadcast(128))
    ssp = S.ps([128, 1], F32, "ssp")
    pt = [S.ps([128, 512], F32, "pt%d" % i) for i in range(2)]
    sv = srcT.rearrange("(k p) t -> p k t", p=128)
    gi = 0
    for t in range(L // 128):
        b = t % 2
        S.dma(xin[b][:], sv[:, :, t * 128:(t + 1) * 128], queue="sync")
        S.act(sq[b][:], xin[b][:], AF.Square)
        for k in range(CT):
            S.mm(ssp, sq[b][:, k, :], ones[:], start=(k == 0), stop=(k == CT - 1))
        S.ts(rstd[b][:], ssp, 1.0 / 1024, eps, ALU.mult, ALU.add)
        S.act(rstd[b][:], rstd[b][:], AF.Sqrt)
        S.op("vector", lambda e, b=b: e.reciprocal(rstd[b][:], rstd[b][:]), reads=[rstd[b][:]], writes=[rstd[b][:]])
        keys = []
        for c in range(CT):
            p = pt[gi % 2]
            S.tr(p[:, 0:128], xin[b][:, c, :], idt)
            key = "%sot%d_%d" % (S.prefix, b, c)
            keys.append(key)
            S.op("vector", lambda e, b=b, c=c, p=p: e.scalar_tensor_tensor(ot[b][:, c * 128:(c + 1) * 128], p[:, 0:128], rstd[b][:], FW[:, c * 128:(c + 1) * 128], ALU.mult, ALU.mult),
                 reads=[p, rstd[b][:], FW[:]], writes=[key])
            gi += 1
        wk = "fin_w%d" % t
        S.dma(out[t * 128:(t + 1) * 128, :], ot[b][:], queue="gpsimd", reads=keys, writes=[wk])
        for key in keys:
            S.readers.setdefault(key, []).append(S.last_w[wk])


def emit_attn(S, dk, dv, L, NH, plan, nmask, q, qp, kpc, kppc, tabs_d, v, masks, idt, y, tag="at"):
    NT = L // 128
    NG = L // 512
    tabs = {}
    for i, n in enumerate(("cq", "sq", "ck", "sk")):
        t = S.sb([dk, L], F32, "tab_" + n)
        S.dma(t[:], tabs_d[i], queue="sync")
        tabs[n] = t
    mk = None
    if nmask:
        mk32 = S.sb([128, 512], F32, "mk32")
        mk = S.sb([128, nmask, 512], BF16, "mk")
        for i in range(nmask):
            S.dma(mk32[:], masks[i])
            S.copy((mk[:, i, :], "mk%d" % i), mk32[:], eng="vector")
    st = [S.sb([dk, L], F32, "st%d" % i) for i in range(2)]
    qr = [S.sb([dk, L], BF16, "qr%d" % h) for h in range(NH)]
    kr = [S.sb([dk, L], BF16, "kr%d" % h) for h in range(NH)]
    va = [S.sb([128, NT, dv + 1], BF16, "va%d" % h) for h in range(NH)]
    v32_ = S.sb([128, NT, dv], F32, "v32")

    def prologue(h):
        for (pcs, pcsp, c, s_, dst) in ((q[h], qp[h], "cq", "sq", qr[h]), (kpc[h], kppc[h], "ck", "sk", kr[h])):
            for (lo, hi, a_) in pcs:
                S.dma(st[0][lo:hi, :], a_, queue="sync", reads=[a_], writes=[st[0][:]])
            for (lo, hi, a_) in pcsp:
                S.dma(st[1][lo:hi, :], a_, queue="gpsimd", reads=[a_], writes=[st[1][:]])
            S.tt(st[0][:], st[0][:], tabs[c][:], ALU.mult, eng="vector")
            S.tt(st[1][:], st[1][:], tabs[s_][:], ALU.mult, eng="gpsimd")
            S.tt(dst[:], st[0][:], st[1][:], ALU.add, eng="vector")
        S.dma(v32_[:], v[h].rearrange("(t p) d -> p t d", p=128), queue="sync")
        S.memset(va[h][:], 1.0, eng="gpsimd")
        S.copy(va[h][:, :, 0:dv], v32_[:], eng="vector")

    prologue(0)
    NPS = 4
    pss = [S.ps([128, 512], F32, "pss%d" % i) for i in range(NPS)]
    pso = [S.ps([dv + 1, 512], F32, "pso%d" % i) for i in range(2)]
    pst = [S.ps([128, dv + 1], F32, "pst%d" % i) for i in range(2)]
    NPT = 4
    pt = [S.sb([128, 512], BF16, "pt%d" % i) for i in range(NPT)]
    ptm = [S.sb([128, 512], BF16, "ptm%d" % i) for i in range(NPT)]
    osb = [S.sb([dv + 1, 512], F32, "osb%d" % i) for i in range(2)]
    rec = [S.sb([128, 1], F32, "rec%d" % i) for i in range(2)]
    yo = [S.sb([128, 4, NH * dv], F32, "yo%d" % i) for i in range(2)]
    iters = [(g, h, n, kt, mid, len(plan[g])) for h in range(NH) for g in range(NG) for n, (kt, mid) in enumerate(plan[g])]
    LOOK = 3

    def emit_qk(i):
        g, h, n, kt, mid, ln = iters[i]
        S.mm(pss[i % NPS], kr[h][:, kt * 128:(kt + 1) * 128], qr[h][:, g * 512:(g + 1) * 512])

    for i in range(min(LOOK, len(iters))):
        emit_qk(i)
    gi = 0
    prepared = 1
    for it, (g, h, n, kt, mid, ln) in enumerate(iters):
        if prepared < NH and h == prepared - 1 and g == 0 and n == ln - 1:
            prologue(prepared)
            prepared += 1
        if it + LOOK < len(iters):
            assert iters[it + LOOK][1] < prepared
            emit_qk(it + LOOK)
        po = pso[gi % 2]
        p_s = pss[it % NPS]
        e = pt[it % NPT]
        S.act(e[:], p_s, AF.Exp)
        if mid is not None:
            em = ptm[it % NPT]
            S.tt(em[:], e[:], (mk[:, mid, :], "mk%d" % mid), ALU.mult,
                 eng="vector" if it % 3 != 2 else "gpsimd")
            e = em
        S.mm(po, va[h][:, kt, :], e[:], start=(n == 0), stop=(n == ln - 1))
        if n != ln - 1:
            continue
        ob = osb[gi % 2]
        S.copy(ob[:], po, eng="vector")
        for j in range(4):
            tp = pst[j % 2]
            S.tr(tp, ob[:, j * 128:(j + 1) * 128], idt[0:dv + 1, 0:dv + 1])
            rc = rec[j % 2]
            S.op("vector", lambda e_, rc=rc, tp=tp: e_.reciprocal(rc[:], tp[:, dv:dv + 1]),
                 reads=[tp], writes=[rc[:]])
            S.ts((yo[g % 2][:, j, h * dv:(h + 1) * dv], "%syo%d_%d_%d" % (S.prefix, g % 2, j, h)), tp[:, 0:dv], rc[:], None, ALU.mult)
        gi += 1
        keys = ["%syo%d_%d_%d" % (S.prefix, g % 2, j, h) for j in range(4)]
        wk = "%s_y%d_%d" % (tag, g, h)
        S.dma(y[g * 512:(g + 1) * 512, h * dv:(h + 1) * dv].rearrange("(j p) d -> p j d", p=128), yo[g % 2][:, :, h * dv:(h + 1) * dv], queue="sync", reads=keys, writes=[wk])
        for key in keys:
            S.readers.setdefault(key, []).append(S.last_w[wk])


def emit_ret(S, L, NH, q, k, kt_, v, g, ck, sk, ct, st_, dtot, cols, y, gn_eps=1e-5):
    NT = L // 128
    CK = S.sb([64, L], F32, "CK"); SK = S.sb([64, L], F32, "SK")
    CT = S.sb([128, NT, 64], F32, "CT"); STt = S.sb([128, NT, 64], F32, "STt")
    S.dma(CK[:], ck); S.dma(SK[:], sk, queue="gpsimd")
    tokv = lambda a: a.rearrange("(t p) d -> p t d", p=128)
    S.dma(CT[:], tokv(ct)); S.dma(STt[:], tokv(st_), queue="gpsimd")
    COL = S.sb([128, NH * 6], F32, "COL"); S.dma(COL[:], cols)
    DT = S.sb([128, NH, 128], F32, "DT")
    for h in range(NH):
        S.dma(DT[:, h, :], dtot[h])
    st0 = S.sb([64, L], F32, "st0"); st1 = S.sb([64, L], F32, "st1")
    qr = S.sb([64, L], BF16, "qr"); kr = S.sb([64, L], BF16, "kr")
    krt = S.sb([128, NT, 64], BF16, "krt")
    scrA = S.sb([128, NT * 128], F32, "scrA"); scrB = S.sb([128, NT * 128], F32, "scrB")
    vbf = S.sb([128, NT, 64], BF16, "vbf")
    vz = S.sb([128, NT, 128], BF16, "vz")
    S32 = S.sb([64, NT + 1, 128], F32, "S32")
    Sst = S.sb([64, NT, 128], BF16, "Sst")
    osb = S.sb([128, NT, NH, 64], F32, "osb")
    pST = [S.ps([128, 128], F32, "pST%d" % i) for i in range(2)]
    pIn = [S.ps([128, 64], F32, "pIn%d" % i) for i in range(2)]
    pX = [S.ps([128, 128], F32, "pX%d" % i) for i in range(2)]
    pU = [S.ps([64, 128], F32, "pU%d" % i) for i in range(2)]
    PT = [S.sb([128, 128], BF16, "PT%d" % i) for i in range(3)]
    tb = [S.sb([128, 64], F32, "tb%d" % i) for i in range(3)]
    P = S.prefix
    for h in range(NH):
        c6 = lambda i: COL[:, h * 6 + i:h * 6 + i + 1]
        for (src, dst) in ((q, qr), (k, kr)):
            S.dma(st0[:], src[h])
            S.dma(st1[0:32, :], src[h][32:64], queue="gpsimd", reads=[src[h]], writes=[st1[:]])
            S.dma(st1[32:64, :], src[h][0:32], queue="gpsimd", reads=[src[h]], writes=[st1[:]])
            S.tt(st0[:], st0[:], CK[:], ALU.mult, eng="vector")
            S.tt(st1[:], st1[:], SK[:], ALU.mult, eng="gpsimd")
            S.tt(dst[:], st0[:], st1[:], ALU.add, eng="vector")
        A3 = scrA[:, 0:NT * 64].rearrange("p (t d) -> p t d", d=64)
        A3b = scrA[:, NT * 64:NT * 128].rearrange("p (t d) -> p t d", d=64)
        S.dma(A3, tokv(kt_[h]), reads=[kt_[h]], writes=[scrA[:]])
        S.dma(A3b[:, :, 0:32], tokv(kt_[h])[:, :, 32:64], queue="gpsimd", reads=[kt_[h]], writes=[P + "scrA_b"])
        S.dma(A3b[:, :, 32:64], tokv(kt_[h])[:, :, 0:32], queue="gpsimd", reads=[kt_[h]], writes=[P + "scrA_b"])
        S.op("vector", lambda e, A3=A3: e.tensor_tensor(A3, A3, CT[:], ALU.mult), reads=[scrA[:], CT[:]], writes=[scrA[:]])
        S.op("gpsimd", lambda e, A3b=A3b: e.tensor_tensor(A3b, A3b, STt[:], ALU.mult), reads=[P + "scrA_b", STt[:]], writes=[P + "scrA_b"])
        S.op("vector", lambda e, A3=A3, A3b=A3b: e.tensor_tensor(krt[:], A3, A3b, ALU.add), reads=[scrA[:], P + "scrA_b"], writes=[krt[:]])
        B3 = scrB[:, 0:NT * 64].rearrange("p (t d) -> p t d", d=64)
        S.dma(B3, tokv(v[h]), reads=[v[h]], writes=[scrB[:]])
        S.op("vector", lambda e, B3=B3: e.tensor_copy(vbf[:], B3), reads=[scrB[:]], writes=[vbf[:]])
        S.op("vector", lambda e, B3=B3, h=h: e.tensor_scalar(vz[:, :, 0:64], B3, COL[:, h * 6 + 2:h * 6 + 3], None, ALU.mult), reads=[scrB[:], COL[:]], writes=[P + "vz_f"])
        S.op("gpsimd", lambda e, B3=B3, h=h: e.tensor_scalar(vz[:, :, 64:128], B3, COL[:, h * 6 + 3:h * 6 + 4], None, ALU.mult), reads=[scrB[:], COL[:]], writes=[P + "vz_b"])
        allS = [S32[:]] + [P + "S32f%d" % c for c in range(NT)] + [P + "S32b%d" % c for c in range(NT)]
        S.op("gpsimd", lambda e: e.memset(S32[:], 0.0), reads=[], writes=allS)
        for c in range(NT):
            pu = pU[c % 2]
            S.op("tensor", lambda e, pu=pu, c=c: e.matmul(pu, krt[:, c, :], vz[:, c, :], start=True, stop=True),
                 reads=[krt[:], P + "vz_f", P + "vz_b"], writes=[pu])
            if c + 1 <= NT - 1:
                S.op("scalar", lambda e, c=c, pu=pu: e.copy(S32[:, c + 1, 0:64], pu[:, 0:64]), reads=[pu, S32[:]], writes=[P + "S32f%d" % (c + 1)])
            if c >= 1:
                S.op("vector", lambda e, c=c, pu=pu: e.tensor_copy(S32[:, c - 1, 64:128], pu[:, 64:128]), reads=[pu, S32[:]], writes=[P + "S32b%d" % (c - 1)])
        for c in range(1, NT):
            S.op("vector", lambda e, c=c, h=h: e.scalar_tensor_tensor(S32[:, c, 0:64], S32[:, c - 1, 0:64], COL[0:64, h * 6 + 4:h * 6 + 5], S32[:, c, 0:64], ALU.mult, ALU.add),
                 reads=[P + "S32f%d" % (c - 1), P + "S32f%d" % c, S32[:]], writes=[P + "S32f%d" % c])
        for c in range(NT - 2, -1, -1):
            S.op("vector", lambda e, c=c, h=h: e.scalar_tensor_tensor(S32[:, c, 64:128], S32[:, c + 1, 64:128], COL[0:64, h * 6 + 5:h * 6 + 6], S32[:, c, 64:128], ALU.mult, ALU.add),
                 reads=[P + "S32b%d" % (c + 1), P + "S32b%d" % c, S32[:]], writes=[P + "S32b%d" % c])
        S.op("vector", lambda e: e.tensor_copy(Sst[:], S32[:, 0:NT, :]), reads=allS, writes=[Sst[:]])
        def front_r(c):
            cs = slice(c * 128, (c + 1) * 128)
            S.mm(pST[c % 2], kr[:, cs], qr[:, cs])
            S.mm(pX[c % 2], qr[:, cs], Sst[:, c, :])

        front_r(0)
        for c in range(NT):
            if c + 1 < NT:
                front_r(c + 1)
            ps_ = pST[c % 2]
            pt = PT[c % 3]
            S.tt(pt[:], ps_, DT[:, h, :], ALU.mult, eng="vector")
            pin = pIn[c % 2]
            S.mm(pin, pt[:], vbf[:, c, :])
            px = pX[c % 2]
            t = tb[c % 3]
            S.act(t[:], px[:, 0:64], AF.Copy, scale=c6(0))
            S.stt(t[:], px[:, 64:128], c6(1), t[:], ALU.mult, ALU.add, eng="vector")
            S.op("vector", lambda e, c=c, h=h, pin=pin, t=t: e.tensor_tensor(osb[:, c, h, :], pin, t[:], ALU.add),
                 reads=[pin, t[:]], writes=[P + "osb%d_%d" % (c, h)])
    G = NT * NH
    allosb = [P + "osb%d_%d" % (c, h) for c in range(NT) for h in range(NH)]
    o3 = osb[:].rearrange("p t h d -> p (t h) d")
    o2 = osb[:].rearrange("p t h d -> p (t h d)")
    s1 = S.sb([128, G], F32, "s1"); s2 = S.sb([128, G], F32, "s2"); mu = S.sb([128, G], F32, "mu")
    rs = S.sb([128, G], F32, "rs"); nb = S.sb([128, G], F32, "nb")
    W_ = NH * 64
    for hh in range(0, NT * W_, 4096):
        n_ = min(4096, NT * W_ - hh)
        g0, g1 = hh // 64, (hh + n_) // 64
        S.op("scalar", lambda e, hh=hh, n_=n_: e.activation(scrA[:, 0:n_], o2[:, hh:hh + n_], AF.Square), reads=allosb, writes=[scrA[:], P + "scrA_b"])
        S.op("vector", lambda e, n_=n_, g0=g0, g1=g1: e.reduce_sum(s2[:, g0:g1], scrA[:, 0:n_].rearrange("p (g d) -> p g d", d=64), AX.X), reads=[scrA[:]], writes=[s2[:]])
    S.op("vector", lambda e: e.reduce_sum(s1[:], o3, AX.X), reads=allosb, writes=[s1[:]])
    S.ts(mu[:], s1[:], 1.0 / 64, None, ALU.mult)
    S.tt(s1[:], mu[:], mu[:], ALU.mult)
    S.stt(rs[:], s2[:], 1.0 / 64, s1[:], ALU.mult, ALU.subtract)
    S.ts(rs[:], rs[:], gn_eps, None, ALU.add)
    S.act(rs[:], rs[:], AF.Sqrt)
    S.op("vector", lambda e: e.reciprocal(rs[:], rs[:]), reads=[rs[:]], writes=[rs[:]])
    S.stt(nb[:], mu[:], -1.0, rs[:], ALU.mult, ALU.mult)
    for gi in range(G):
        S.op("scalar", lambda e, gi=gi: e.activation(o3[:, gi, :], o3[:, gi, :], AF.Identity, bias=nb[:, gi:gi + 1], scale=rs[:, gi:gi + 1]),
             reads=[allosb[gi], nb[:], rs[:]], writes=[allosb[gi]])
    gv = g.rearrange("(t p) d -> p t d", p=128)
    o4 = osb[:].rearrange("p t h d -> p t (h d)")
    TC = 4096 // W_
    for t0 in range(0, NT, TC):
        sB = scrB[:, 0:TC * W_].rearrange("p (t d) -> p t d", d=W_)
        S.dma(sB, gv[:, t0:t0 + TC, :], reads=[g], writes=[scrB[:]])
        S.act(scrB[:, 0:TC * W_], scrB[:, 0:TC * W_], AF.Silu)
        S.op("vector", lambda e, t0=t0, sB=sB: e.tensor_tensor(o4[:, t0:t0 + TC, :], o4[:, t0:t0 + TC, :], sB, ALU.mult),
             reads=allosb + [scrB[:]], writes=[P + "osbfin%d" % t0])
    S.dma(y.rearrange("(t p) d -> p t d", p=128), o4, reads=[P + "osbfin%d" % t0 for t0 in range(0, NT, TC)], writes=[P + "yout"])


def emit_ssd(S, L, xr, br, cr, z, dtr, cw, cb, dtb, alog, dsk, consts, y):
    NT = L // 128
    P = S.prefix
    CN = S.sb([128, 5, 128], F32, "CN")
    for i in range(5):
        S.dma(CN[:, i, :], consts[i])
    triF, triB, mF, mB, idt = (CN[:, i, :] for i in range(5))
    ones = S.sb([128, 128], F32, "ones"); S.memset(ones[:], 1.0)
    CW = S.sb([128, 3, 5], F32, "CW"); CB = S.sb([128, 3], F32, "CB"); DSK = S.sb([128, 2], F32, "DSK")
    for i in range(3):
        S.dma(CW[:, i, :], cw[i]); S.dma(CB[:, i:i + 1], cb[i])
    S.dma(DSK[:], dsk)
    xin = S.sb([128, L + 4], F32, "xin")
    acc = S.sb([128, L], F32, "acc")
    XS = S.sb([128, L], F32, "XS")
    Bf = S.sb([128, L], F32, "Bf")
    BT = S.sb([128, L], BF16, "BT"); CT = S.sb([128, L], BF16, "CT")
    for i, src in enumerate((xr, br, cr)):
        S.op("gpsimd", lambda e: e.memset(xin[:, 0:2], 0.0), reads=[], writes=[xin[:]])
        S.op("gpsimd", lambda e: e.memset(xin[:, L + 2:L + 4], 0.0), reads=[], writes=[xin[:]])
        S.dma(xin[:, 2:L + 2], src, reads=[src], writes=[xin[:]])
        S.ts(acc[:], xin[:, 0:L], CW[:, i, 0:1], None, ALU.mult)
        for w in range(1, 5):
            S.stt(acc[:], xin[:, w:w + L], CW[:, i, w:w + 1], acc[:], ALU.mult, ALU.add)
        if i == 0:
            S.act(XS[:], acc[:], AF.Silu, bias=CB[:, 0:1])
        elif i == 1:
            S.act(Bf[:], acc[:], AF.Silu, bias=CB[:, 1:2])
            S.copy(BT[:], Bf[:], eng="gpsimd")
        else:
            S.act(CT[:], acc[:], AF.Silu, bias=CB[:, 2:3])
    DT = S.sb([128, NT, 4], F32, "DT"); T1 = S.sb([128, NT, 4], F32, "T1"); DTA = S.sb([128, NT, 4], F32, "DTA")
    tokv = lambda a: a.rearrange("(t p) d -> p t d", p=128)
    S.dma(DT[:], tokv(dtr), allow_slow_non_contiguous=True); S.dma(T1[:], tokv(dtb))
    S.tt(DT[:], DT[:], T1[:], ALU.add)
    S.act(DT[:], DT[:], AF.Exp)
    S.act(DT[:], DT[:], AF.Ln, bias=1.0)
    S.dma(T1[:], tokv(alog))
    S.act(T1[:], T1[:], AF.Exp)
    S.stt(DTA[:], T1[:], -1.0, DT[:], ALU.mult, ALU.mult)
    dta2 = DTA[:].rearrange("p t d -> p (t d)")
    pA = S.ps([128, 512], F32, "pA"); pB = S.ps([128, 512], F32, "pB"); pC = S.ps([128, 512], F32, "pC")
    CS = S.sb([128, NT, 4], F32, "CS"); TOT = S.sb([128, NT, 4], F32, "TOT")
    S.mm(pA[:, 0:NT * 4], triF, dta2)
    S.mm(pB[:, 0:NT * 4], triB, dta2)
    S.mm(pC[:, 0:NT * 4], ones[:], dta2)
    pA3 = pA[:, 0:NT * 4].rearrange("p (t d) -> p t d", d=4); pB3 = pB[:, 0:NT * 4].rearrange("p (t d) -> p t d", d=4)
    S.op("vector", lambda e: e.tensor_copy(CS[:, :, 0:2], pA3[:, :, 0:2]), reads=[pA], writes=[P + "CSf"])
    S.op("vector", lambda e: e.tensor_copy(CS[:, :, 2:4], pB3[:, :, 2:4]), reads=[pB], writes=[P + "CSb"])
    S.copy(TOT[:].rearrange("p t d -> p (t d)"), pC[:, 0:NT * 4], eng="vector")
    TE = S.sb([128, NT, 4], F32, "TE"); ECS = S.sb([128, NT, 4], F32, "ECS"); ED = S.sb([128, NT, 4], F32, "ED")
    S.op("vector", lambda e: e.tensor_tensor(TE[:], TOT[:], CS[:], ALU.subtract), reads=[TOT[:], P + "CSf", P + "CSb"], writes=[TE[:]])
    S.act(TE[:], TE[:], AF.Exp)
    S.op("scalar", lambda e: e.activation(ECS[:], CS[:], AF.Exp), reads=[P + "CSf", P + "CSb"], writes=[ECS[:]])
    S.act(ED[:], TOT[:], AF.Exp)
    XTK = S.sb([128, NT, 128], F32, "XTK")
    BTK = S.sb([128, NT, 128], BF16, "BTK")
    XDT = S.sb([128, NT, 256], BF16, "XDT")
    XTE = S.sb([128, NT, 256], BF16, "XTE")
    pT = [S.ps([128, 512], F32, "pT%d" % i) for i in range(2)]
    for c in range(NT):
        cs_ = slice(c * 128, (c + 1) * 128)
        p = pT[c % 2]
        S.tr(p[:, 0:128], XS[:, cs_], idt)
        S.tr(p[:, 128:256], Bf[:, cs_], idt)
        S.op("vector", lambda e, c=c, p=p: e.tensor_copy(XTK[:, c, :], p[:, 0:128]), reads=[p], writes=[P + "XTK%d" % c])
        S.op("vector", lambda e, c=c, p=p: e.tensor_copy(BTK[:, c, :], p[:, 128:256]), reads=[p], writes=[P + "BTK%d" % c])
        for s4 in range(4):
            h = s4 % 2
            S.op("gpsimd", lambda e, c=c, s4=s4, h=h: e.tensor_scalar(XDT[:, c, s4 * 64:(s4 + 1) * 64], XTK[:, c, h * 64:(h + 1) * 64], DT[:, c, s4:s4 + 1], None, ALU.mult),
                 reads=[P + "XTK%d" % c, DT[:]], writes=[P + "XDT%d_%d" % (c, s4)])
            S.op("gpsimd", lambda e, c=c, s4=s4: e.tensor_scalar(XTE[:, c, s4 * 64:(s4 + 1) * 64], XDT[:, c, s4 * 64:(s4 + 1) * 64], TE[:, c, s4:s4 + 1], None, ALU.mult),
                 reads=[P + "XDT%d_%d" % (c, s4), TE[:]], writes=[P + "XTE%d_%d" % (c, s4)])
    P32 = S.sb([128, NT, 256], F32, "P32")
    PBF = S.sb([128, NT, 256], BF16, "PBF")
    S.memset(P32[:], 0.0, eng="gpsimd")
    for c in range(NT):
        p = pT[c % 2]
        S.op("tensor", lambda e, c=c, p=p: e.matmul(p[:, 0:256], BTK[:, c, :], XTE[:, c, :], start=True, stop=True),
             reads=[P + "BTK%d" % c] + [P + "XTE%d_%d" % (c, s4) for s4 in range(4)], writes=[p])
        if c + 1 < NT:
            S.op("vector", lambda e, c=c, p=p: e.tensor_copy(P32[:, c + 1, 0:128], p[:, 0:128]), reads=[p, P32[:]], writes=[P + "Pf%d" % (c + 1)])
        if c >= 1:
            S.op("vector", lambda e, c=c, p=p: e.tensor_copy(P32[:, c - 1, 128:256], p[:, 128:256]), reads=[p, P32[:]], writes=[P + "Pb%d" % (c - 1)])
    for c in range(1, NT):
        for h in range(2):
            sl = slice(h * 64, (h + 1) * 64)
            S.op("vector", lambda e, c=c, h=h, sl=sl: e.scalar_tensor_tensor(P32[:, c, sl], P32[:, c - 1, sl], ED[:, c - 1, h:h + 1], P32[:, c, sl], ALU.mult, ALU.add),
                 reads=[P + "Pf%d" % (c - 1), P + "Pf%d" % c, P32[:], ED[:]], writes=[P + "Pf%d" % c])
    for c in range(NT - 2, -1, -1):
        for h in range(2):
            sl = slice(128 + h * 64, 128 + (h + 1) * 64)
            S.op("vector", lambda e, c=c, h=h, sl=sl: e.scalar_tensor_tensor(P32[:, c, sl], P32[:, c + 1, sl], ED[:, c + 1, 2 + h:3 + h], P32[:, c, sl], ALU.mult, ALU.add),
                 reads=[P + "Pb%d" % (c + 1), P + "Pb%d" % c, P32[:], ED[:]], writes=[P + "Pb%d" % c])
    S.op("gpsimd", lambda e: e.tensor_copy(PBF[:], P32[:]), reads=[P32[:]] + [P + "Pf%d" % c for c in range(NT)] + [P + "Pb%d" % c for c in range(NT)], writes=[PBF[:]])
    ZS = xin[:, 0:L].rearrange("p (t d) -> p t d", d=128)
    S.dma(ZS, tokv(z), reads=[z], writes=[xin[:]]); S.act(ZS, ZS, AF.Silu)
    YO = acc[:].rearrange("p (t d) -> p t d", d=128)
    pG = pA; pBc = [S.ps([128, 512], F32, "pBc%d" % i) for i in range(2)]
    pYd = pB; pYo = pC
    GS = [S.sb([128, 128], F32, "GS%d" % i) for i in range(2)]
    REP = [S.sb([128, 128], F32, "REP%d" % i) for i in range(8)]
    SEG = [S.sb([128, 512], F32, "SEG%d" % i) for i in range(2)]
    MT = [S.sb([128, 512], BF16, "MT%d" % i) for i in range(2)]
    YD = [S.sb([128, 256], F32, "YD%d" % i) for i in range(2)]
    AC = [S.sb([128, 256], F32, "AC%d" % i) for i in range(2)]
    MK4 = S.sb([128, 512], BF16, "MK4")
    for s4 in range(4):
        S.op("vector", lambda e, s4=s4: e.tensor_copy(MK4[:, s4 * 128:(s4 + 1) * 128], mF if s4 < 2 else mB), reads=[CN[:]], writes=[MK4[:]])

    def front(c):
        cs_ = slice(c * 128, (c + 1) * 128)
        S.mm(pG[:, 0:128], BT[:, cs_], CT[:, cs_])
        S.copy(GS[c % 2][:], pG[:, 0:128], eng="scalar")
        pb = pBc[c % 2]
        for s4 in range(4):
            rep = REP[(c % 2) * 4 + s4]
            S.act(rep[:], ones[:], AF.Copy, scale=DTA[:, c, s4:s4 + 1])
            S.mm(pb[:, s4 * 128:(s4 + 1) * 128], rep[:], triF if s4 < 2 else triB)

    def back(c):
        cs_ = slice(c * 128, (c + 1) * 128)
        pb = pBc[c % 2]; seg = SEG[c % 2]; mt = MT[c % 2]; gs = GS[c % 2]
        for s4 in range(4):
            key = P + ("CSf" if s4 < 2 else "CSb")
            S.op("vector", lambda e, seg=seg, pb=pb, c=c, s4=s4: e.tensor_scalar(seg[:, s4 * 128:(s4 + 1) * 128], pb[:, s4 * 128:(s4 + 1) * 128], CS[:, c, s4:s4 + 1], 0.0, ALU.subtract, ALU.min),
                 reads=[pb, key], writes=[seg[:]])
        S.act(seg[:], seg[:], AF.Exp)
        S.tt(seg[:], seg[:], MK4[:], ALU.mult, eng="gpsimd")
        for s4 in range(4):
            S.op("vector" if s4 % 2 == 0 else "gpsimd", lambda e, seg=seg, mt=mt, gs=gs, s4=s4: e.tensor_tensor(mt[:, s4 * 128:(s4 + 1) * 128], seg[:, s4 * 128:(s4 + 1) * 128], gs[:], ALU.mult),
                 reads=[seg[:], gs[:]], writes=[mt[:]])
        for s4 in range(4):
            S.op("tensor", lambda e, mt=mt, c=c, s4=s4: e.matmul(pYd[:, s4 * 64:(s4 + 1) * 64], mt[:, s4 * 128:(s4 + 1) * 128], XDT[:, c, s4 * 64:(s4 + 1) * 64], start=True, stop=True),
                 reads=[mt[:], P + "XDT%d_%d" % (c, s4)], writes=[pYd])
        S.mm(pYo[:, 0:256], CT[:, cs_], PBF[:, c, :])
        yd, ac = YD[c % 2], AC[c % 2]
        S.copy(yd[:], pYd[:, 0:256], eng="scalar")
        for s4 in range(4):
            sl = slice(s4 * 64, (s4 + 1) * 64)
            S.op("vector", lambda e, c=c, s4=s4, sl=sl, ac=ac, yd=yd: e.scalar_tensor_tensor(ac[:, sl], pYo[:, sl], ECS[:, c, s4:s4 + 1], yd[:, sl], ALU.mult, ALU.add),
                 reads=[pYo, ECS[:], yd[:]], writes=[ac[:]])
        S.op("vector", lambda e, c=c, ac=ac: e.tensor_tensor(YO[:, c, :], ac[:, 0:128], ac[:, 128:256], ALU.add), reads=[ac[:], acc[:]], writes=[P + "YO%d" % c])
        for h in range(2):
            sl = slice(h * 64, (h + 1) * 64)
            S.op("vector", lambda e, c=c, h=h, sl=sl: e.scalar_tensor_tensor(YO[:, c, sl], XTK[:, c, sl], DSK[:, h:h + 1], YO[:, c, sl], ALU.mult, ALU.add),
                 reads=[P + "XTK%d" % c, P + "YO%d" % c, DSK[:]], writes=[P + "YO%d" % c])

    front(0)
    for c in range(NT):
        if c + 1 < NT:
            front(c + 1)
        back(c)
    S.op("vector", lambda e: e.tensor_tensor(YO, YO, ZS, ALU.mult), reads=[xin[:]] + [P + "YO%d" % c for c in range(NT)], writes=[acc[:]])
    S.dma(y.rearrange("(t p) d -> p t d", p=128), YO, reads=[acc[:]], writes=[P + "yout"])


def emit_topk(S, aff, vals, idxu, idt, L, NE=16, KSEL=512, mid_hook=None):
    NT = L // 128
    A = S.sb([128, NT, NE], F32, "A")
    S.dma(A[:], aff.rearrange("(t p) e -> p t e", p=128))
    wk = S.sb([NE, L], F32, "wk")
    vs = S.sb([NE, KSEL], F32, "vs")
    ix = S.sb([NE, KSEL], U32, "ix")
    pt = [S.ps([128, 512], F32, "pt%d" % i) for i in range(2)]
    P = S.prefix
    for t4 in range(NT // 4):
        p = pt[t4 % 2]
        for j in range(4):
            S.tr(p[0:NE, j * 128:(j + 1) * 128], A[:, t4 * 4 + j, :], idt)
        S.op("vector", lambda e, t4=t4, p=p: e.tensor_copy(wk[:, t4 * 512:(t4 + 1) * 512], p[0:NE, :]), reads=[p], writes=[P + "wk%d" % t4])
    allwk = [P + "wk%d" % t4 for t4 in range(NT // 4)]
    if mid_hook is not None:
        mid_hook()
    for it in range(KSEL // 8):
        sl = slice(it * 8, it * 8 + 8)
        S.op("vector", lambda e, sl=sl: e.max(vs[:, sl], wk[:]), reads=[wk[:]] + (allwk if it == 0 else []), writes=[P + "vs%d" % it])
        S.op("vector", lambda e, sl=sl: e.max_index(ix[:, sl], vs[:, sl], wk[:]), reads=[wk[:], P + "vs%d" % it], writes=[P + "ix%d" % it])
        S.op("vector", lambda e, sl=sl: e.match_replace(wk[:], vs[:, sl], wk[:], -1e30), reads=[wk[:], P + "vs%d" % it], writes=[wk[:]])
    S.dma(vals, vs[:], reads=[P + "vs%d" % i for i in range(KSEL // 8)], writes=[P + "vout"])
    S.dma(idxu, ix[:], queue="gpsimd", reads=[P + "ix%d" % i for i in range(KSEL // 8)], writes=[P + "iout"])


def emit_moe(S, x1k, vals, idxu, g2, wg, wu, wd, x2k, idt, L, NE=16, eps=1e-6):
    P = S.prefix
    NS = NE * 2
    G2 = S.sb([128, 8], F32, "G2")
    S.dma(G2[:], g2.rearrange("(k p) -> p k", p=128), allow_slow_non_contiguous=True)
    WG = [S.sb([128, 8, 1024], BF16, "WG%d" % i) for i in range(2)]
    WU = [S.sb([128, 8, 1024], BF16, "WU%d" % i) for i in range(2)]
    WD = [S.sb([128, 8, 1024], BF16, "WD%d" % i) for i in range(2)]
    XGT = [S.sb([128, 4, 1024], F32, "XGT%d" % i) for i in range(2)]
    IX = [S.sb([128, 4], U32, "IX%d" % i) for i in range(2)]
    GT = [S.sb([128, 4], F32, "GT%d" % i) for i in range(2)]
    SS = [S.sb([128, 4], F32, "SS%d" % i) for i in range(2)]
    SQ = S.sb([128, 1024], F32, "SQ")
    XNs = [S.sb([128, 8, 512], BF16, "XN%d" % i) for i in range(2)]
    HID = S.sb([128, 8, 512], BF16, "HID")
    ACC = S.sb([128, 4, 1024], F32, "ACC")
    GS = [S.sb([128, 512], F32, "GS%d" % i) for i in range(2)]
    OB = [S.sb([128, 1024], F32, "OB%d" % i) for i in range(2)]
    pt = [S.ps([128, 512], F32, "pt%d" % i) for i in range(2)]
    pg = [S.ps([128, 512], F32, "pg%d" % i) for i in range(2)]
    pu = [S.ps([128, 512], F32, "pu%d" % i) for i in range(2)]
    po = [S.ps([128, 512], F32, "po%d" % i) for i in range(2)]

    def load_w(s):
        e_, hf = divmod(s, 2)
        sl = s % 2
        fs = slice(hf * 1024, (hf + 1) * 1024)
        S.dma(WG[sl][:], wg[e_].rearrange("(k p) f -> p k f", p=128)[:, :, fs], queue="gpsimd")
        S.dma(WU[sl][:], wu[e_].rearrange("(k p) f -> p k f", p=128)[:, :, fs], queue="gpsimd")
        S.dma(WD[sl][:], wd[e_][fs, :].rearrange("(k p) d -> p k d", p=128), queue="gpsimd")

    def gather(e_):
        b = e_ % 2
        S.dma(IX[b][:], idxu[e_].rearrange("(j p) -> p j", p=128), queue="sync", allow_slow_non_contiguous=True)
        S.dma(GT[b][:], vals[e_].rearrange("(j p) -> p j", p=128), queue="sync", allow_slow_non_contiguous=True)
        for j in range(4):
            S.raw_dma("gpsimd", lambda e, b=b, j=j: e.indirect_dma_start(out=XGT[b][:, j, :], out_offset=None, in_=x1k,
                      in_offset=bass.IndirectOffsetOnAxis(ap=IX[b][:, j:j + 1], axis=0)),
                      reads=[IX[b][:], x1k], writes=[XGT[b][:]])

    def prep(e_):
        b = e_ % 2
        XN = XNs[b]
        for j in range(4):
            S.op("scalar", lambda e, b=b, j=j: e.activation(SQ[:], XGT[b][:, j, :], AF.Square, accum_out=SS[b][:, j:j + 1]),
                 reads=[XGT[b][:]], writes=[SQ[:], SS[b][:]])
        S.ts(SS[b][:], SS[b][:], 1.0 / 1024, eps, ALU.mult, ALU.add)
        S.act(SS[b][:], SS[b][:], AF.Sqrt)
        S.op("vector", lambda e, b=b: e.reciprocal(SS[b][:], SS[b][:]), reads=[SS[b][:]], writes=[SS[b][:]])
        for j in range(4):
            S.op("vector" if j % 2 == 0 else "gpsimd", lambda e, b=b, j=j: e.tensor_scalar(XGT[b][:, j, :], XGT[b][:, j, :], SS[b][:, j:j + 1], None, ALU.mult),
                 reads=[XGT[b][:], SS[b][:]], writes=[XGT[b][:]])
        for k in range(8):
            p = pt[k % 2]
            for j in range(4):
                S.tr(p[:, j * 128:(j + 1) * 128], XGT[b][:, j, k * 128:(k + 1) * 128], idt)
            if k % 2 == 0:
                S.op("scalar", lambda e, k=k, p=p, XN=XN: e.activation(XN[:, k, :], p, AF.Copy, scale=G2[:, k:k + 1]), reads=[p, G2[:]], writes=[XN[:]])
            else:
                S.op("vector", lambda e, k=k, p=p, XN=XN: e.tensor_scalar(XN[:, k, :], p, G2[:, k:k + 1], None, ALU.mult), reads=[p, G2[:]], writes=[XN[:]])

    load_w(0)
    gather(0)
    prep(0)
    for s in range(NS):
        e_, hf = divmod(s, 2)
        sl = s % 2
        b = e_ % 2
        XN = XNs[b]
        if s + 1 < NS:
            load_w(s + 1)
        if hf == 0 and e_ + 1 < NE:
            gather(e_ + 1)
        for f in range(8):
            a, u = pg[f % 2], pu[f % 2]
            for k in range(8):
                S.op("tensor", lambda e, a=a, k=k, f=f, sl=sl, XN=XN: e.matmul(a, WG[sl][:, k, f * 128:(f + 1) * 128], XN[:, k, :], start=(k == 0), stop=(k == 7)),
                     reads=[WG[sl][:], XN[:]], writes=[a])
            for k in range(8):
                S.op("tensor", lambda e, u=u, k=k, f=f, sl=sl, XN=XN: e.matmul(u, WU[sl][:, k, f * 128:(f + 1) * 128], XN[:, k, :], start=(k == 0), stop=(k == 7)),
                     reads=[WU[sl][:], XN[:]], writes=[u])
            gs = GS[f % 2]
            S.act(gs[:], a, AF.Silu)
            S.op("vector", lambda e, f=f, u=u, gs=gs: e.tensor_tensor(HID[:, f, :], u, gs[:], ALU.mult), reads=[u, gs[:]], writes=[HID[:]])
        if hf == 1 and e_ + 1 < NE:
            prep(e_ + 1)
        for j in range(4):
            ob = OB[j % 2]
            for dh in range(2):
                p = po[dh]
                ds_ = slice(dh * 512, (dh + 1) * 512)
                for k in range(8):
                    S.op("tensor", lambda e, p=p, k=k, j=j, ds_=ds_, sl=sl: e.matmul(p, HID[:, k, j * 128:(j + 1) * 128], WD[sl][:, k, ds_], start=(k == 0), stop=(k == 7)),
                         reads=[HID[:], WD[sl][:]], writes=[p])
                if hf == 0:
                    if dh == 0:
                        S.op("scalar", lambda e, p=p, j=j, ds_=ds_: e.copy(ACC[:, j, ds_], p), reads=[p], writes=[ACC[:]])
                    else:
                        S.op("vector", lambda e, p=p, j=j, ds_=ds_: e.tensor_copy(ACC[:, j, ds_], p), reads=[p], writes=[ACC[:]])
                else:
                    S.op("vector", lambda e, p=p, j=j, ds_=ds_, ob=ob: e.tensor_tensor(ob[:, ds_], p, ACC[:, j, ds_], ALU.add), reads=[p, ACC[:]], writes=[ob[:]])
            if hf == 1:
                S.op("scalar", lambda e, ob=ob, j=j, b=b: e.activation(ob[:], ob[:], AF.Copy, scale=GT[b][:, j:j + 1]), reads=[ob[:], GT[b][:]], writes=[ob[:]])
                S.raw_dma("gpsimd", lambda e, ob=ob, j=j, b=b: e.indirect_dma_start(out=x2k, out_offset=bass.IndirectOffsetOnAxis(ap=IX[b][:, j:j + 1], axis=0),
                          in_=ob[:], in_offset=None, compute_op=ALU.add), reads=[ob[:], IX[b][:]], writes=[P + "x2k"])


L_ = 4096
NF_, NT_ = 2208, 1288

def build_fused(depth=2, upto=99, debug=False):
    nc = bass.Bass("TRN2", target_bir_lowering=False)
    L = L_
    def din(n, s, dt=F32):
        return nc.dram_tensor(n, s, dt, kind="ExternalInput").ap()
    def scr(n, s, dt=F32):
        if debug:
            return nc.dram_tensor(n, s, dt, kind="ExternalOutput").ap()
        return nc.dram_tensor(n, s, dt, kind="Internal", addr_space="Local").ap()
    xT = din("xT", [1024, L])
    Wl = []
    for l in range(depth):
        d = {}
        for n, s in (("wF", [1024, NF_]), ("wT", [1024, NT_]), ("ln1", [1024]), ("wuq", [256, 384]), ("qn", [256]),
                     ("wkn", [128, 256]), ("wv", [128, 256]), ("kvn", [128]), ("cw", [2, 3, 128, 5]), ("cb", [2, 3, 128, 1]),
                     ("dtb", [2, L, 4]), ("alog", [2, L, 4]), ("dsk", [2, 128, 2]), ("snw", [256]), ("wout", [1024, 1024]),
                     ("ln2", [1024]), ("rw", [1024, 16]), ("wg", [16, 1024, 2048]), ("wu", [16, 1024, 2048]), ("wd", [16, 2048, 1024])):
            d[n] = din("%s%d" % (n, l), s)
        Wl.append(d)
    fnw = din("fnw", [1024]); ones1k = din("ones1k", [1024])
    C = {}
    for n, s in (("rck", [64, L]), ("rsk", [64, L]), ("rct", [L, 64]), ("rst", [L, 64]), ("rdtot", [4, 128, 128]), ("rcols", [2, 128, 12]),
                 ("mcq", [96, L]), ("msq", [96, L]), ("mck", [96, L]), ("msk", [96, L]),
                 ("dcq", [64, L]), ("dsq", [64, L]), ("dck", [64, L]), ("dsk_", [64, L]), ("dmask", [20, 128, 512]),
                 ("ident", [128, 128]), ("sconst", [5, 128, 128]), ("tokid", [128, 32]), ("iota512", [128, 512])):
        C[n] = din(n, s)
    out = nc.dram_tensor("out", [L, 1024], F32, kind="ExternalOutput").ap()
    PF = scr("PF", [NF_, L]); PT = scr("PT", [L, NT_]); QF = scr("QF", [384, L]); KF = scr("KF", [256, L]); VT = scr("VT", [L, 256])
    YA, YB, YC, YD = (scr(n, [L, 256]) for n in ("YA", "YB", "YC", "YD"))
    MT = scr("MT", [1024, L]); X1T = scr("X1T", [1024, L])
    AFF = scr("AFF", [L, 16]); VALS = scr("VALS", [16, 512]); IDXU = scr("IDXU", [16, 512], U32)
    X1K = scr("X1K", [L, 1024]); X2K = scr("X2K", [L, 1024])
    X2T = [scr("X2T%d" % l, [1024, L]) for l in range(depth)]
    with ExitStack() as es:
        S = Sched(nc, es)
        with S.scope("c_"):
            pass
        IDT = S.sb([128, 128], F32, "IDT"); S.dma(IDT[:], C["ident"])
        idt = IDT[:]
        stage = [0]
        def go():
            stage[0] += 1
            return stage[0] <= upto
        cur = xT
        for l in range(depth):
            W = Wl[l]; pf = "l%d" % l
            if go():
                with S.scope(pf + "a_"):
                    emit_lin(S, cur, W["wT"], W["ln1"], PT, 1024, L, NT_, tag=pf + "a", NB=3)
            if go():
                with S.scope(pf + "b_"):
                    emit_linF(S, cur, W["wF"], W["ln1"], PF, 1024, L, NF_, tag=pf + "b")
            if go():
                for c2 in range(2):
                    hs = [2 * c2, 2 * c2 + 1]
                    with S.scope(pf + "r%d_" % c2):
                        emit_ret(S, L, 2, q=[PF[h * 64:(h + 1) * 64] for h in hs], k=[PF[256 + h * 64:256 + (h + 1) * 64] for h in hs],
                                 kt_=[PT[:, h * 64:(h + 1) * 64] for h in hs],
                                 v=[PT[:, 256 + h * 64:256 + (h + 1) * 64] for h in hs], g=PT[:, 512 + c2 * 128:512 + (c2 + 1) * 128],
                                 ck=C["rck"], sk=C["rsk"], ct=C["rct"], st_=C["rst"], dtot=C["rdtot"][2 * c2:2 * c2 + 2], cols=C["rcols"][c2],
                                 y=YA[:, c2 * 128:(c2 + 1) * 128])
            if go():
                with S.scope(pf + "m1_"):
                    emit_linF(S, PF[512:768], W["wuq"], W["qn"], QF, 256, L, 384, tag=pf + "m1")
                with S.scope(pf + "m2_"):
                    emit_linF(S, PF[768:896], W["wkn"], W["kvn"], KF, 128, L, 256, tag=pf + "m2")
                with S.scope(pf + "m3_"):
                    emit_lin(S, PF[768:896], W["wv"], W["kvn"], VT, 128, L, 256, tag=pf + "m3")
            if go():
                for c2 in range(2):
                    hs = [2 * c2, 2 * c2 + 1]
                    with S.scope(pf + "ma%d_" % c2):
                        emit_attn(S, 96, 64, L, 2, mla_plan(), 0, q=[[(0, 96, QF[h * 96:(h + 1) * 96])] for h in hs],
                                  qp=[[(0, 64, QF[h * 96:h * 96 + 64]), (64, 80, QF[h * 96 + 80:h * 96 + 96]), (80, 96, QF[h * 96 + 64:h * 96 + 80])] for h in hs],
                                  kpc=[[(0, 64, KF[h * 64:(h + 1) * 64]), (64, 96, PF[896:928])] for h in hs],
                                  kppc=[[(0, 64, KF[h * 64:(h + 1) * 64]), (64, 80, PF[912:928]), (80, 96, PF[896:912])] for h in hs],
                                  tabs_d=[C["mcq"], C["msq"], C["mck"], C["msk"]], v=[VT[:, h * 64:(h + 1) * 64] for h in hs], masks=None, idt=idt,
                                  y=YB[:, c2 * 128:(c2 + 1) * 128], tag=pf + "ma%d" % c2)
            if go():
                for g_ in range(2):
                    with S.scope(pf + "s%d_" % g_):
                        emit_ssd(S, L, PF[928 + g_ * 128:928 + (g_ + 1) * 128], PF[1184 + g_ * 128:1184 + (g_ + 1) * 128], PF[1440 + g_ * 128:1440 + (g_ + 1) * 128],
                                 z=PT[:, 768 + g_ * 128:768 + (g_ + 1) * 128], dtr=PT[:, 1024 + g_ * 4:1024 + (g_ + 1) * 4], cw=W["cw"][g_], cb=W["cb"][g_],
                                 dtb=W["dtb"][g_], alog=W["alog"][g_], dsk=W["dsk"][g_], consts=C["sconst"], y=YC[:, g_ * 128:(g_ + 1) * 128])
            if go():
                for c2 in range(2):
                    hs = [2 * c2, 2 * c2 + 1]
                    with S.scope(pf + "d%d_" % c2):
                        dq_ = lambda h: PF[1696 + h * 64:1696 + (h + 1) * 64]
                        dk_ = lambda h: PF[1952 + h * 64:1952 + (h + 1) * 64]
                        sw_ = lambda a: [(0, 8, a[8:16]), (8, 16, a[0:8]), (16, 64, a[16:64])]
                        emit_attn(S, 64, 64, L, 2, dil_plan(), 20, q=[[(0, 64, dq_(h))] for h in hs], qp=[sw_(dq_(h)) for h in hs],
                                  kpc=[[(0, 64, dk_(h))] for h in hs], kppc=[sw_(dk_(h)) for h in hs],
                                  tabs_d=[C["dcq"], C["dsq"], C["dck"], C["dsk_"]], v=[PT[:, 1032 + h * 64:1032 + (h + 1) * 64] for h in hs], masks=C["dmask"], idt=idt,
                                  y=YD[:, c2 * 128:(c2 + 1) * 128], tag=pf + "d%d" % c2)
            if go():
                with S.scope(pf + "t_"):
                    for i, (src, nw) in enumerate(((YA, None), (YB, None), (YC, W["snw"]), (YD, None))):
                        emit_transpose(S, src, MT[i * 256:(i + 1) * 256], L, 256, idt, norm_w=nw, tag=pf + "t%d" % i, nm="m%d" % i)
            if go():
                with S.scope(pf + "o_"):
                    emit_linF(S, MT, W["wout"], ones1k, X1T, 1024, L, 1024, norm=False, resid=cur, tag=pf + "o")
            if go():
                with S.scope(pf + "q_"):
                    emit_lin(S, X1T, W["rw"], W["ln2"], AFF, 1024, L, 16, softmax=True, fp32=True, tag=pf + "q", NB=4)
                with S.scope(pf + "k_"):
                    emit_topk(S, AFF, VALS, IDXU, idt, L,
                              mid_hook=lambda: emit_untranspose(S, X1T, X1K, L, 1024, idt, tag=pf + "u", evac="scalar", dst2=X2K))
            if go():
                with S.scope(pf + "e_"):
                    emit_moe(S, X1K, VALS, IDXU, W["ln2"], W["wg"], W["wu"], W["wd"], X2K, idt, L)
            if go():
                with S.scope(pf + "c_"):
                    emit_transpose(S, X2K, X2T[l], L, 1024, idt, tag=pf + "c")
            cur = X2T[l]
            S.new_epoch()
        if go():
            with S.scope("fin_"):
                emit_finalnorm(S, cur, fnw, out, L, idt)
        else:
            with S.scope("fin_"):
                z_ = S.sb([128, 1024], F32, "z_"); S.memset(z_[:], 0.0)
                S.dma(out[0:128, :], z_[:])
        S.finish()
    return nc


def _perm_ret():
    return np.array([h * 64 + ((d + 32) % 64) for h in range(4) for d in range(64)])

def _perm_dil():
    def sw(d):
        return d + 8 if d < 8 else (d - 8 if d < 16 else d)
    return np.array([h * 64 + sw(d) for h in range(4) for d in range(64)])

def host_consts():
    L = L_
    C = {}
    rc = ret_consts(L, [0, 1, 2, 3])
    C["rck"], C["rsk"], C["rct"], C["rst"], C["rdtot"] = rc["ck"], rc["sk"], rc["ct"], rc["st"], rc["dtot"]
    C["rcols"] = np.ascontiguousarray(np.stack([rc["cols"][:, 0:12], rc["cols"][:, 12:24]]))
    C["mcq"], C["msq"] = rot_tables(96, L, 64, 96, 500000.0, 96 ** -0.5)
    C["mck"], C["msk"] = rot_tables(96, L, 64, 96, 500000.0, 1.0)
    C["dcq"], C["dsq"] = rot_tables(64, L, 0, 16, 500000.0, 64 ** -0.5)
    C["dck"], C["dsk_"] = rot_tables(64, L, 0, 16, 500000.0, 1.0)
    C["dmask"] = dil_masks()
    C["ident"] = np.eye(128, dtype=np.float32)
    C["sconst"] = ssd_consts()
    C["tokid"] = (np.arange(128)[:, None] + 128 * np.arange(32)[None, :]).astype(np.float32)
    C["iota512"] = np.broadcast_to(np.arange(512, dtype=np.float32)[None, :], (128, 512)).copy()
    return C

def host_layer_weights(P, l):
    L = L_
    f = lambda a: np.ascontiguousarray(a, dtype=np.float32)
    w = P['w_in'][l]
    rq, rk, rv, rg = w[:, 0:256], w[:, 256:512], w[:, 512:768], w[:, 768:1024]
    cq, ckv, kr = w[:, 1024:1280], w[:, 1280:1408], w[:, 1408:1440]
    sz, sxbc, sdt = w[:, 1440:1696], w[:, 1696:2464], w[:, 2464:2472]
    dq, dk, dv = w[:, 2472:2728], w[:, 2728:2984], w[:, 2984:3240]
    pr, pd = _perm_ret(), _perm_dil()
    krs = np.concatenate([kr[:, 16:32], kr[:, 0:16]], axis=1)
    wF = np.concatenate([rq, rk, cq, ckv, kr, sxbc, dq, dk], axis=1)
    dtc = [0, 1, 4, 5, 2, 3, 6, 7]
    wT = np.concatenate([rk, rv, rg, sz, sdt[:, dtc], dv], axis=1)
    assert wF.shape[1] == NF_ and wT.shape[1] == NT_
    uq = P['mla_w_uq'][l]
    def swq(j):
        return j + 16 if 64 <= j < 80 else (j - 16 if 80 <= j < 96 else j)
    pq = np.array([h * 96 + swq(j) for h in range(4) for j in range(96)])
    wuq = uq
    ukv = P['mla_w_ukv'][l].reshape(128, 4, 128)
    wkn = ukv[:, :, 0:64].reshape(128, 256); wv = ukv[:, :, 64:128].reshape(128, 256)
    cwt = np.zeros((2, 3, 128, 5), np.float32); cbt = np.zeros((2, 3, 128, 1), np.float32)
    dtb = np.zeros((2, L, 4), np.float32); alog = np.zeros((2, L, 4), np.float32); dsk = np.zeros((2, 128, 2), np.float32)
    for g_ in range(2):
        for i, c0 in enumerate((g_ * 128, 256 + g_ * 128, 512 + g_ * 128)):
            cols = list(range(c0, c0 + 128))
            cwt[g_, i] = P['ssm_conv_w'][l][:, cols].T
            cbt[g_, i, :, 0] = P['ssm_conv_b'][l][cols]
        hs = [2 * g_, 2 * g_ + 1]
        dcols = [hs[0], hs[1], 4 + hs[0], 4 + hs[1]]
        dtb[g_] = P['ssm_dt_bias'][l].reshape(8)[dcols][None]
        alog[g_] = P['ssm_a_log'][l].reshape(8)[dcols][None]
        dsk[g_] = P['ssm_d'][l][hs][None]
    return dict(wF=f(wF), wT=f(wT), ln1=f(P['ln1_w'][l]), wuq=f(wuq), qn=f(P['mla_q_norm_w'][l]), wkn=f(wkn), wv=f(wv),
                kvn=f(P['mla_kv_norm_w'][l]), cw=cwt, cb=cbt, dtb=dtb, alog=alog, dsk=dsk, snw=f(P['ssm_norm_w'][l]),
                wout=f(P['w_out'][l]), ln2=f(P['ln2_w'][l]), rw=f(P['router_w'][l]), wg=f(P['exp_w_gate'][l]), wu=f(P['exp_w_up'][l]),
                wd=f(P['exp_w_down'][l]))

def kernel(**inputs):
    P = {k: np.asarray(v) for k, v in inputs.items()}
    x = np.asarray(P['x'], dtype=np.float32)
    nc = build_fused(depth=2)
    base = dict(host_consts())
    base["fnw"] = np.ascontiguousarray(P['final_norm_w'], dtype=np.float32)
    base["ones1k"] = np.ones(1024, np.float32)
    for l in range(2):
        for k_, v_ in host_layer_weights(P, l).items():
            base["%s%d" % (k_, l)] = v_
    in_maps = []
    for b in range(4):
        m = dict(base)
        m["xT"] = np.ascontiguousarray(x[b].T)
        in_maps.append(m)
    res = run_bass_kernel_spmd(nc, in_maps, core_ids=[0, 1, 2, 3])
    return np.stack([res.results[b]["out"] for b in range(4)]).astype(np.float32)
```
